# Optimizing a Trainium2 kernel written in Bass

```python
import math
import jax
import jax.numpy as jnp
from jax import lax
import numpy as np

D_MODEL = 1024
BATCH = 8
SEQ = 4096
DEPTH = 4

PLE_DIM = 256
D_FF = 2816
EPS = 1e-6
Q_BLOCK = 128
MLA_HEADS = 8
MLA_Q_LORA = 384
MLA_KV_LORA = 256
MLA_NOPE = 64
MLA_ROPE = 32
MLA_V = 64
MLA_QK = MLA_NOPE + MLA_ROPE
ROPE_THETA = 10000.0
GDN_HEADS = 4
GDN_DK = 128
GDN_DV = 128
GDN_CONV = 4
GDN_CHUNK = 64
GDN_KW = GDN_HEADS * GDN_DK
GDN_VW = GDN_HEADS * GDN_DV
GDN_CONV_CH = 2 * GDN_KW + GDN_VW
DSA_HEADS = 8
DSA_KV_HEADS = 2
DSA_HEAD_DIM = 128
IDX_HEADS = 8
IDX_DIM = 64
DSA_TOPK = 256
EVEN_IN = MLA_Q_LORA + MLA_KV_LORA + MLA_ROPE + GDN_CONV_CH + GDN_VW + 2 * GDN_HEADS
EVEN_OUT = MLA_HEADS * MLA_V + GDN_VW
ODD_IN = DSA_HEADS * DSA_HEAD_DIM + 2 * DSA_KV_HEADS * DSA_HEAD_DIM + IDX_HEADS * IDX_DIM + IDX_DIM + IDX_HEADS
ODD_OUT = DSA_HEADS * DSA_HEAD_DIM

kernel_name = 'hybrid_mla_gdn_dsa_macaron_ple'


def rms_norm(x, g):
    xf = x.astype(jnp.float32)
    y = xf * lax.rsqrt(jnp.mean(xf * xf, axis=-1, keepdims=True) + EPS)
    return (y * g.astype(jnp.float32)).astype(x.dtype)


def l2_norm(x):
    return x * lax.rsqrt(jnp.sum(x * x, axis=-1, keepdims=True) + EPS)


def split_last(t, sizes):
    out, off = [], 0
    for s in sizes:
        out.append(t[..., off:off + s])
        off += s
    return out


def swiglu(h, w_gu, w_down):
    g, u = jnp.split(h @ w_gu, 2, axis=-1)
    return (jax.nn.silu(g) * u) @ w_down


def rope_tail(x, pos):
    half = MLA_ROPE // 2
    inv_freq = ROPE_THETA ** (-jnp.arange(half, dtype=jnp.float32) / half)
    ang = pos.astype(jnp.float32)[:, None] * inv_freq[None, :]
    cos = jnp.cos(ang)[None, :, None, :]
    sin = jnp.sin(ang)[None, :, None, :]
    xf = x.astype(jnp.float32)
    keep, x1, x2 = xf[..., :-MLA_ROPE], xf[..., -MLA_ROPE:-half], xf[..., -half:]
    return jnp.concatenate([keep, x1 * cos - x2 * sin, x2 * cos + x1 * sin], axis=-1).astype(x.dtype)


def to_blocks(t):
    b, s = t.shape[0], t.shape[1]
    return jnp.moveaxis(t.reshape((b, s // Q_BLOCK, Q_BLOCK) + t.shape[2:]), 1, 0)


def from_blocks(t):
    t = jnp.moveaxis(t, 0, 1)
    return t.reshape((t.shape[0], t.shape[1] * t.shape[2]) + t.shape[3:])


def causal_attention(q, k, v):
    S, dk = q.shape[1], q.shape[-1]
    scale = dk ** -0.5
    kpos = jnp.arange(S)
    qpos = jnp.arange(S).reshape(S // Q_BLOCK, Q_BLOCK)

    def one_block(blk):
        qb, pb = blk
        s = jnp.einsum('bqhd,bkhd->bhqk', qb, k).astype(jnp.float32) * scale
        s = jnp.where(kpos[None, None, None, :] <= pb[None, None, :, None], s, -jnp.inf)
        pr = jax.nn.softmax(s, axis=-1).astype(v.dtype)
        return jnp.einsum('bhqk,bkhd->bqhd', pr, v)

    return from_blocks(lax.map(one_block, (to_blocks(q), qpos)))


def causal_depthwise_conv(x, w):
    K, C = w.shape
    return lax.conv_general_dilated(x, w[:, None, :].astype(x.dtype), window_strides=(1,),
                                    padding=[(K - 1, 0)], dimension_numbers=('NWC', 'WIO', 'NWC'),
                                    feature_group_count=C)


def gated_delta_chunked(q, k, v, g, beta):
    B, S, H, DK = q.shape
    DV = v.shape[-1]
    C = GDN_CHUNK
    N = S // C

    def chunks(t):
        return jnp.moveaxis(t.reshape((B, N, C, H) + t.shape[3:]), 3, 1)

    qc = chunks(q) * DK ** -0.5
    kc, vc = chunks(k), chunks(v)
    gc = jnp.cumsum(chunks(g), axis=-1)
    bc = chunks(beta)
    incl = jnp.tril(jnp.ones((C, C), dtype=bool))
    strict = jnp.tril(jnp.ones((C, C), dtype=bool), -1)
    decay = jnp.exp(jnp.where(incl, gc[..., :, None] - gc[..., None, :], -jnp.inf))
    kb = kc * bc[..., None]
    vb = vc * bc[..., None]
    a_low = jnp.where(strict, jnp.einsum('bhncd,bhnsd->bhncs', kb, kc) * decay, 0.0)
    eye = jnp.eye(C, dtype=jnp.float32)
    t_inv = lax.linalg.triangular_solve(a_low + eye, jnp.broadcast_to(eye, a_low.shape),
                                        left_side=True, lower=True, unit_diagonal=True)
    u = jnp.einsum('bhncs,bhnse->bhnce', t_inv, vb)
    w = jnp.einsum('bhncs,bhnsd->bhncd', t_inv, kb * jnp.exp(gc)[..., None])
    qk = jnp.einsum('bhncd,bhnsd->bhncs', qc, kc) * decay
    qg = qc * jnp.exp(gc)[..., None]
    kd = kc * jnp.exp(gc[..., -1:] - gc)[..., None]
    g_last = jnp.exp(gc[..., -1])

    def step(state, xs):
        u_n, w_n, qk_n, qg_n, kd_n, gl_n = xs
        v_new = u_n - jnp.einsum('bhcd,bhde->bhce', w_n, state)
        o_n = jnp.einsum('bhcd,bhde->bhce', qg_n, state) + jnp.einsum('bhcs,bhse->bhce', qk_n, v_new)
        state = state * gl_n[..., None, None] + jnp.einsum('bhcd,bhce->bhde', kd_n, v_new)
        return state, o_n

    xs = tuple(jnp.moveaxis(t, 2, 0) for t in (u, w, qk, qg, kd, g_last))
    state0 = jnp.zeros((B, H, DK, DV), jnp.float32)
    _, o = lax.scan(step, state0, xs)
    return jnp.transpose(o, (1, 0, 3, 2, 4)).reshape(B, S, H, DV)


def dsa_sparse_attention(q, k, v, q_idx, k_idx, w_idx, topk):
    B, S, HQ, DH = q.shape
    HKV = k.shape[2]
    G = HQ // HKV
    scale = DH ** -0.5
    kpos = jnp.arange(S)
    qpos = jnp.arange(S).reshape(S // Q_BLOCK, Q_BLOCK)
    bidx = jnp.arange(B)[:, None, None]

    def one_block(blk):
        qb, qib, wb, pb = blk
        rel = jax.nn.relu(jnp.einsum('bthd,bsd->bths', qib, k_idx).astype(jnp.float32))
        score = jnp.einsum('bth,bths->bts', wb.astype(jnp.float32), rel)
        score = jnp.where(kpos[None, None, :] <= pb[None, :, None], score, -jnp.inf)
        _, sel = lax.top_k(score, topk)
        valid = sel <= pb[None, :, None]
        k_sel = k[bidx, sel]
        v_sel = v[bidx, sel]
        qg = qb.reshape(B, Q_BLOCK, HKV, G, DH)
        s = jnp.einsum('btngd,btjnd->btngj', qg, k_sel).astype(jnp.float32) * scale
        s = jnp.where(valid[:, :, None, None, :], s, -jnp.inf)
        pr = jax.nn.softmax(s, axis=-1).astype(v.dtype)
        o = jnp.einsum('btngj,btjnd->btngd', pr, v_sel)
        return o.reshape(B, Q_BLOCK, HQ, DH)

    out = lax.map(one_block, (to_blocks(q), to_blocks(q_idx), to_blocks(w_idx), qpos))
    return from_blocks(out)


def even_mixer(h, w_in, q_a_norm, w_q_up, kv_a_norm, w_kv_up, q_norm, k_norm,
               conv_w, a_log, dt_bias, out_norm, w_out):
    B, S, _ = h.shape
    c_q, c_kv, k_pe, qkv, z, a, b = split_last(
        h @ w_in, [MLA_Q_LORA, MLA_KV_LORA, MLA_ROPE, GDN_CONV_CH, GDN_VW, GDN_HEADS, GDN_HEADS])
    pos = jnp.arange(S)
    q = (rms_norm(c_q, q_a_norm) @ w_q_up).reshape(B, S, MLA_HEADS, MLA_QK)
    kv = (rms_norm(c_kv, kv_a_norm) @ w_kv_up).reshape(B, S, MLA_HEADS, MLA_NOPE + MLA_V)
    k_nope, v = kv[..., :MLA_NOPE], kv[..., MLA_NOPE:]
    k = jnp.concatenate([k_nope, jnp.broadcast_to(k_pe[:, :, None, :], (B, S, MLA_HEADS, MLA_ROPE))], axis=-1)
    q = rope_tail(rms_norm(q, q_norm), pos)
    k = rope_tail(rms_norm(k, k_norm), pos)
    o_a = causal_attention(q, k, v).reshape(B, S, MLA_HEADS * MLA_V)
    qkv = jax.nn.silu(causal_depthwise_conv(qkv, conv_w)).astype(jnp.float32)
    gq, gk, gv = split_last(qkv, [GDN_KW, GDN_KW, GDN_VW])
    gq = l2_norm(gq.reshape(B, S, GDN_HEADS, GDN_DK))
    gk = l2_norm(gk.reshape(B, S, GDN_HEADS, GDN_DK))
    gv = gv.reshape(B, S, GDN_HEADS, GDN_DV)
    g = -jnp.exp(a_log.astype(jnp.float32)) * jax.nn.softplus(a.astype(jnp.float32) + dt_bias.astype(jnp.float32))
    beta = jax.nn.sigmoid(b.astype(jnp.float32))
    o_b = gated_delta_chunked(gq, gk, gv, g, beta)
    o_b = rms_norm(o_b, out_norm) * jax.nn.silu(z.astype(jnp.float32).reshape(B, S, GDN_HEADS, GDN_DV))
    o_b = o_b.reshape(B, S, GDN_VW).astype(h.dtype)
    return jnp.concatenate([o_a, o_b], axis=-1) @ w_out


def odd_mixer(h, w_in, q_norm, k_norm, kidx_norm, w_out):
    B, S, _ = h.shape
    q, k, v, q_idx, k_idx, w_idx = split_last(
        h @ w_in, [DSA_HEADS * DSA_HEAD_DIM, DSA_KV_HEADS * DSA_HEAD_DIM, DSA_KV_HEADS * DSA_HEAD_DIM,
                   IDX_HEADS * IDX_DIM, IDX_DIM, IDX_HEADS])
    q = rms_norm(q.reshape(B, S, DSA_HEADS, DSA_HEAD_DIM), q_norm)
    k = rms_norm(k.reshape(B, S, DSA_KV_HEADS, DSA_HEAD_DIM), k_norm)
    v = v.reshape(B, S, DSA_KV_HEADS, DSA_HEAD_DIM)
    q_idx = q_idx.reshape(B, S, IDX_HEADS, IDX_DIM)
    k_idx = rms_norm(k_idx, kidx_norm)
    topk = min(DSA_TOPK, S // 4)
    o = dsa_sparse_attention(q, k, v, q_idx, k_idx, w_idx, topk)
    return o.reshape(B, S, ODD_OUT) @ w_out


def setup_inputs(seed: int = 0) -> dict:
    key = jax.random.key(seed)
    keys = jax.random.split(key, 40)
    ctr = iter(range(40))
    ne = (DEPTH + 1) // 2
    no = DEPTH // 2

    def nk():
        return keys[next(ctr)]

    def w(shape, fan_in):
        return jax.random.normal(nk(), shape, jnp.float32) * fan_in ** -0.5

    def gain(shape):
        return 1.0 + 0.02 * jax.random.normal(nk(), shape, jnp.float32)

    x = jax.random.normal(nk(), (BATCH, SEQ, D_MODEL), jnp.float32)
    p = jax.random.normal(nk(), (DEPTH, BATCH, SEQ, PLE_DIM), jnp.float32)
    a_log = jnp.log(jax.random.uniform(nk(), (ne, GDN_HEADS), jnp.float32, 1.0, 16.0))
    dt = jnp.exp(jax.random.uniform(nk(), (ne, GDN_HEADS), jnp.float32, math.log(1e-3), math.log(1e-1)))
    dt_bias = dt + jnp.log(-jnp.expm1(-dt))
    return {
        'x': x,
        'p': p,
        'ln_ffn1': gain((DEPTH, D_MODEL)),
        'ffn1_w_gu': w((DEPTH, D_MODEL, 2 * D_FF), D_MODEL),
        'ffn1_w_down': w((DEPTH, D_FF, D_MODEL), D_FF),
        'ln_mix': gain((DEPTH, D_MODEL)),
        'even_w_in': w((ne, D_MODEL, EVEN_IN), D_MODEL),
        'mla_q_a_norm': gain((ne, MLA_Q_LORA)),
        'mla_w_q_up': w((ne, MLA_Q_LORA, MLA_HEADS * MLA_QK), MLA_Q_LORA),
        'mla_kv_a_norm': gain((ne, MLA_KV_LORA)),
        'mla_w_kv_up': w((ne, MLA_KV_LORA, MLA_HEADS * (MLA_NOPE + MLA_V)), MLA_KV_LORA),
        'mla_q_norm': gain((ne, MLA_QK)),
        'mla_k_norm': gain((ne, MLA_QK)),
        'gdn_conv_w': w((ne, GDN_CONV, GDN_CONV_CH), GDN_CONV),
        'gdn_a_log': a_log,
        'gdn_dt_bias': dt_bias,
        'gdn_out_norm': gain((ne, GDN_DV)),
        'even_w_out': w((ne, EVEN_OUT, D_MODEL), EVEN_OUT),
        'odd_w_in': w((no, D_MODEL, ODD_IN), D_MODEL),
        'dsa_q_norm': gain((no, DSA_HEAD_DIM)),
        'dsa_k_norm': gain((no, DSA_HEAD_DIM)),
        'dsa_kidx_norm': gain((no, IDX_DIM)),
        'odd_w_out': w((no, ODD_OUT, D_MODEL), ODD_OUT),
        'ln_ffn2': gain((DEPTH, D_MODEL)),
        'ffn2_w_gu': w((DEPTH, D_MODEL, 2 * D_FF), D_MODEL),
        'ffn2_w_down': w((DEPTH, D_FF, D_MODEL), D_FF),
        'ln_ple': gain((DEPTH, D_MODEL)),
        'ple_w_in': w((DEPTH, PLE_DIM, D_MODEL), PLE_DIM),
        'ple_w_gate': w((DEPTH, D_MODEL, D_MODEL), D_MODEL),
    }


def reference(x, p, ln_ffn1, ffn1_w_gu, ffn1_w_down, ln_mix, even_w_in, mla_q_a_norm, mla_w_q_up,
              mla_kv_a_norm, mla_w_kv_up, mla_q_norm, mla_k_norm, gdn_conv_w, gdn_a_log, gdn_dt_bias,
              gdn_out_norm, even_w_out, odd_w_in, dsa_q_norm, dsa_k_norm, dsa_kidx_norm, odd_w_out,
              ln_ffn2, ffn2_w_gu, ffn2_w_down, ln_ple, ple_w_in, ple_w_gate):
    for i in range(DEPTH):
        j = i // 2
        x = x + 0.5 * swiglu(rms_norm(x, ln_ffn1[i]), ffn1_w_gu[i], ffn1_w_down[i])
        h = rms_norm(x, ln_mix[i])
        if i % 2 == 0:
            x = x + even_mixer(h, even_w_in[j], mla_q_a_norm[j], mla_w_q_up[j], mla_kv_a_norm[j],
                               mla_w_kv_up[j], mla_q_norm[j], mla_k_norm[j], gdn_conv_w[j], gdn_a_log[j],
                               gdn_dt_bias[j], gdn_out_norm[j], even_w_out[j])
        else:
            x = x + odd_mixer(h, odd_w_in[j], dsa_q_norm[j], dsa_k_norm[j], dsa_kidx_norm[j], odd_w_out[j])
        x = x + 0.5 * swiglu(rms_norm(x, ln_ffn2[i]), ffn2_w_gu[i], ffn2_w_down[i])
        gate = jax.nn.sigmoid(rms_norm(x, ln_ple[i]) @ ple_w_gate[i])
        x = x + gate * (p[i] @ ple_w_in[i])
    return x
```

```python
import numpy as np
from contextlib import ExitStack
import concourse.bass as bass
import concourse.mybir as mybir
from concourse.bass_utils import run_bass_kernel_spmd

F32 = mybir.dt.float32
BF16 = mybir.dt.bfloat16
AF = mybir.ActivationFunctionType
ALU = mybir.AluOpType

S = 4096
D = 1024
T = 512
NT = S // T
DFF = 2816
NJ = DFF // 128
EPS = 1e-6
DEPTH = 4
PLE = 256

SAME_ENGINE_SYNC = ("act", "dve", "pool")
SYNC_ALL_SAME = True


class Tok:
    __slots__ = ("w", "re", "rd", "excl", "hard")

    def __init__(self, excl=False, hard=False):
        self.w = None
        self.re = {}
        self.rd = []
        self.hard = hard
        self.excl = excl


class Fw:
    ENG = ("pe", "act", "dve", "pool", "sp")

    def __init__(self, nc, n_dma_sems=48):
        self.nc = nc
        self.stack = ExitStack()
        self.sem = {e: self.stack.enter_context(nc.semaphore("s_" + e)) for e in self.ENG}
        self.dsem = [self.stack.enter_context(nc.semaphore("d%d" % i)) for i in range(n_dma_sems)]
        self.dcnt = [0] * n_dma_sems
        self.dnext = 0
        self.ops = {e: [] for e in self.ENG}
        self.nops = {e: 0 for e in self.ENG}
        self.signal = {e: set() for e in self.ENG}
        self.seen_e = {e: {f: -1 for f in self.ENG} for e in self.ENG}
        self.seen_d = {e: [0] * n_dma_sems for e in self.ENG}

    def _need(self, eng, ev, hard=True):
        if ev is None:
            return
        if ev[0] == "e":
            _, f, idx = ev
            if f == eng and (eng not in SAME_ENGINE_SYNC or not (hard or SYNC_ALL_SAME)):
                return
            if self.seen_e[eng][f] >= idx:
                return
            self.seen_e[eng][f] = idx
            self.signal[f].add(idx)
            self.ops[eng].append(("wait_e", f, idx))
        else:
            _, s, val = ev
            if self.seen_d[eng][s] >= val:
                return
            self.seen_d[eng][s] = val
            self.ops[eng].append(("wait_d", s, val))

    def _deps(self, eng, reads, writes):
        for t in reads:
            self._need(eng, t.w, t.hard)
            if t.excl:
                for f, idx in t.re.items():
                    if f != eng:
                        self._need(eng, ("e", f, idx))
        for t in writes:
            self._need(eng, t.w, t.hard)
            for f, idx in t.re.items():
                self._need(eng, ("e", f, idx), t.hard)
            for r in t.rd:
                self._need(eng, r)

    def _commit(self, ev, reads, writes):
        for t in reads:
            if ev[0] == "e":
                t.re[ev[1]] = ev[2]
            else:
                t.rd.append(ev)
        for t in writes:
            t.w = ev
            t.re = {}
            t.rd = []

    def op(self, eng, fn, reads=(), writes=()):
        self._deps(eng, reads, writes)
        idx = self.nops[eng]
        self.nops[eng] += 1
        self.ops[eng].append(("op", fn, idx))
        ev = ("e", eng, idx)
        self._commit(ev, reads, writes)
        return ev

    def dma(self, q, out, in_, reads=(), writes=(), **kw):
        self._deps(q, reads, writes)
        s = self.dnext
        self.dnext = (self.dnext + 1) % len(self.dsem)
        if self.dcnt[s] > 0:
            self._need(q, ("d", s, self.dcnt[s]))
        self.dcnt[s] += 16
        val = self.dcnt[s]
        self.ops[q].append(("dma", out, in_, s, kw))
        ev = ("d", s, val)
        self._commit(ev, reads, writes)
        return ev

    def barrier(self):
        last = {}
        for e in self.ENG:
            if self.nops[e] > 0:
                last[e] = ("e", e, self.nops[e] - 1)
        for e in self.ENG:
            for f_, ev in last.items():
                if f_ != e:
                    self._need(e, ev)
            for s in range(len(self.dsem)):
                if self.dcnt[s] > 0:
                    self._need(e, ("d", s, self.dcnt[s]))

    def emit(self):
        nc = self.nc
        cnt = {}
        for e in self.ENG:
            m = {}
            c = 0
            for idx in sorted(self.signal[e]):
                c += 1
                m[idx] = c
            cnt[e] = m
        self.sigcount = {e: len(cnt[e]) for e in self.ENG}

        def run(e):
            def body(engh):
                for it in self.ops[e]:
                    k = it[0]
                    if k == "op":
                        ins = it[1](engh)
                        if it[2] in cnt[e]:
                            ins.then_inc(self.sem[e], 1)
                    elif k == "wait_e":
                        engh.wait_ge(self.sem[it[1]], cnt[it[1]][it[2]])
                    elif k == "wait_d":
                        engh.wait_ge(self.dsem[it[1]], it[2])
                    elif k == "dma":
                        engh.dma_start(out=it[1], in_=it[2], **it[4]).then_inc(self.dsem[it[3]], 16)
            return body

        with nc.Block() as block:
            block.sync(run("sp"))
            block.scalar(run("act"))
            block.vector(run("dve"))
            block.gpsimd(run("pool"))
            block.tensor(run("pe"))

    def close(self):
        self.stack.close()


class Ring:
    def __init__(self, items):
        self.items = items
        self.i = 0

    def next(self):
        it = self.items[self.i]
        self.i = (self.i + 1) % len(self.items)
        return it


def _vec_layout():
    off = {}
    n = 0
    for i in range(DEPTH):
        for nm in ("ln_ffn1", "ln_mix", "ln_ffn2", "ln_ple"):
            off[(nm, i)] = n
            n += 8
    for j in range(2):
        off[("mla_q_a_norm", j)] = n; n += 3
        off[("mla_kv_a_norm", j)] = n; n += 2
        off[("mla_q_norm", j)] = n; n += 1
        off[("mla_k_norm", j)] = n; n += 1
        off[("gdn_conv_w", j)] = n; n += 48
        off[("gdn_out_norm", j)] = n; n += 1
        off[("dsa_q_norm", j)] = n; n += 1
        off[("dsa_k_norm", j)] = n; n += 1
        off[("dsa_kidx_norm", j)] = n; n += 1
    return off, n


VOFF, NV = _vec_layout()


def pack_vecs(inp):
    v = np.zeros((128, NV), np.float32)
    for i in range(DEPTH):
        for nm in ("ln_ffn1", "ln_mix", "ln_ffn2", "ln_ple"):
            o = VOFF[(nm, i)]
            v[:, o:o + 8] = np.asarray(inp[nm][i]).reshape(8, 128).T
    for j in range(2):
        o = VOFF[("mla_q_a_norm", j)]; v[:, o:o + 3] = np.asarray(inp["mla_q_a_norm"][j]).reshape(3, 128).T
        o = VOFF[("mla_kv_a_norm", j)]; v[:, o:o + 2] = np.asarray(inp["mla_kv_a_norm"][j]).reshape(2, 128).T
        o = VOFF[("mla_q_norm", j)]; v[:96, o] = np.asarray(inp["mla_q_norm"][j])
        o = VOFF[("mla_k_norm", j)]; v[:96, o] = np.asarray(inp["mla_k_norm"][j])
        o = VOFF[("gdn_conv_w", j)]
        cw = np.asarray(inp["gdn_conv_w"][j])
        v[:, o:o + 48] = cw.reshape(4, 12, 128).transpose(2, 1, 0).reshape(128, 48)
        o = VOFF[("gdn_out_norm", j)]; v[:, o] = np.asarray(inp["gdn_out_norm"][j])
        o = VOFF[("dsa_q_norm", j)]; v[:, o] = np.asarray(inp["dsa_q_norm"][j])
        o = VOFF[("dsa_k_norm", j)]; v[:, o] = np.asarray(inp["dsa_k_norm"][j])
        o = VOFF[("dsa_kidx_norm", j)]; v[:64, o] = np.asarray(inp["dsa_kidx_norm"][j]); v[64:, o] = np.asarray(inp["dsa_kidx_norm"][j])
    return v


C_IDENT = 0
C_ONES = 128
C_NEG = 256
C_PM = 384
C_CM = 512
C_MS = 2560
C_MI = 2688
C_UI = 2816
C_CIND = 2944
NC_CONST = 2946
NEGBIG = -1.0e30


def make_consts():
    c = np.zeros((128, NC_CONST), np.float32)
    c[:, C_IDENT:C_IDENT + 128] = np.eye(128, dtype=np.float32)
    c[:, C_ONES:C_ONES + 128] = 1.0
    c[:, C_NEG:C_NEG + 128] = np.triu(np.full((128, 128), NEGBIG, np.float32), 1)
    for m in range(64, 80):
        c[m + 16, C_PM + m] = 1.0
        c[m, C_PM + m + 16] = 1.0
    a_ = np.arange(128)
    same = (a_[:, None] // 64) == (a_[None, :] // 64)
    c[:, C_MS:C_MS + 128] = (same & (a_[None, :] < a_[:, None])).astype(np.float32)
    c[:, C_MI:C_MI + 128] = (same & (a_[None, :] <= a_[:, None])).astype(np.float32)
    c[:, C_UI:C_UI + 128] = (same & (a_[:, None] <= a_[None, :])).astype(np.float32)
    c[:64, C_CIND] = 1.0
    c[64:, C_CIND + 1] = 1.0
    kk = np.arange(128)[:, None]
    qq = np.arange(512)[None, :]
    for off in range(4):
        c[:, C_CM + off * 512:C_CM + (off + 1) * 512] = (off * 128 + kk <= qq).astype(np.float32)
    return c


def make_rope():
    half = 16
    inv_freq = 10000.0 ** (-np.arange(half, dtype=np.float64) / half)
    ang = np.arange(S, dtype=np.float64)[None, :] * inv_freq[:, None]
    cos = np.ones((96, S), np.float64)
    sin = np.zeros((96, S), np.float64)
    cos[64:80] = np.cos(ang); cos[80:96] = np.cos(ang)
    sin[64:80] = -np.sin(ang); sin[80:96] = np.sin(ang)
    return np.stack([cos, sin]).astype(np.float32)


WEIGHT_SHAPES = {
    "ffn1_w_gu": (4, D, 2 * DFF), "ffn1_w_down": (4, DFF, D),
    "ffn2_w_gu": (4, D, 2 * DFF), "ffn2_w_down": (4, DFF, D),
    "even_w_in": (2, D, 2728), "mla_w_q_up": (2, 384, 768), "mla_w_kv_up": (2, 256, 1024),
    "even_w_out": (2, 1024, D), "odd_w_in": (2, D, 2120), "odd_w_out": (2, 1024, D),
    "ple_w_in": (4, PLE, D), "ple_w_gate": (4, D, D),
    "gdn_a_log": (2, 4), "gdn_dt_bias": (2, 4),
}


class Prog:
    def __init__(self, phases):
        self.nc = nc = bass.Bass("TRN2", target_bir_lowering=False)
        self.f = Fw(nc)
        self.phases = phases
        self.d = {}
        self.d["xT"] = nc.dram_tensor("xT", [D, S], F32, kind="ExternalInput").ap()
        self.d["pT"] = nc.dram_tensor("pT", [DEPTH, PLE, S], F32, kind="ExternalInput").ap()
        self.d["vecs"] = nc.dram_tensor("vecs", [128, NV], F32, kind="ExternalInput").ap()
        self.d["consts"] = nc.dram_tensor("consts", [128, NC_CONST], F32, kind="ExternalInput").ap()
        self.d["rope"] = nc.dram_tensor("rope", [2, 96, S], F32, kind="ExternalInput").ap()
        for k, shp in WEIGHT_SHAPES.items():
            self.d[k] = nc.dram_tensor(k, list(shp), F32, kind="ExternalInput").ap()
        self.xres = nc.dram_tensor("outT", [D, S], F32, kind="ExternalOutput").ap()
        self.scr = {}
        for nm, shp, dt in (("dsa_qT", [8, 128, S], BF16), ("dsa_kT", [2, 128, S], BF16), ("dsa_v", [S, 256], BF16),
                            ("dsa_qiT", [4, 128, S], BF16), ("dsa_kiT", [128, S], BF16), ("dsa_w", [S, 8], F32),
                            ("mla_qT", [8, 96, S], BF16), ("mla_kT", [8, 96, S], BF16), ("mla_vext", [8, S, 128], BF16),
                            ("gdn_qT", [4, 128, S], F32), ("gdn_kT", [4, 128, S], F32), ("gdn_vT", [4, 128, S], F32),
                            ("gdn_zs", [4, 128, S], F32), ("gdn_gb", [S, 8], F32), ("even_oT", [D, S], BF16)):
            self.scr[nm] = nc.dram_tensor(nm, shp, dt).ap()
        self.scr_k = {nm: [Tok() for _ in range(32)] for nm in self.scr}
        self.xsrc = self.d["xT"]
        self.xtok = [[Tok() for _ in range(8)] for _ in range(NT)]
        self.pref = None
        self.gst = ExitStack()
        self.pst = None
        self._n = 0

    def _name(self, p):
        self._n += 1
        return "%s_%d" % (p, self._n)

    def gsb(self, shape, dt, name="g"):
        return self.gst.enter_context(self.nc.sbuf_tensor(self._name(name), list(shape), dt))

    def sb(self, shape, dt, name="t"):
        return self.pst.enter_context(self.nc.sbuf_tensor(self._name(name), list(shape), dt))

    def ring(self, n, shape, dt, name="r"):
        small = int(np.prod(shape[1:])) < 64
        return Ring([(self.sb(shape, dt, name), Tok(hard=small)) for _ in range(n)])

    def mm(self, out, lhsT, rhs, start, stop, reads, writes):
        self.f.op("pe", lambda e: e.matmul(out, lhsT=lhsT, rhs=rhs, start=start, stop=stop), reads, writes)

    def tr(self, out, in_, ident, reads, writes):
        self.f.op("pe", lambda e: e.transpose(out, in_, ident), reads, writes)

    def act(self, out, in_, func, reads, writes, scale=None, bias=None):
        kw = {}
        if scale is not None:
            kw["scale"] = scale
        if bias is not None:
            kw["bias"] = bias
        self.f.op("act", lambda e: e.activation(out=out, in_=in_, func=func, **kw), reads, writes)

    def tt(self, eng, out, in0, in1, op, reads, writes):
        self.f.op(eng, lambda e: e.tensor_tensor(out=out, in0=in0, in1=in1, op=op), reads, writes)

    def ts(self, eng, out, in0, s1, op0, reads, writes, s2=None, op1=None):
        if op1 is None:
            self.f.op(eng, lambda e: e.tensor_scalar(out=out, in0=in0, scalar1=s1, scalar2=None, op0=op0), reads, writes)
        else:
            self.f.op(eng, lambda e: e.tensor_scalar(out=out, in0=in0, scalar1=s1, scalar2=s2, op0=op0, op1=op1), reads, writes)

    def stt(self, out, in0, scalar, in1, op0, op1, reads, writes):
        self.f.op("dve", lambda e: e.scalar_tensor_tensor(out=out, in0=in0, scalar=scalar, in1=in1, op0=op0, op1=op1), reads, writes)

    def cp(self, eng, out, in_, reads, writes):
        if eng == "act":
            self.f.op("act", lambda e: e.activation(out=out, in_=in_, func=AF.Copy), reads, writes)
        else:
            self.f.op(eng, lambda e: e.tensor_copy(out=out, in_=in_), reads, writes)

    def recip(self, out, in_, reads, writes):
        self.f.op("dve", lambda e: e.reciprocal(out=out, in_=in_), reads, writes)

    def dma(self, out, in_, reads, writes, q="sp"):
        self.f.dma(q, out, in_, reads, writes)

    def setup(self):
        nc = self.nc
        self.psum = []
        for i in range(8):
            t = self.gst.enter_context(nc.psum_tensor("ps%d" % i, [128, 512], F32))
            self.psum.append((t, Tok(excl=True)))
        self.vec = self.gsb([128, NV], F32, "vec")
        self.vec_k = Tok()
        self.dma(self.vec[:], self.d["vecs"], [], [self.vec_k])
        self.cst = self.gsb([128, 256], F32, "cst")
        self.cst_k = Tok()
        self.dma(self.cst[:], self.d["consts"][:, 0:256], [], [self.cst_k])
        self.cb = self.gsb([128, 256], BF16, "cstb")
        self.cb_k = Tok()
        self.cp("dve", self.cb[:], self.cst[:], [self.cst_k], [self.cb_k])
        self.ident_f = self.cst[:, C_IDENT:C_IDENT + 128]
        self.ones_f = self.cst[:, C_ONES:C_ONES + 128]
        self.ident_b = self.cb[:, C_IDENT:C_IDENT + 128]
        self.ones_b = self.cb[:, C_ONES:C_ONES + 128]

    def vcol(self, key, c=0, rows=128):
        o = VOFF[key] + c
        return self.vec[0:rows, o:o + 1]

    def lconst(self, c0, n, bf16=False):
        t = self.sb([128, n], F32, "lc")
        k = Tok()
        self.dma(t[:], self.d["consts"][:, c0:c0 + n], [], [k])
        if not bf16:
            return t, k
        tb = self.sb([128, n], BF16, "lcb")
        kb = Tok()
        self.cp("pool", tb[:], t[:], [k], [kb])
        return tb, kb

    def begin_phase(self):
        self.pst = ExitStack()

    def end_phase(self):
        self.f.barrier()
        self.pst.close()
        self.pst = None

    def load_norm(self, tt, xstage, sqring, xb, xb_k, rstd, rstd_k, ssb):
        ss, ss_k = ssb
        for c in range(8):
            xs, xs_k = xstage.next()
            self.dma(xs[:], self.xsrc[c * 128:(c + 1) * 128, tt * T:(tt + 1) * T], [self.xtok[tt][c]], [xs_k])
            sq, sq_k = sqring.next()
            self.act(sq[:], xs[:], AF.Square, [xs_k], [sq_k])
            self.cp("pool", xb[:, c, :], xs[:], [xs_k], [xb_k])
            self.mm(ss[:], self.ones_b, sq[:], c == 0, c == 7, [sq_k, self.cb_k], [ss_k])
        self.act(rstd[:], ss[:], AF.Sqrt, [ss_k], [rstd_k], scale=1.0 / D, bias=EPS)
        self.recip(rstd[:], rstd[:], [rstd_k], [rstd_k])

    def load_w(self, dst, dst_k, src, stage, gain, eng_i):
        st, st_k = stage.next()
        n = src.shape[-1]
        self.dma(st[:, 0:n], src, [], [st_k])
        eng = ("dve", "pool", "act")[eng_i % 3] if gain is None else ("dve", "act")[eng_i % 2]
        rd = [st_k, self.vec_k]
        if gain is None:
            self.cp(eng, dst, st[:, 0:n], rd, [dst_k])
        elif eng == "act":
            self.act(dst, st[:, 0:n], AF.Copy, rd, [dst_k], scale=gain)
        else:
            self.ts(eng, dst, st[:, 0:n], gain, ALU.mult, rd, [dst_k])


    def ffn_weights_begin(self, i, which):
        st = ExitStack()
        nc = self.nc
        wgu = st.enter_context(nc.sbuf_tensor(self._name("wgu"), [128, 8, 2 * DFF], BF16))
        wd = st.enter_context(nc.sbuf_tensor(self._name("wd"), [128, NJ, D], BF16))
        stage = Ring([(st.enter_context(nc.sbuf_tensor(self._name("wst"), [128, 1408], F32)), Tok()) for _ in range(2)])
        wgu_k = [[Tok() for _ in range(4)] for _ in range(8)]
        wd_k = [Tok() for _ in range(NJ)]
        Wgu = self.d["ffn%d_w_gu" % which][i]
        Wd = self.d["ffn%d_w_down" % which][i]
        gkey = ("ln_ffn%d" % which, i)
        tasks = []
        n = 0
        for c in range(8):
            for pc in range(4):
                tasks.append((wgu[:, c, pc * 1408:(pc + 1) * 1408], wgu_k[c][pc], Wgu[c * 128:(c + 1) * 128, pc * 1408:(pc + 1) * 1408], self.vcol(gkey, c), n))
                n += 1
        for j in range(NJ):
            tasks.append((wd[:, j, :], wd_k[j], Wd[j * 128:(j + 1) * 128, :], None, n))
            n += 1
        self.pref = {"key": (i, which), "st": st, "wgu": wgu, "wd": wd, "wgu_k": wgu_k, "wd_k": wd_k, "stage": stage, "tasks": tasks}

    def pref_step(self, n=None):
        if not self.pref:
            return
        tasks = self.pref["tasks"]
        k = len(tasks) if n is None else min(n, len(tasks))
        for _ in range(k):
            dst, dst_k, src, gain, idx = tasks.pop(0)
            self.load_w(dst, dst_k, src, self.pref["stage"], gain, idx)

    def next_ffn(self, i, which):
        return (i, which)

    def phase_ffn(self, i, which):
        if not (self.pref and self.pref["key"] == (i, which)):
            assert not self.pref
            self.ffn_weights_begin(i, which)
        self.begin_phase()
        pf = self.pref
        wgu, wgu_k, wd, wd_k = pf["wgu"], pf["wgu_k"], pf["wd"], pf["wd_k"]
        xstage = self.ring(4, [128, T], F32, "xst")
        sqring = self.ring(2, [128, T], BF16, "sq")
        xbs = [(self.sb([128, 8, T], BF16, "xb"), Tok())] * 2
        rstds = [(self.sb([128, T], F32, "rstd"), Tok())] * 2
        actb = self.sb([128, NJ, T], BF16, "actb")
        act_k = [Tok() for _ in range(NJ)]
        aring = self.ring(3, [128, T], F32, "A")
        oring = self.ring(3, [128, T], F32, "ob")
        ssb = self.psum[0]
        gps = [self.psum[1], self.psum[2]]
        ups = [self.psum[3], self.psum[4]]
        yps = [self.psum[5], self.psum[6]]
        self.pref_step()
        self.load_norm(0, xstage, sqring, xbs[0][0], xbs[0][1], rstds[0][0], rstds[0][1], ssb)
        for tt in range(NT):
            xb, xb_k = xbs[tt % 2]
            rstd, rstd_k = rstds[tt % 2]
            for j in range(NJ):
                gp, gp_k = gps[j % 2]
                up, up_k = ups[j % 2]
                for half, (pp, pp_k) in enumerate(((gp, gp_k), (up, up_k))):
                    col = half * DFF + j * 128
                    pc = col // 1408
                    assert (col + 127) // 1408 == pc
                    for c in range(8):
                        self.mm(pp[:], wgu[:, c, col:col + 128], xb[:, c, :], c == 0, c == 7,
                                [wgu_k[c][pc], xb_k], [pp_k])
                A, A_k = aring.next()
                self.tt("dve", A[:], gp[:], rstd[:], ALU.mult, [gp_k, rstd_k], [A_k])
                self.act(A[:], A[:], AF.Silu, [A_k], [A_k])
                self.tt("pool", A[:], A[:], rstd[:], ALU.mult, [A_k, rstd_k], [A_k])
                self.tt("dve", actb[:, j, :], up[:], A[:], ALU.mult, [up_k, A_k], [act_k[j]])
            if tt + 1 < NT:
                nb = xbs[(tt + 1) % 2]
                nr = rstds[(tt + 1) % 2]
                self.load_norm(tt + 1, xstage, sqring, nb[0], nb[1], nr[0], nr[1], ssb)
            for m in range(8):
                yp, yp_k = yps[m % 2]
                for j in range(NJ):
                    self.mm(yp[:], wd[:, j, m * 128:(m + 1) * 128], actb[:, j, :], j == 0, j == NJ - 1,
                            [wd_k[j], act_k[j]], [yp_k])
                xs, xs_k = xstage.next()
                self.dma(xs[:], self.xsrc[m * 128:(m + 1) * 128, tt * T:(tt + 1) * T], [self.xtok[tt][m]], [xs_k])
                ob, ob_k = oring.next()
                self.stt(ob[:], yp[:], 0.5, xs[:], ALU.mult, ALU.add, [yp_k, xs_k], [ob_k])
                self.dma(self.xres[m * 128:(m + 1) * 128, tt * T:(tt + 1) * T], ob[:], [ob_k], [self.xtok[tt][m]], q="act")
        self.end_phase()
        self.pref["st"].close()
        self.pref = None
        self.xsrc = self.xres

    def phase_ple(self, i):
        if i + 1 < DEPTH and ("ffn", i + 1, 1) in self.phases:
            self.ffn_weights_begin(i + 1, 1)
        self.begin_phase()
        Wg = self.d["ple_w_gate"][i]
        Wp = self.d["ple_w_in"][i]
        gkey = ("ln_ple", i)
        wg = self.sb([128, 8, D], BF16, "wg")
        wg_k = [Tok() for _ in range(8)]
        wp = self.sb([128, 2, D], BF16, "wp")
        wp_k = [Tok() for _ in range(2)]
        wstage = self.pref["stage"] if self.pref else self.ring(2, [128, D], F32, "wst")
        xstage = self.ring(3, [128, T], F32, "xst")
        sqring = self.ring(2, [128, T], BF16, "sq")
        xbs = [(self.sb([128, 8, T], BF16, "xb"), Tok()) for _ in range(2)]
        rstds = [(self.sb([128, T], F32, "rstd"), Tok()) for _ in range(2)]
        pstage = self.ring(1, [128, T], F32, "pst")
        pbs = [(self.sb([128, 2, T], BF16, "pb"), Tok()) for _ in range(2)]
        aring = self.ring(2, [128, T], F32, "A")
        oring = self.ring(2, [128, T], F32, "ob")
        ssb = self.psum[0]
        gps = [self.psum[1], self.psum[2]]
        eps_ = [self.psum[3], self.psum[4]]
        for c in range(8):
            self.load_w(wg[:, c, :], wg_k[c], Wg[c * 128:(c + 1) * 128, :], wstage, self.vcol(gkey, c), c)
        for c in range(2):
            self.load_w(wp[:, c, :], wp_k[c], Wp[c * 128:(c + 1) * 128, :], wstage, None, c)

        def load_p(tt):
            pb, pb_k = pbs[tt % 2]
            for c in range(2):
                st, st_k = pstage.next()
                self.dma(st[:], self.d["pT"][i, c * 128:(c + 1) * 128, tt * T:(tt + 1) * T], [], [st_k])
                self.cp("pool", pb[:, c, :], st[:], [st_k], [pb_k])

        pend_st = []
        self.load_norm(0, xstage, sqring, xbs[0][0], xbs[0][1], rstds[0][0], rstds[0][1], ssb)
        load_p(0)
        for tt in range(NT):
            xb, xb_k = xbs[tt % 2]
            rstd, rstd_k = rstds[tt % 2]
            pb, pb_k = pbs[tt % 2]
            if tt + 1 < NT:
                nb = xbs[(tt + 1) % 2]
                nr = rstds[(tt + 1) % 2]
                self.load_norm(tt + 1, xstage, sqring, nb[0], nb[1], nr[0], nr[1], ssb)
                load_p(tt + 1)
            for m in range(8):
                gp, gp_k = gps[m % 2]
                ep, ep_k = eps_[m % 2]
                for c in range(8):
                    self.mm(gp[:], wg[:, c, m * 128:(m + 1) * 128], xb[:, c, :], c == 0, c == 7, [wg_k[c], xb_k], [gp_k])
                for c in range(2):
                    self.mm(ep[:], wp[:, c, m * 128:(m + 1) * 128], pb[:, c, :], c == 0, c == 1, [wp_k[c], pb_k], [ep_k])
                A, A_k = aring.next()
                self.tt("dve", A[:], gp[:], rstd[:], ALU.mult, [gp_k, rstd_k], [A_k])
                self.act(A[:], A[:], AF.Sigmoid, [A_k], [A_k])
                self.tt("dve", A[:], ep[:], A[:], ALU.mult, [ep_k, A_k], [A_k])
                xs, xs_k = xstage.next()
                self.dma(xs[:], self.xsrc[m * 128:(m + 1) * 128, tt * T:(tt + 1) * T], [self.xtok[tt][m]], [xs_k])
                ob, ob_k = oring.next()
                self.tt("pool", ob[:], A[:], xs[:], ALU.add, [A_k, xs_k], [ob_k])
                pend_st.append((self.xres[m * 128:(m + 1) * 128, tt * T:(tt + 1) * T], ob, ob_k, self.xtok[tt][m]))
                if len(pend_st) > 1:
                    d_, ob_, obk_, xk_ = pend_st.pop(0)
                    self.dma(d_, ob_[:], [obk_], [xk_], q="act")
            self.pref_step(7)
        while pend_st:
            d_, ob_, obk_, xk_ = pend_st.pop(0)
            self.dma(d_, ob_[:], [obk_], [xk_], q="act")
        self.pref_step()
        self.end_phase()
        self.xsrc = self.xres


    def load_hn(self, tt, xstage, sqring, xb, xb_k, rstd, rstd_k, ssb, hn, hn_k):
        self.load_norm(tt, xstage, sqring, xb, xb_k, rstd, rstd_k, ssb)
        for c in range(8):
            self.tt(("pool", "dve")[c % 2], hn[:, c, :], xb[:, c, :], rstd[:], ALU.mult, [xb_k, rstd_k], [hn_k])

    def head_norm(self, src, src_k, rows, gaincol, div, sqring, ssring, rring, dst, dst_k):
        sq, sq_k = sqring.next()
        self.act(sq[0:rows, :], src, AF.Square, [src_k], [sq_k])
        ss, ss_k = ssring.next()
        self.mm(ss[0:rows, :], self.ones_b[0:rows, 0:rows], sq[0:rows, :], True, True, [sq_k, self.cb_k], [ss_k])
        r, r_k = rring.next()
        self.act(r[0:rows, :], ss[0:rows, :], AF.Sqrt, [ss_k], [r_k], scale=1.0 / div, bias=EPS)
        self.recip(r[0:rows, :], r[0:rows, :], [r_k], [r_k])
        self.stt(dst, src, gaincol, r[0:rows, :], ALU.mult, ALU.mult, [src_k, r_k, self.vec_k], [dst_k])

    def phase_odd_proj(self, i):
        j = i // 2
        self.begin_phase()
        W = self.d["odd_w_in"][j]
        NW = 2120
        win = self.sb([128, 8, NW + 128], BF16, "win")
        win_k = [Tok() for _ in range(8)]
        wstage = self.ring(2, [128, NW], F32, "wst")
        xstage = self.ring(4, [128, T], F32, "xst")
        sqring = self.ring(2, [128, T], BF16, "sq")
        xb, xb_k = self.sb([128, 8, T], BF16, "xb"), Tok()
        rstd, rstd_k = self.sb([128, T], F32, "rstd"), Tok()
        hns = [(self.sb([128, 8, T], BF16, "hn"), Tok()) for _ in range(2)]
        sq2 = self.ring(2, [128, T], BF16, "sq2")
        rring = self.ring(2, [128, T], F32, "rr")
        oring = self.ring(4, [128, T], BF16, "ob")
        wring = self.ring(2, [128, 8], F32, "wb")
        ssb = self.psum[0]
        pring = Ring([self.psum[1], self.psum[2], self.psum[3]])
        ssring = Ring([self.psum[4], self.psum[5]])
        sring = Ring([self.psum[6], self.psum[7]])
        for c in range(8):
            self.load_w(win[:, c, 0:NW], win_k[c], W[c * 128:(c + 1) * 128, :], wstage, self.vcol(("ln_mix", i), c), c)
            self.cp("pool", win[:, c, NW:NW + 64], win[:, c, 2048:2112], [win_k[c]], [win_k[c]])
            self.cp("pool", win[:, c, NW + 64:NW + 128], win[:, c, 2048:2112], [win_k[c]], [win_k[c]])
        sc = self.scr
        sk = self.scr_k
        self.load_hn(0, xstage, sqring, xb, xb_k, rstd, rstd_k, ssb, hns[0][0], hns[0][1])
        for tt in range(NT):
            hn, hn_k = hns[tt % 2]
            if tt + 1 < NT:
                self.load_hn(tt + 1, xstage, sqring, xb, xb_k, rstd, rstd_k, ssb, hns[(tt + 1) % 2][0], hns[(tt + 1) % 2][1])
            tsl = slice(tt * T, (tt + 1) * T)

            def proj(col, width=128):
                pp, pp_k = pring.next()
                for c in range(8):
                    self.mm(pp[0:width, :], win[:, c, col:col + width], hn[:, c, :], c == 0, c == 7, [win_k[c], hn_k], [pp_k])
                return pp, pp_k
            for h in range(8):
                pp, pp_k = proj(h * 128)
                ob, ob_k = oring.next()
                self.head_norm(pp[:], pp_k, 128, self.vcol(("dsa_q_norm", j)), 128.0, sq2, ssring, rring, ob[:], ob_k)
                self.dma(sc["dsa_qT"][h, :, tsl], ob[:], [ob_k], [sk["dsa_qT"][tt]])
            for n in range(2):
                pp, pp_k = proj(1024 + n * 128)
                ob, ob_k = oring.next()
                self.head_norm(pp[:], pp_k, 128, self.vcol(("dsa_k_norm", j)), 128.0, sq2, ssring, rring, ob[:], ob_k)
                self.dma(sc["dsa_kT"][n, :, tsl], ob[:], [ob_k], [sk["dsa_kT"][tt]])
            pp, pp_k = proj(NW)
            ob, ob_k = oring.next()
            self.head_norm(pp[:], pp_k, 128, self.vcol(("dsa_kidx_norm", j)), 128.0, sq2, ssring, rring, ob[:], ob_k)
            self.dma(sc["dsa_kiT"][:, tsl], ob[:], [ob_k], [sk["dsa_kiT"][tt]])
            for c4 in range(4):
                pp, pp_k = proj(1536 + c4 * 128)
                ob, ob_k = oring.next()
                self.cp("act", ob[:], pp[:], [pp_k], [ob_k])
                self.dma(sc["dsa_qiT"][c4, :, tsl], ob[:], [ob_k], [sk["dsa_qiT"][tt]])
            for sub in range(4):
                sp_, sp_k = sring.next()
                for c in range(8):
                    self.mm(sp_[:, 0:256], hn[:, c, sub * 128:(sub + 1) * 128], win[:, c, 1280:1536], c == 0, c == 7, [win_k[c], hn_k], [sp_k])
                ob, ob_k = oring.next()
                self.cp("act", ob[:, 0:256], sp_[:, 0:256], [sp_k], [ob_k])
                r0 = tt * T + sub * 128
                self.dma(sc["dsa_v"][r0:r0 + 128, :], ob[:, 0:256], [ob_k], [sk["dsa_v"][tt]])
                sp_, sp_k = sring.next()
                for c in range(8):
                    self.mm(sp_[:, 0:8], hn[:, c, sub * 128:(sub + 1) * 128], win[:, c, 2112:2120], c == 0, c == 7, [win_k[c], hn_k], [sp_k])
                wb, wb_k = wring.next()
                self.cp("dve", wb[:], sp_[:, 0:8], [sp_k], [wb_k])
                self.dma(sc["dsa_w"][r0:r0 + 128, :], wb[:], [wb_k], [sk["dsa_w"][tt]])
        self.end_phase()

    def phase_dsa(self, i, nq=32):
        j = i // 2
        self.begin_phase()
        sc = self.scr
        REPL = 2.0 * NEGBIG
        kT, kT_k = self.sb([128, 2, S], BF16, "kT"), Tok()
        V, V_k = self.sb([128, 32, 256], BF16, "V"), Tok()
        kiT, kiT_k = self.sb([128, S], BF16, "kiT"), Tok()
        for n in range(2):
            self.dma(kT[:, n, :], sc["dsa_kT"][n], [], [kT_k])
        for kq in range(4):
            self.dma(V[:, kq * 8:(kq + 1) * 8, :], sc["dsa_v"][kq * 1024:(kq + 1) * 1024, :].rearrange("(kt p) d -> p kt d", p=128), [], [V_k])
        self.dma(kiT[:], sc["dsa_kiT"], [], [kiT_k])
        scbs = [(self.sb([128, S], F32, "scb"), [Tok(hard=True) for _ in range(8)]) for _ in range(4)]
        mbs = [(self.sb([128, S], BF16, "mb"), Tok()) for _ in range(4)]
        bss = [(self.sb([128, 8], F32, "bs"), Tok(hard=True)) for _ in range(4)]
        qring = self.ring(4, [128, 8, 128], BF16, "q")
        qiring = self.ring(4, [128, 4, 128], BF16, "qi")
        wring = Ring([(self.sb([128, 8], F32, "w"), Tok()) for _ in range(4)])
        mTring = self.ring(2, [128, 32, 128], BF16, "mT")
        rring = self.ring(3, [128, T], F32, "relu")
        junk, junk_k = self.sb([128, S], BF16, "junk"), Tok()
        ering = self.ring(4, [128, T], BF16, "e")
        pring = self.ring(4, [128, T], BF16, "pT")
        o32ring = self.ring(1, [128, T], F32, "o32")
        rdring = self.ring(1, [128, T], F32, "rd")
        oTring = self.ring(2, [128, 8, 128], BF16, "oT")
        ps_sc = Ring([self.psum[0], self.psum[1]])
        ps_st = Ring([self.psum[2], self.psum[3], self.psum[7]])
        ps_o = self.psum[4]
        ps_d = self.psum[5]
        ps_t = self.psum[6]
        ps_tb = ps_t[0][:].bitcast(BF16)
        negt, neg_k = self.lconst(C_NEG, 128)
        neg = negt[:, :]
        scale = 128.0 ** -0.5
        oT_d = sc["even_oT"].rearrange("(c p) t -> p c t", p=128)
        qbuf = {}

        def s1(qs):
            info = {}
            for qi in qs:
                L = (qi + 1) * 128
                qsl = slice(qi * 128, L)
                qi_sb, qi_k = qiring.next()
                self.dma(qi_sb[:], sc["dsa_qiT"][:, :, qsl].rearrange("h p q -> p h q"), [], [qi_k])
                w_sb, w_k = wring.next()
                self.dma(w_sb[:], sc["dsa_w"][qsl, :], [], [w_k])
                info[qi] = (L, qsl, qi_sb, qi_k, w_sb, w_k)
            for h in range(8):
                pb = (h % 2) * 64
                for qi in qs:
                    L, qsl, qi_sb, qi_k, w_sb, w_k = info[qi]
                    scb, sc_ks = scbs[qi % 4]
                    for st in range((L + 511) // 512):
                        w_ = min(512, L - st * 512)
                        seg = slice(st * 512, st * 512 + w_)
                        ps, ps_k = ps_sc.next()
                        self.mm(ps[:, 0:w_], qi_sb[pb:pb + 64, h // 2, :], kiT[pb:pb + 64, seg], True, True, [qi_k, kiT_k], [ps_k])
                        r, r_k = rring.next()
                        self.act(r[:, 0:w_], ps[:, 0:w_], AF.Relu, [ps_k], [r_k])
                        if h == 0:
                            self.ts("dve", scb[:, seg], r[:, 0:w_], w_sb[:, 0:1], ALU.mult, [r_k, w_k], [sc_ks[st]])
                        else:
                            self.stt(scb[:, seg], r[:, 0:w_], w_sb[:, h:h + 1], scb[:, seg], ALU.mult, ALU.add, [r_k, w_k, sc_ks[st]], [sc_ks[st]])
            for qi in qs:
                if qi >= 2:
                    bounds(qi)
            for qi in qs:
                L, qsl = info[qi][0], info[qi][1]
                scb, sc_ks = scbs[qi % 4]
                self.tt("pool", scb[:, qsl], scb[:, qsl], neg, ALU.add, [sc_ks[qi // 4], neg_k], [sc_ks[qi // 4]])

        def loadq(qs):
            for qi in qs:
                qsl = slice(qi * 128, (qi + 1) * 128)
                q_sb, q_k = qring.next()
                self.dma(q_sb[:], sc["dsa_qT"][:, :, qsl].rearrange("h p q -> p h q"), [], [q_k])
                qbuf[qi] = (q_sb, q_k)

        K_IT = 16

        def bounds(qi):
            L = (qi + 1) * 128
            scb, sc_ks = scbs[qi % 4]
            mb, mb_k = mbs[qi % 4]
            bs, bs_k = bss[qi % 4]
            self.f.op("dve", lambda e: e.tensor_scalar(out=junk[:, 0:L], in0=scb[:, 0:L], scalar1=0.0, scalar2=-3.0e38, op0=ALU.add, op1=ALU.max,
                                                        accum_out=bs[:, 1:2]), sc_ks, [junk_k, bs_k])
            self.f.op("dve", lambda e: e.tensor_scalar(out=junk[:, 0:L], in0=scb[:, 0:L], scalar1=0.0, scalar2=3.0e38, op0=ALU.add, op1=ALU.min,
                                                        accum_out=bs[:, 0:1]), sc_ks, [junk_k, bs_k])
            self.tt("dve", bs[:, 2:3], bs[:, 1:2], bs[:, 0:1], ALU.subtract, [bs_k], [bs_k])
            self.ts("dve", bs[:, 2:3], bs[:, 2:3], 0.5, ALU.mult, [bs_k], [bs_k])

        def topk(qs):
            qs = [q_ for q_ in qs if q_ >= 2]
            for it in range(K_IT):
                for q_ in qs:
                    L = (q_ + 1) * 128
                    scb, sc_ks = scbs[q_ % 4]
                    mb, mb_k = mbs[q_ % 4]
                    bs, bs_k = bss[q_ % 4]
                    self.tt("dve", bs[:, 3:4], bs[:, 0:1], bs[:, 2:3], ALU.add, [bs_k], [bs_k])
                    self.f.op("dve", lambda e, L=L, scb=scb, mb=mb, bs=bs: e.tensor_scalar(out=mb[:, 0:L], in0=scb[:, 0:L], scalar1=bs[:, 3:4], scalar2=0.0,
                                                                                          op0=ALU.is_ge, op1=ALU.add, accum_out=bs[:, 4:5]),
                              sc_ks + [bs_k], [mb_k, bs_k])
                for q_ in qs:
                    bs, bs_k = bss[q_ % 4]
                    self.stt(bs[:, 5:6], bs[:, 4:5], 256.0, bs[:, 2:3], ALU.is_ge, ALU.mult, [bs_k], [bs_k])
                    self.tt("dve", bs[:, 0:1], bs[:, 0:1], bs[:, 5:6], ALU.add, [bs_k], [bs_k])
                    self.ts("dve", bs[:, 2:3], bs[:, 2:3], 0.5, ALU.mult, [bs_k], [bs_k])

        def mask(qi):
            L = (qi + 1) * 128
            scb, sc_ks = scbs[qi % 4]
            mb, mb_k = mbs[qi % 4]
            bs, bs_k = bss[qi % 4]
            if qi >= 2:
                self.ts("dve", mb[:, 0:L], scb[:, 0:L], bs[:, 0:1], ALU.is_ge, sc_ks + [bs_k], [mb_k])
            else:
                self.ts("dve", mb[:, 0:L], scb[:, 0:L], 0.1 * NEGBIG, ALU.is_ge, sc_ks, [mb_k])

        def s3(qi):
            L = (qi + 1) * 128
            qsl = slice(qi * 128, L)
            q_sb, q_k = qbuf.pop(qi)
            mb, mb_k = mbs[qi % 4]
            mT, mT_k = mTring.next()
            for k0 in range(0, qi + 1, 4):
                nk = min(4, qi + 1 - k0)
                for kk in range(nk):
                    kt = k0 + kk
                    self.tr(ps_tb[:, kk * 128:(kk + 1) * 128], mb[:, kt * 128:(kt + 1) * 128], self.ident_b, [mb_k, self.cb_k], [ps_t[1]])
                self.cp("act", mT[:, k0:k0 + nk, :].rearrange("p k q -> p (k q)"), ps_tb[:, 0:nk * 128], [ps_t[1]], [mT_k])
            oT, oT_k = oTring.next()
            for n in range(2):
                pend = []

                def stage_a(kt):
                    ps, ps_k = ps_st.next()
                    self.mm(ps[:], kT[:, n, kt * 128:(kt + 1) * 128], q_sb[:, 4 * n:4 * n + 4, :].rearrange("p h q -> p (h q)"), True, True, [kT_k, q_k], [ps_k])
                    e, e_k = ering.next()
                    self.act(e[:], ps[:], AF.Exp, [ps_k], [e_k], scale=scale)
                    pT, pT_k = pring.next()
                    self.tt("pool", pT[:].rearrange("p (h q) -> p h q", h=4), e[:].rearrange("p (h q) -> p h q", h=4),
                            mT[:, kt:kt + 1, :].to_broadcast([128, 4, 128]), ALU.mult, [e_k, mT_k], [pT_k])
                    pend.append((kt, pT, pT_k))

                def stage_b():
                    kt, pT, pT_k = pend.pop(0)
                    self.mm(ps_o[0][:], V[:, kt, n * 128:(n + 1) * 128], pT[:], kt == 0, kt == qi, [V_k, pT_k], [ps_o[1]])
                    self.mm(ps_d[0][:], self.ones_b, pT[:], kt == 0, kt == qi, [self.cb_k, pT_k], [ps_d[1]])

                for kt in range(qi + 1):
                    stage_a(kt)
                    if len(pend) > 2:
                        stage_b()
                while pend:
                    stage_b()
                rd, rd_k = rdring.next()
                self.act(rd[:], ps_d[0][:], AF.Ln, [ps_d[1]], [rd_k])
                self.act(rd[:], rd[:], AF.Exp, [rd_k], [rd_k], scale=-1.0)
                o32, o32_k = o32ring.next()
                self.cp("act", o32[:], ps_o[0][:], [ps_o[1]], [o32_k])
                self.tt("pool", oT[:, 4 * n:4 * n + 4, :].rearrange("p h q -> p (h q)"), o32[:], rd[:], ALU.mult, [o32_k, rd_k], [oT_k])
            self.dma(oT_d[:, :, qsl], oT[:], [oT_k], [self.scr_k["even_oT"][qi // 4]])

        nquad = (nq + 3) // 4

        def quad(g):
            return [q_ for q_ in range(4 * g, 4 * g + 4) if q_ < nq]

        s1(quad(0))
        for g in range(nquad):
            qs = quad(g)
            loadq(qs)
            topk(qs)
            for q_ in qs:
                mask(q_)
            if g + 1 < nquad:
                s1(quad(g + 1))
            for q_ in qs:
                s3(q_)
        self.end_phase()

    def phase_even_proj(self, i, do_gdn=True):
        j = i // 2
        self.begin_phase()
        W = self.d["even_w_in"][j]
        NW = 2728
        win = self.sb([128, 8, NW], BF16, "win")
        win_k = [Tok() for _ in range(8)]
        wstage = self.ring(2, [128, NW // 2], F32, "wst")
        wq = self.sb([128, 3, 768], BF16, "wq"); wq_k = [Tok() for _ in range(3)]
        wkv = self.sb([128, 2, 1024], BF16, "wkv"); wkv_k = [Tok() for _ in range(2)]
        wv = self.sb([128, 2, 512], BF16, "wv")
        xstage = self.ring(3, [128, T], F32, "xst")
        sqring = self.ring(2, [128, T], BF16, "sq")
        xb, xb_k = self.sb([128, 8, T], BF16, "xb"), Tok()
        rstd, rstd_k = self.sb([128, T], F32, "rstd"), Tok()
        hns = [(self.sb([128, 8, T], BF16, "hn"), Tok()) for _ in range(2)]
        sq2 = self.ring(4, [128, T], BF16, "sq2")
        rring = self.ring(4, [128, T], F32, "rr")
        rawring = self.ring(5, [128, T], F32, "raw")
        cq_raw, cq_k = self.sb([128, 3, T], F32, "cqraw"), Tok()
        cqn, cqn_k = self.sb([128, 3, T], BF16, "cqn"), Tok()
        ckvn, ckvn_k = self.sb([128, 2, T], BF16, "ckvn"), Tok()
        kpe, kpe_k = self.sb([128, T], F32, "kpe"), Tok()
        ropes = [(self.sb([96, 2, T], F32, "rope"), Tok()) for _ in range(2)]
        nb_ring = self.ring(4, [96, T], BF16, "nb")
        t1_ring = self.ring(4, [96, T], F32, "t1")
        fin_ring = self.ring(4, [96, T], BF16, "fin")
        vext_ring = self.ring(2, [128, 8, 128], BF16, "vext")
        f32o = self.ring(4, [128, T], F32, "f32o")
        gbring = self.ring(2, [128, 8], F32, "gb")
        tmp4 = self.ring(2, [128, 4], F32, "tmp4")
        ssb = self.psum[0]
        pring = Ring([self.psum[1], self.psum[2], self.psum[3]])
        ssring = Ring([self.psum[4], self.psum[5], self.psum[6]])
        sring = Ring([self.psum[7]])
        pmt, pm_k = self.lconst(C_PM, 96, True)
        pm_b = pmt[0:96, 0:96]
        for c in range(8):
            for hf in range(2):
                self.load_w(win[:, c, hf * (NW // 2):(hf + 1) * (NW // 2)], win_k[c], W[c * 128:(c + 1) * 128, hf * (NW // 2):(hf + 1) * (NW // 2)], wstage, self.vcol(("ln_mix", i), c), 2 * c + hf)
        for c in range(3):
            self.load_w(wq[:, c, :], wq_k[c], self.d["mla_w_q_up"][j][c * 128:(c + 1) * 128, :], wstage, self.vcol(("mla_q_a_norm", j), c), c)
        for c in range(2):
            self.load_w(wkv[:, c, :], wkv_k[c], self.d["mla_w_kv_up"][j][c * 128:(c + 1) * 128, :], wstage, self.vcol(("mla_kv_a_norm", j), c), c)
            self.cp("pool", wv[:, c, :].rearrange("p (h d) -> p h d", d=64),
                    wkv[:, c, :].rearrange("p (h two d) -> p h two d", two=2, d=64)[:, :, 1, :], [wkv_k[c]], [wkv_k[c]])
        for (vx, vx_k) in vext_ring.items:
            self.f.op("pool", lambda e, vx=vx: e.memset(vx[:], 1.0), [], [vx_k])
        if do_gdn:
            convbuf, conv_k = self.sb([128, 12, 3 + T], F32, "convbuf"), [Tok() for _ in range(12)]
            self.f.op("pool", lambda e: e.memset(convbuf[:, :, 0:3], 0.0), [], conv_k)
            yring = self.ring(5, [128, T], F32, "y")
            negA, negA_k = self.sb([128, 4], F32, "negA"), Tok(hard=True)
            dtb, dtb_k = self.sb([128, 4], F32, "dtb"), Tok()
            self.dma(negA[:], self.d["gdn_a_log"][j:j + 1, :].partition_broadcast(128), [], [negA_k])
            self.dma(dtb[:], self.d["gdn_dt_bias"][j:j + 1, :].partition_broadcast(128), [], [dtb_k])
            self.act(negA[:], negA[:], AF.Exp, [negA_k], [negA_k])
            self.ts("dve", negA[:], negA[:], -1.0, ALU.mult, [negA_k], [negA_k])
        sc = self.scr
        sk = self.scr_k

        def load_rope(tt):
            rp, rp_k = ropes[tt % 2]
            self.dma(rp[:], self.d["rope"][:, :, tt * T:(tt + 1) * T].rearrange("a p t -> p a t"), [], [rp_k])

        self.load_hn(0, xstage, sqring, xb, xb_k, rstd, rstd_k, ssb, hns[0][0], hns[0][1])
        load_rope(0)
        for tt in range(NT):
            hn, hn_k = hns[tt % 2]
            rp, rp_k = ropes[tt % 2]
            if tt + 1 < NT:
                self.load_hn(tt + 1, xstage, sqring, xb, xb_k, rstd, rstd_k, ssb, hns[(tt + 1) % 2][0], hns[(tt + 1) % 2][1])
                load_rope(tt + 1)
            tsl = slice(tt * T, (tt + 1) * T)

            def proj(col, width=128, pbase=0):
                pp, pp_k = pring.next()
                for c in range(8):
                    self.mm(pp[pbase:pbase + width, :], win[:, c, col:col + width], hn[:, c, :], c == 0, c == 7, [win_k[c], hn_k], [pp_k])
                return pp, pp_k

            def latent(col0, nch, raw, raw_k, dst, dst_k):
                ss, ss_k = ssring.next()
                for c in range(nch):
                    pp, pp_k = proj(col0 + c * 128)
                    self.cp("act", raw[:, c, :], pp[:], [pp_k], [raw_k])
                    sq, sq_k = sq2.next()
                    self.act(sq[:], pp[:], AF.Square, [pp_k], [sq_k])
                    self.mm(ss[:], self.ones_b, sq[:], c == 0, c == nch - 1, [sq_k, self.cb_k], [ss_k])
                r, r_k = rring.next()
                self.act(r[:], ss[:], AF.Sqrt, [ss_k], [r_k], scale=1.0 / (nch * 128), bias=EPS)
                self.recip(r[:], r[:], [r_k], [r_k])
                for c in range(nch):
                    self.tt(("dve", "pool")[c % 2], dst[:, c, :], raw[:, c, :], r[:], ALU.mult, [raw_k, r_k], [dst_k])

            latent(0, 3, cq_raw, cq_k, cqn, cqn_k)
            latent(384, 2, cq_raw, cq_k, ckvn, ckvn_k)
            pp, pp_k = proj(640, 32, 64)
            self.cp("act", kpe[64:96, :], pp[64:96, :], [pp_k], [kpe_k])

            def norm_rope(raw, raw_k, gkey, dst, dst_tok):
                sq, sq_k = sq2.next()
                self.act(sq[0:96, :], raw[0:96, :], AF.Square, [raw_k], [sq_k])
                ss, ss_k = ssring.next()
                self.mm(ss[0:96, :], self.ones_b[0:96, 0:96], sq[0:96, :], True, True, [sq_k, self.cb_k], [ss_k])
                r, r_k = rring.next()
                self.act(r[0:96, :], ss[0:96, :], AF.Sqrt, [ss_k], [r_k], scale=1.0 / 96, bias=EPS)
                self.recip(r[0:96, :], r[0:96, :], [r_k], [r_k])
                nb, nb_k = nb_ring.next()
                self.stt(nb[:], raw[0:96, :], self.vcol(gkey, 0, 96), r[0:96, :], ALU.mult, ALU.mult, [raw_k, r_k, self.vec_k], [nb_k])
                rot, rot_k = ssring.next()
                self.mm(rot[0:96, :], pm_b, nb[:], True, True, [nb_k, pm_k], [rot_k])
                t1, t1_k = t1_ring.next()
                self.tt("pool", t1[:], nb[:], rp[:, 0, :], ALU.mult, [nb_k, rp_k], [t1_k])
                fin, fin_k = fin_ring.next()
                self.tt("dve", fin[:], rot[0:96, :], rp[:, 1, :], ALU.mult, [rot_k, rp_k], [fin_k])
                self.tt("pool", fin[:], fin[:], t1[:], ALU.add, [fin_k, t1_k], [fin_k])
                self.dma(dst, fin[:], [fin_k], [dst_tok])

            for h in range(8):
                pp, pp_k = pring.next()
                for c in range(3):
                    self.mm(pp[0:96, :], wq[:, c, h * 96:(h + 1) * 96], cqn[:, c, :], c == 0, c == 2, [wq_k[c], cqn_k], [pp_k])
                raw, raw_k = rawring.next()
                self.cp("act", raw[0:96, :], pp[0:96, :], [pp_k], [raw_k])
                norm_rope(raw, raw_k, ("mla_q_norm", j), sc["mla_qT"][h, :, tsl], sk["mla_qT"][tt])
            for h in range(8):
                pp, pp_k = pring.next()
                for c in range(2):
                    self.mm(pp[0:64, :], wkv[:, c, h * 128:h * 128 + 64], ckvn[:, c, :], c == 0, c == 1, [wkv_k[c], ckvn_k], [pp_k])
                raw, raw_k = rawring.next()
                self.cp("act", raw[0:64, :], pp[0:64, :], [pp_k], [raw_k])
                self.cp("pool", raw[64:96, :], kpe[64:96, :], [kpe_k], [raw_k])
                norm_rope(raw, raw_k, ("mla_k_norm", j), sc["mla_kT"][h, :, tsl], sk["mla_kT"][tt])
            for sub in range(4):
                sp_, sp_k = sring.next()
                for c in range(2):
                    self.mm(sp_[:], ckvn[:, c, sub * 128:(sub + 1) * 128], wv[:, c, :], c == 0, c == 1, [ckvn_k, wkv_k[c]], [sp_k])
                vx, vx_k = vext_ring.next()
                self.cp("act", vx[:, :, 0:64], sp_[:].rearrange("p (h d) -> p h d", d=64), [sp_k], [vx_k])
                r0 = tt * T + sub * 128
                self.dma(sc["mla_vext"][:, r0:r0 + 128, :].rearrange("h t d -> t h d"), vx[:], [vx_k], [sk["mla_vext"][tt]])
            if not do_gdn:
                continue
            cw = VOFF[("gdn_conv_w", j)]
            for g0 in range(0, 12, 4):
                grp = list(range(g0, g0 + 4))
                ys = {}
                for c in grp:
                    pp, pp_k = proj(672 + c * 128)
                    self.cp("act", convbuf[:, c, 3:3 + T], pp[:], [pp_k], [conv_k[c]])
                    y, y_k = yring.next()
                    ys[c] = (y, y_k)
                    self.act(y[:], convbuf[:, c, 0:T], AF.Copy, [conv_k[c], self.vec_k], [y_k], scale=self.vec[:, cw + c * 4:cw + c * 4 + 1])
                for tap in range(1, 4):
                    for c in grp:
                        y, y_k = ys[c]
                        self.stt(y[:], convbuf[:, c, tap:tap + T], self.vec[:, cw + c * 4 + tap:cw + c * 4 + tap + 1], y[:], ALU.mult, ALU.add,
                                 [conv_k[c], y_k, self.vec_k], [y_k])
                for c in grp:
                    y, y_k = ys[c]
                    self.cp("pool", convbuf[:, c, 0:3], convbuf[:, c, T:T + 3], [conv_k[c]], [conv_k[c]])
                    self.act(y[:], y[:], AF.Silu, [y_k], [y_k])
                if g0 < 8:
                    st_ = {}
                    for c in grp:
                        y, y_k = ys[c]
                        sq, sq_k = sq2.next()
                        self.act(sq[:], y[:], AF.Square, [y_k], [sq_k])
                        st_[c] = [sq, sq_k]
                    for c in grp:
                        sq, sq_k = st_[c]
                        ss, ss_k = ssring.next()
                        self.mm(ss[:], self.ones_b, sq[:], True, True, [sq_k, self.cb_k], [ss_k])
                        r, r_k = rring.next()
                        self.act(r[:], ss[:], AF.Sqrt, [ss_k], [r_k], scale=1.0, bias=EPS)
                        st_[c] = [r, r_k]
                    for c in grp:
                        r, r_k = st_[c]
                        self.recip(r[:], r[:], [r_k], [r_k])
                    for c in grp:
                        y, y_k = ys[c]
                        r, r_k = st_[c]
                        ob, ob_k = f32o.next()
                        self.stt(ob[:], y[:], (128.0 ** -0.5) if c < 4 else 1.0, r[:], ALU.mult, ALU.mult, [y_k, r_k], [ob_k])
                        nm = "gdn_qT" if c < 4 else "gdn_kT"
                        self.dma(sc[nm][c % 4, :, tsl], ob[:], [ob_k], [sk[nm][tt]])
                else:
                    for c in grp:
                        y, y_k = ys[c]
                        self.dma(sc["gdn_vT"][c - 8, :, tsl], y[:], [y_k], [sk["gdn_vT"][tt]])
            for c in range(4):
                pp, pp_k = proj(2208 + c * 128)
                ob, ob_k = f32o.next()
                self.act(ob[:], pp[:], AF.Silu, [pp_k], [ob_k])
                self.dma(sc["gdn_zs"][c, :, tsl], ob[:], [ob_k], [sk["gdn_zs"][tt]])
            for sub in range(4):
                sp_, sp_k = sring.next()
                for c in range(8):
                    self.mm(sp_[:, 0:8], hn[:, c, sub * 128:(sub + 1) * 128], win[:, c, 2720:2728], c == 0, c == 7, [win_k[c], hn_k], [sp_k])
                gb, gb_k = gbring.next()
                t4, t4_k = tmp4.next()
                self.tt("dve", t4[:], sp_[:, 0:4], dtb[:], ALU.add, [sp_k, dtb_k], [t4_k])
                self.act(t4[:], t4[:], AF.Exp, [t4_k], [t4_k])
                self.act(t4[:], t4[:], AF.Ln, [t4_k], [t4_k], bias=1.0)
                self.tt("dve", gb[:, 0:4], t4[:], negA[:], ALU.mult, [t4_k, negA_k], [gb_k])
                self.act(gb[:, 4:8], sp_[:, 4:8], AF.Sigmoid, [sp_k], [gb_k])
                r0 = tt * T + sub * 128
                self.dma(sc["gdn_gb"][r0:r0 + 128, :], gb[:], [gb_k], [sk["gdn_gb"][tt]])
        self.end_phase()

    def phase_mla_attn(self, i, nqt=NT):
        self.begin_phase()
        sc = self.scr
        kTs = [(self.sb([96, S], BF16, "kT"), Tok()) for _ in range(2)]
        Vs = [(self.sb([128, 32, 128], BF16, "V"), Tok()) for _ in range(2)]
        qring = self.ring(2, [96, T], BF16, "q")
        ering = self.ring(6, [128, T], BF16, "e")
        rden, rden_k = self.sb([64, T], F32, "rden"), Tok()
        oring = self.ring(2, [64, T], BF16, "o")
        ps_st = Ring([self.psum[0], self.psum[1], self.psum[2], self.psum[5]])
        ps_o = Ring([self.psum[3], self.psum[4]])
        scale = 96.0 ** -0.5
        cmt, cm_k = self.lconst(C_CM, 2048, True)
        cm = cmt[:, :]

        def load_head(h):
            kT, kT_k = kTs[h % 2]
            self.dma(kT[:], sc["mla_kT"][h], [], [kT_k])
            V, V_k = Vs[h % 2]
            self.dma(V[:], sc["mla_vext"][h].rearrange("(kt p) d -> p kt d", p=128), [], [V_k])

        load_head(0)
        for h in range(8):
            if h + 1 < 8:
                load_head(h + 1)
            kT, kT_k = kTs[h % 2]
            V, V_k = Vs[h % 2]
            for qt in range(nqt):
                q, q_k = qring.next()
                self.dma(q[:], sc["mla_qT"][h, :, qt * T:(qt + 1) * T], [], [q_k])
                po, po_k = ps_o.next()
                nk = 4 * qt + 4
                pend = []
                for kt in range(nk):
                    ps, ps_k = ps_st.next()
                    self.mm(ps[:], kT[:, kt * 128:(kt + 1) * 128], q[:], True, True, [kT_k, q_k], [ps_k])
                    e, e_k = ering.next()
                    self.act(e[:], ps[:], AF.Exp, [ps_k], [e_k], scale=scale)
                    off = kt - 4 * qt
                    if off >= 0:
                        self.tt("pool", e[:], e[:], cm[:, off * 512:(off + 1) * 512], ALU.mult, [e_k, cm_k], [e_k])
                    pend.append((kt, e, e_k))
                    if len(pend) > 2:
                        kt2, e2, e2_k = pend.pop(0)
                        self.mm(po[:], V[:, kt2, :], e2[:], kt2 == 0, kt2 == nk - 1, [V_k, e2_k], [po_k])
                while pend:
                    kt2, e2, e2_k = pend.pop(0)
                    self.mm(po[:], V[:, kt2, :], e2[:], kt2 == 0, kt2 == nk - 1, [V_k, e2_k], [po_k])
                self.recip(rden[:], po[64:128, :], [po_k], [rden_k])
                o, o_k = oring.next()
                self.tt("dve", o[:], po[0:64, :], rden[:], ALU.mult, [po_k, rden_k], [o_k])
                self.dma(sc["even_oT"][h * 64:(h + 1) * 64, qt * T:(qt + 1) * T], o[:], [o_k], [self.scr_k["even_oT"][qt]])
        self.end_phase()

    def phase_even_out(self, i, ntt=NT, wname="even_w_out"):
        j = i // 2
        if ("ffn", i, 2) in self.phases:
            self.ffn_weights_begin(i, 2)
        self.begin_phase()
        wout = self.sb([128, 8, D], BF16, "wout")
        wout_k = [Tok() for _ in range(8)]
        wstage = self.ring(2, [128, D], F32, "wst")
        for c in range(8):
            self.load_w(wout[:, c, :], wout_k[c], self.d[wname][j][c * 128:(c + 1) * 128, :], wstage, None, c)
        oring = self.ring(2, [128, 8, T], BF16, "oT")
        xstage = self.ring(4, [128, T], F32, "xst")
        obring = self.ring(3, [128, T], F32, "ob")
        yps = Ring([self.psum[0], self.psum[1], self.psum[2]])
        oT_d = self.scr["even_oT"].rearrange("(c p) t -> p c t", p=128)
        for tt in range(ntt):
            tsl = slice(tt * T, (tt + 1) * T)
            o, o_k = oring.next()
            self.dma(o[:], oT_d[:, :, tsl], [], [o_k])
            for m in range(8):
                yp, yp_k = yps.next()
                for c in range(8):
                    self.mm(yp[:], wout[:, c, m * 128:(m + 1) * 128], o[:, c, :], c == 0, c == 7, [wout_k[c], o_k], [yp_k])
                xs, xs_k = xstage.next()
                self.dma(xs[:], self.xsrc[m * 128:(m + 1) * 128, tsl], [self.xtok[tt][m]], [xs_k])
                ob, ob_k = obring.next()
                self.tt("dve", ob[:], yp[:], xs[:], ALU.add, [yp_k, xs_k], [ob_k])
                self.dma(self.xres[m * 128:(m + 1) * 128, tsl], ob[:], [ob_k], [self.xtok[tt][m]], q="act")
            self.pref_step(7)
        self.pref_step()
        self.end_phase()

    def phase_gdn(self, i, nsc=32, stop=None):
        j = i // 2
        self.begin_phase()
        sc = self.scr
        gct, gc_k0 = self.lconst(C_MS, 386)
        MS = gct[:, 0:128]
        MI = gct[:, 128:256]
        UI = gct[:, 256:384]
        CIND = gct[:, 384:386]
        self.f.barrier()
        ck = self.cst_k

        def t4(name, n=1):
            return [(self.sb([128, 4, 128], F32, name), Tok()) for _ in range(n)]
        kTs = t4("kT", 2); qTs = t4("qT", 2); vTs = t4("vT", 2); zss = t4("zs", 2)
        gbs = [(self.sb([128, 8], F32, "gb"), Tok()) for _ in range(2)]
        (GU, GU_k), (gB, gB_k), (dec, dec_k), (dmI, dmI_k), (dmS, dmS_k) = t4("GU")[0], t4("gB")[0], t4("dec")[0], t4("dmI")[0], t4("dmS")[0]
        (qk, qk_k), (Nn, Nn_k), (qkT, qkT_k) = t4("qk")[0], t4("N")[0], t4("qkT")[0]
        Ps = t4("P", 2); Qs = t4("Q", 2)
        (X, X_k), (vb, vb_k), (kbg, kbg_k), (kd, kd_k) = t4("X")[0], t4("vb")[0], t4("kbg")[0], t4("kd")[0]
        (u, u_k), (wT, wT_k), (vnew, vnew_k), (o2s, o2s_k), (o, o_k) = t4("u")[0], t4("wT")[0], t4("vnew")[0], t4("o2s")[0], t4("o")[0]
        (oTs, oTs_k), (rr, rr_k), (fin32, fin32_k) = t4("oTs")[0], t4("rr")[0], t4("fin32")[0]
        sqb, sqb_k = self.sb([128, 4, 128], BF16, "sqb"), Tok()

        def b4(name):
            return self.sb([128, 4, 128], BF16, name), Tok()
        (qTb, qTb_k), (wTb, wTb_k), (Sb, Sb_k), (vnb, vnb_k), (qkTb, qkTb_k), (kdb, kdb_k) = b4("qTb"), b4("wTb"), b4("Sb"), b4("vnb"), b4("qkTb"), b4("kdb")
        self.f.op("pool", lambda e: e.memset(Sb[:], 0.0), [], [Sb_k])
        finb = self.ring(2, [128, 4, 128], BF16, "finb")
        Sst, S_k = self.sb([128, 4, 128], F32, "Sst"), Tok()
        self.f.op("pool", lambda e: e.memset(Sst[:], 0.0), [], [S_k])
        sm, sm_k = self.sb([128, 32], F32, "sm"), Tok(hard=True)
        B = self.psum
        oT_d = sc["even_oT"].rearrange("(c p) t -> p c t", p=128)

        def F(ap):
            return ap.rearrange("p h x -> p (h x)")

        def load(sci):
            t0 = sci * 128
            for nm, bufs in (("gdn_kT", kTs), ("gdn_qT", qTs), ("gdn_vT", vTs), ("gdn_zs", zss)):
                b, b_k = bufs[sci % 2]
                self.dma(b[:], sc[nm][:, :, t0:t0 + 128].rearrange("h d t -> d h t"), [], [b_k])
            gb, gb_k = gbs[sci % 2]
            self.dma(gb[:], sc["gdn_gb"][t0:t0 + 128, :], [], [gb_k])

        load(0)
        for sci in range(nsc):
            if sci + 1 < nsc:
                load(sci + 1)
            t0 = sci * 128
            kT, kT_k = kTs[sci % 2]; qT, qT_k = qTs[sci % 2]; vT, vT_k = vTs[sci % 2]; zs, zs_k = zss[sci % 2]
            gb, gb_k = gbs[sci % 2]
            for h in range(4):
                self.act(GU[:, h, :], UI, AF.Copy, [ck, gb_k], [GU_k], scale=gb[:, h:h + 1])
                self.act(gB[:, h, :], self.ones_f, AF.Copy, [ck, gb_k], [gB_k], scale=gb[:, h:h + 1])
            for h in range(4):
                self.mm(B[3][0][:, 2 * h:2 * h + 2], GU[:, h, :], self.ones_f[:, 0:2], True, True, [GU_k, ck], [B[3][1]])
                self.mm(B[3][0][:, 8 + 2 * h:10 + 2 * h], gB[:, h, :], CIND, True, True, [gB_k, ck], [B[3][1]])
                self.mm(B[0][0][:, h * 128:(h + 1) * 128], GU[:, h, :], MS, True, True, [GU_k, ck], [B[0][1]])
            self.cp("dve", sm[:, 0:4], B[3][0][:, 0:8].rearrange("p (h two) -> p h two", two=2)[:, :, 0], [B[3][1]], [sm_k])
            self.cp("dve", sm[:, 4:12], B[3][0][:, 8:16], [B[3][1]], [sm_k])
            for h in range(4):
                self.tt("pool", sm[0:64, 12 + h:13 + h], sm[0:64, 4 + 2 * h:5 + 2 * h], sm[0:64, h:h + 1], ALU.subtract, [sm_k], [sm_k])
                self.tt("pool", sm[64:128, 12 + h:13 + h], sm[64:128, 5 + 2 * h:6 + 2 * h], sm[64:128, h:h + 1], ALU.subtract, [sm_k], [sm_k])
            self.act(sm[:, 0:16], sm[:, 0:16], AF.Exp, [sm_k], [sm_k])
            self.ts("pool", sm[:, 16:20], gb[:, 4:8], -1.0, ALU.mult, [gb_k, sm_k], [sm_k])
            self.tt("pool", sm[:, 20:24], gb[:, 4:8], sm[:, 0:4], ALU.mult, [gb_k, sm_k], [sm_k])
            self.act(F(dec[:]), B[0][0][:], AF.Exp, [B[0][1]], [dec_k])
            self.tt("pool", dmI[:], dec[:], MI.unsqueeze(1).to_broadcast([128, 4, 128]), ALU.mult, [dec_k, ck], [dmI_k])
            self.tt("pool", dmS[:], dec[:], MS.unsqueeze(1).to_broadcast([128, 4, 128]), ALU.mult, [dec_k, ck], [dmS_k])
            for h in range(4):
                self.mm(B[1][0][:, h * 128:(h + 1) * 128], kT[:, h, :], kT[:, h, :], True, True, [kT_k], [B[1][1]])
                self.mm(B[2][0][:, h * 128:(h + 1) * 128], qT[:, h, :], kT[:, h, :], True, True, [qT_k, kT_k], [B[2][1]])
            self.tt("dve", F(qk[:]), B[2][0][:], F(dmI[:]), ALU.mult, [B[2][1], dmI_k], [qk_k])
            for h in range(4):
                self.stt(Nn[:, h, :], B[1][0][:, h * 128:(h + 1) * 128], sm[:, 16 + h:17 + h], dmS[:, h, :], ALU.mult, ALU.mult, [B[1][1], sm_k, dmS_k], [Nn_k])
            for h in range(4):
                self.tr(B[0][0][:, h * 128:(h + 1) * 128], Nn[:, h, :], self.ident_f, [Nn_k, ck], [B[0][1]])
                self.tr(B[1][0][:, h * 128:(h + 1) * 128], qk[:, h, :], self.ident_f, [qk_k, ck], [B[1][1]])
            P, P_k = Ps[0]
            Q, Q_k = Nn, Nn_k
            self.cp("act", F(P[:]), B[0][0][:], [B[0][1]], [P_k])
            self.cp("act", F(qkTb[:]), B[1][0][:], [B[1][1]], [qkTb_k])
            if stop == 'A':
                continue
            self.tt("pool", X[:], P[:], self.ident_f.unsqueeze(1).to_broadcast([128, 4, 128]), ALU.add, [P_k, ck], [X_k])
            for lv in range(1, 6):
                Pn, Pn_k = Ps[lv % 2]
                Qn, Qn_k = Qs[lv % 2]
                for h in range(4):
                    if lv < 5:
                        self.mm(B[1][0][:, h * 128:(h + 1) * 128], Q[:, h, :], P[:, h, :], True, True, [Q_k, P_k], [B[1][1]])
                    self.mm(B[2][0][:, h * 128:(h + 1) * 128], P[:, h, :], Q[:, h, :], True, True, [Q_k, P_k], [B[2][1]])
                if lv < 5:
                    self.cp("act", F(Pn[:]), B[1][0][:], [B[1][1]], [Pn_k])
                self.cp("dve", F(Qn[:]), B[2][0][:], [B[2][1]], [Qn_k])
                for h in range(4):
                    self.mm(B[0][0][:, h * 128:(h + 1) * 128], Qn[:, h, :], X[:, h, :], True, True, [Qn_k, X_k], [B[0][1]])
                self.tt("dve", F(X[:]), F(X[:]), B[0][0][:], ALU.add, [X_k, B[0][1]], [X_k])
                P, P_k, Q, Q_k = Pn, Pn_k, Qn, Qn_k
            if stop == 'B':
                continue
            for h in range(4):
                self.tr(B[1][0][:, h * 128:(h + 1) * 128], vT[:, h, :], self.ident_f, [vT_k, ck], [B[1][1]])
                self.tr(B[2][0][:, h * 128:(h + 1) * 128], kT[:, h, :], self.ident_f, [kT_k, ck], [B[2][1]])
            for h in range(4):
                self.act(vb[:, h, :], B[1][0][:, h * 128:(h + 1) * 128], AF.Copy, [B[1][1], gb_k], [vb_k], scale=gb[:, 4 + h:5 + h])
                self.ts("dve", kbg[:, h, :], B[2][0][:, h * 128:(h + 1) * 128], sm[:, 20 + h:21 + h], ALU.mult, [B[2][1], sm_k], [kbg_k])
                self.act(kdb[:, h, :], B[2][0][:, h * 128:(h + 1) * 128], AF.Copy, [B[2][1], sm_k], [kdb_k], scale=sm[:, 12 + h:13 + h])
            for h in range(4):
                self.mm(B[0][0][:, h * 128:(h + 1) * 128], X[:, h, :], vb[:, h, :], True, True, [X_k, vb_k], [B[0][1]])
                self.mm(B[1][0][:, h * 128:(h + 1) * 128], kbg[:, h, :], X[:, h, :], True, True, [X_k, kbg_k], [B[1][1]])
            self.cp("act", F(u[:]), B[0][0][:], [B[0][1]], [u_k])
            self.cp("dve", F(wTb[:]), B[1][0][:], [B[1][1]], [wTb_k])
            if stop == 'C':
                continue
            self.cp("pool", qTb[:], qT[:], [qT_k], [qTb_k])
            for ch in range(2):
                r = slice(ch * 64, ch * 64 + 64)
                for h in range(4):
                    self.mm(B[4][0][r, h * 128:(h + 1) * 128], wTb[:, h, r], Sb[:, h, :], True, True, [wTb_k, Sb_k], [B[4][1]])
                self.tt("dve", F(vnb[r]), F(u[r]), B[4][0][r, :], ALU.subtract, [u_k, B[4][1]], [vnb_k])
                for h in range(4):
                    self.mm(B[5][0][r, h * 128:(h + 1) * 128], qTb[:, h, r], Sb[:, h, :], True, True, [qTb_k, Sb_k], [B[5][1]])
                for h in range(4):
                    self.mm(B[6][0][r, h * 128:(h + 1) * 128], qkTb[r, h, r], vnb[r, h, :], True, True, [qkTb_k, vnb_k], [B[6][1]])
                self.cp("act", F(o2s[r]), B[6][0][r, :], [B[6][1]], [o2s_k])
                for h in range(4):
                    self.stt(o[r, h, :], B[5][0][r, h * 128:(h + 1) * 128], sm[r, h:h + 1], o2s[r, h, :], ALU.mult, ALU.add, [B[5][1], sm_k, o2s_k], [o_k])
                for h in range(4):
                    self.mm(B[7][0][:, h * 128:(h + 1) * 128], kdb[r, h, :], vnb[r, h, :], True, True, [kdb_k, vnb_k], [B[7][1]])
                for h in range(4):
                    self.stt(Sst[:, h, :], Sst[:, h, :], sm[:, 4 + 2 * h + ch:5 + 2 * h + ch], B[7][0][:, h * 128:(h + 1) * 128], ALU.mult, ALU.add, [S_k, sm_k, B[7][1]], [S_k])
                self.cp("pool", Sb[:], Sst[:], [S_k], [Sb_k])
            if stop == 'D':
                continue
            for h in range(4):
                self.tr(B[2][0][:, h * 128:(h + 1) * 128], o[:, h, :], self.ident_f, [o_k, ck], [B[2][1]])
            self.cp("act", F(oTs[:]), B[2][0][:], [B[2][1]], [oTs_k])
            self.act(F(sqb[:]), B[2][0][:], AF.Square, [B[2][1]], [sqb_k])
            self.mm(B[3][0][:], self.ones_b, F(sqb[:]), True, True, [sqb_k, self.cb_k], [B[3][1]])
            self.act(F(rr[:]), B[3][0][:], AF.Sqrt, [B[3][1]], [rr_k], scale=1.0 / 128, bias=EPS)
            self.recip(F(rr[:]), F(rr[:]), [rr_k], [rr_k])
            self.stt(F(fin32[:]), F(oTs[:]), self.vcol(("gdn_out_norm", j)), F(rr[:]), ALU.mult, ALU.mult, [oTs_k, rr_k, self.vec_k], [fin32_k])
            fb, fb_k = finb.next()
            self.tt("pool", fb[:], fin32[:], zs[:], ALU.mult, [fin32_k, zs_k], [fb_k])
            self.dma(oT_d[:, 4:8, t0:t0 + 128], fb[:], [fb_k], [self.scr_k["even_oT"][sci // 4]])
        self.end_phase()

    def phase_zero_gdn(self):
        self.begin_phase()
        z, z_k = self.sb([128, T], BF16, "z"), Tok()
        self.f.op("pool", lambda e: e.memset(z[:], 0.0), [], [z_k])
        for c in range(4, 8):
            for tt in range(NT):
                self.dma(self.scr["even_oT"][c * 128:(c + 1) * 128, tt * T:(tt + 1) * T], z[:], [z_k], [self.scr_k["even_oT"][tt]])
        self.end_phase()

    def phase_copy(self):
        self.begin_phase()
        xstage = self.ring(4, [128, T], F32, "xst")
        for tt in range(NT):
            for c in range(8):
                xs, xs_k = xstage.next()
                self.dma(xs[:], self.xsrc[c * 128:(c + 1) * 128, tt * T:(tt + 1) * T], [self.xtok[tt][c]], [xs_k])
                self.dma(self.xres[c * 128:(c + 1) * 128, tt * T:(tt + 1) * T], xs[:], [xs_k], [self.xtok[tt][c]])
        self.end_phase()
        self.xsrc = self.xres

    def build(self):
        self.setup()
        for ph in self.phases:
            kind = ph[0]
            if kind == "ffn":
                self.phase_ffn(ph[1], ph[2])
            elif kind == "ple":
                self.phase_ple(ph[1])
            elif kind == "copy":
                self.phase_copy()
            elif kind == "evenproj":
                self.phase_even_proj(ph[1], *ph[2:])
            elif kind == "mla":
                self.phase_mla_attn(ph[1], *ph[2:])
            elif kind == "evenout":
                self.phase_even_out(ph[1], *ph[2:])
            elif kind == "gdn":
                self.phase_gdn(ph[1], *ph[2:])
            elif kind == "oddout":
                self.phase_even_out(ph[1], ph[2] if len(ph) > 2 else NT, "odd_w_out")
            elif kind == "zero_gdn":
                self.phase_zero_gdn()
            elif kind == "oddproj":
                self.phase_odd_proj(ph[1])
            elif kind == "dsa":
                self.phase_dsa(ph[1], *ph[2:])
            else:
                raise ValueError(ph)
        self.f.barrier()
        self.f.emit()
        self.gst.close()
        self.f.close()
        return self.nc


def all_phases():
    ph = []
    for i in range(DEPTH):
        ph.append(("ffn", i, 1))
        if i % 2 == 0:
            ph += [("evenproj", i), ("mla", i), ("gdn", i), ("evenout", i)]
        else:
            ph += [("oddproj", i), ("dsa", i), ("oddout", i)]
        ph.append(("ffn", i, 2))
        ph.append(("ple", i))
    return ph


def make_in_maps(inputs, cores):
    vecs = pack_vecs(inputs)
    consts = make_consts()
    rope = make_rope()
    x = np.asarray(inputs["x"])
    p = np.asarray(inputs["p"])
    shared = {k: np.ascontiguousarray(np.asarray(inputs[k], dtype=np.float32)) for k in WEIGHT_SHAPES}
    maps = []
    for b in cores:
        m = dict(shared)
        m["xT"] = np.ascontiguousarray(x[b].T)
        m["pT"] = np.ascontiguousarray(p[:, b].transpose(0, 2, 1))
        m["vecs"] = vecs
        m["consts"] = consts
        m["rope"] = rope
        maps.append(m)
    return maps


def kernel(**inputs):
    prog = Prog(all_phases())
    nc = prog.build()
    maps = make_in_maps(inputs, list(range(8)))
    res = run_bass_kernel_spmd(nc, maps, core_ids=list(range(8)))
    out = np.stack([np.ascontiguousarray(r["outT"].T) for r in res.results], axis=0)
    return out.astype(np.float32)
```

```python
import numpy as np
from contextlib import ExitStack
import concourse.bass as bass
import concourse.mybir as mybir
from concourse.bass_utils import run_bass_kernel_spmd

F32 = mybir.dt.float32
BF16 = mybir.dt.bfloat16
AF = mybir.ActivationFunctionType
ALU = mybir.AluOpType

S = 4096
D = 1024
T = 512
NT = S // T
DFF = 2816
NJ = DFF // 128
EPS = 1e-6
DEPTH = 4
PLE = 256

SAME_ENGINE_SYNC = ("act", "dve", "pool")
SYNC_ALL_SAME = True


class Tok:
    __slots__ = ("w", "re", "rd", "excl", "hard")

    def __init__(self, excl=False, hard=False):
        self.w = None
        self.re = {}
        self.rd = []
        self.hard = hard
        self.excl = excl


class Fw:
    ENG = ("pe", "act", "dve", "pool", "sp")

    def __init__(self, nc, n_dma_sems=48):
        self.nc = nc
        self.stack = ExitStack()
        self.sem = {e: self.stack.enter_context(nc.semaphore("s_" + e)) for e in self.ENG}
        self.dsem = [self.stack.enter_context(nc.semaphore("d%d" % i)) for i in range(n_dma_sems)]
        self.dcnt = [0] * n_dma_sems
        self.dnext = 0
        self.ops = {e: [] for e in self.ENG}
        self.nops = {e: 0 for e in self.ENG}
        self.signal = {e: set() for e in self.ENG}
        self.seen_e = {e: {f: -1 for f in self.ENG} for e in self.ENG}
        self.seen_d = {e: [0] * n_dma_sems for e in self.ENG}

    def _need(self, eng, ev, hard=True):
        if ev is None:
            return
        if ev[0] == "e":
            _, f, idx = ev
            if f == eng and (eng not in SAME_ENGINE_SYNC or not (hard or SYNC_ALL_SAME)):
                return
            if self.seen_e[eng][f] >= idx:
                return
            self.seen_e[eng][f] = idx
            self.signal[f].add(idx)
            self.ops[eng].append(("wait_e", f, idx))
        else:
            _, s, val = ev
            if self.seen_d[eng][s] >= val:
                return
            self.seen_d[eng][s] = val
            self.ops[eng].append(("wait_d", s, val))

    def _deps(self, eng, reads, writes):
        for t in reads:
            self._need(eng, t.w, t.hard)
            if t.excl:
                for f, idx in t.re.items():
                    if f != eng:
                        self._need(eng, ("e", f, idx))
        for t in writes:
            self._need(eng, t.w, t.hard)
            for f, idx in t.re.items():
                self._need(eng, ("e", f, idx), t.hard)
            for r in t.rd:
                self._need(eng, r)

    def _commit(self, ev, reads, writes):
        for t in reads:
            if ev[0] == "e":
                t.re[ev[1]] = ev[2]
            else:
                t.rd.append(ev)
        for t in writes:
            t.w = ev
            t.re = {}
            t.rd = []

    def op(self, eng, fn, reads=(), writes=()):
        self._deps(eng, reads, writes)
        idx = self.nops[eng]
        self.nops[eng] += 1
        self.ops[eng].append(("op", fn, idx))
        ev = ("e", eng, idx)
        self._commit(ev, reads, writes)
        return ev

    def dma(self, q, out, in_, reads=(), writes=(), **kw):
        self._deps(q, reads, writes)
        s = self.dnext
        self.dnext = (self.dnext + 1) % len(self.dsem)
        if self.dcnt[s] > 0:
            self._need(q, ("d", s, self.dcnt[s]))
        self.dcnt[s] += 16
        val = self.dcnt[s]
        self.ops[q].append(("dma", out, in_, s, kw))
        ev = ("d", s, val)
        self._commit(ev, reads, writes)
        return ev

    def barrier(self):
        last = {}
        for e in self.ENG:
            if self.nops[e] > 0:
                last[e] = ("e", e, self.nops[e] - 1)
        for e in self.ENG:
            for f_, ev in last.items():
                if f_ != e:
                    self._need(e, ev)
            for s in range(len(self.dsem)):
                if self.dcnt[s] > 0:
                    self._need(e, ("d", s, self.dcnt[s]))

    def emit(self):
        nc = self.nc
        cnt = {}
        for e in self.ENG:
            m = {}
            c = 0
            for idx in sorted(self.signal[e]):
                c += 1
                m[idx] = c
            cnt[e] = m
        self.sigcount = {e: len(cnt[e]) for e in self.ENG}

        def run(e):
            def body(engh):
                for it in self.ops[e]:
                    k = it[0]
                    if k == "op":
                        ins = it[1](engh)
                        if it[2] in cnt[e]:
                            ins.then_inc(self.sem[e], 1)
                    elif k == "wait_e":
                        engh.wait_ge(self.sem[it[1]], cnt[it[1]][it[2]])
                    elif k == "wait_d":
                        engh.wait_ge(self.dsem[it[1]], it[2])
                    elif k == "dma":
                        engh.dma_start(out=it[1], in_=it[2], **it[4]).then_inc(self.dsem[it[3]], 16)
            return body

        with nc.Block() as block:
            block.sync(run("sp"))
            block.scalar(run("act"))
            block.vector(run("dve"))
            block.gpsimd(run("pool"))
            block.tensor(run("pe"))

    def close(self):
        self.stack.close()


class Ring:
    def __init__(self, items):
        self.items = items
        self.i = 0

    def next(self):
        it = self.items[self.i]
        self.i = (self.i + 1) % len(self.items)
        return it


def _vec_layout():
    off = {}
    n = 0
    for i in range(DEPTH):
        for nm in ("ln_ffn1", "ln_mix", "ln_ffn2", "ln_ple"):
            off[(nm, i)] = n
            n += 8
    for j in range(2):
        off[("mla_q_a_norm", j)] = n; n += 3
        off[("mla_kv_a_norm", j)] = n; n += 2
        off[("mla_q_norm", j)] = n; n += 1
        off[("mla_k_norm", j)] = n; n += 1
        off[("gdn_conv_w", j)] = n; n += 48
        off[("gdn_out_norm", j)] = n; n += 1
        off[("dsa_q_norm", j)] = n; n += 1
        off[("dsa_k_norm", j)] = n; n += 1
        off[("dsa_kidx_norm", j)] = n; n += 1
    return off, n


VOFF, NV = _vec_layout()


def pack_vecs(inp):
    v = np.zeros((128, NV), np.float32)
    for i in range(DEPTH):
        for nm in ("ln_ffn1", "ln_mix", "ln_ffn2", "ln_ple"):
            o = VOFF[(nm, i)]
            v[:, o:o + 8] = np.asarray(inp[nm][i]).reshape(8, 128).T
    for j in range(2):
        o = VOFF[("mla_q_a_norm", j)]; v[:, o:o + 3] = np.asarray(inp["mla_q_a_norm"][j]).reshape(3, 128).T
        o = VOFF[("mla_kv_a_norm", j)]; v[:, o:o + 2] = np.asarray(inp["mla_kv_a_norm"][j]).reshape(2, 128).T
        o = VOFF[("mla_q_norm", j)]; v[:96, o] = np.asarray(inp["mla_q_norm"][j])
        o = VOFF[("mla_k_norm", j)]; v[:96, o] = np.asarray(inp["mla_k_norm"][j])
        o = VOFF[("gdn_conv_w", j)]
        cw = np.asarray(inp["gdn_conv_w"][j])
        v[:, o:o + 48] = cw.reshape(4, 12, 128).transpose(2, 1, 0).reshape(128, 48)
        o = VOFF[("gdn_out_norm", j)]; v[:, o] = np.asarray(inp["gdn_out_norm"][j])
        o = VOFF[("dsa_q_norm", j)]; v[:, o] = np.asarray(inp["dsa_q_norm"][j])
        o = VOFF[("dsa_k_norm", j)]; v[:, o] = np.asarray(inp["dsa_k_norm"][j])
        o = VOFF[("dsa_kidx_norm", j)]; v[:64, o] = np.asarray(inp["dsa_kidx_norm"][j]); v[64:, o] = np.asarray(inp["dsa_kidx_norm"][j])
    return v


C_IDENT = 0
C_ONES = 128
C_NEG = 256
C_PM = 384
C_CM = 512
C_MS = 2560
C_MI = 2688
C_UI = 2816
C_CIND = 2944
NC_CONST = 2946
NEGBIG = -1.0e30


def make_consts():
    c = np.zeros((128, NC_CONST), np.float32)
    c[:, C_IDENT:C_IDENT + 128] = np.eye(128, dtype=np.float32)
    c[:, C_ONES:C_ONES + 128] = 1.0
    c[:, C_NEG:C_NEG + 128] = np.triu(np.full((128, 128), NEGBIG, np.float32), 1)
    for m in range(64, 80):
        c[m + 16, C_PM + m] = 1.0
        c[m, C_PM + m + 16] = 1.0
    a_ = np.arange(128)
    same = (a_[:, None] // 64) == (a_[None, :] // 64)
    c[:, C_MS:C_MS + 128] = (same & (a_[None, :] < a_[:, None])).astype(np.float32)
    c[:, C_MI:C_MI + 128] = (same & (a_[None, :] <= a_[:, None])).astype(np.float32)
    c[:, C_UI:C_UI + 128] = (same & (a_[:, None] <= a_[None, :])).astype(np.float32)
    c[:64, C_CIND] = 1.0
    c[64:, C_CIND + 1] = 1.0
    kk = np.arange(128)[:, None]
    qq = np.arange(512)[None, :]
    for off in range(4):
        c[:, C_CM + off * 512:C_CM + (off + 1) * 512] = (off * 128 + kk <= qq).astype(np.float32)
    return c


def make_rope():
    half = 16
    inv_freq = 10000.0 ** (-np.arange(half, dtype=np.float64) / half)
    ang = np.arange(S, dtype=np.float64)[None, :] * inv_freq[:, None]
    cos = np.ones((96, S), np.float64)
    sin = np.zeros((96, S), np.float64)
    cos[64:80] = np.cos(ang); cos[80:96] = np.cos(ang)
    sin[64:80] = -np.sin(ang); sin[80:96] = np.sin(ang)
    return np.stack([cos, sin]).astype(np.float32)


WEIGHT_SHAPES = {
    "ffn1_w_gu": (4, D, 2 * DFF), "ffn1_w_down": (4, DFF, D),
    "ffn2_w_gu": (4, D, 2 * DFF), "ffn2_w_down": (4, DFF, D),
    "even_w_in": (2, D, 2728), "mla_w_q_up": (2, 384, 768), "mla_w_kv_up": (2, 256, 1024),
    "even_w_out": (2, 1024, D), "odd_w_in": (2, D, 2120), "odd_w_out": (2, 1024, D),
    "ple_w_in": (4, PLE, D), "ple_w_gate": (4, D, D),
    "gdn_a_log": (2, 4), "gdn_dt_bias": (2, 4),
}


class Prog:
    def __init__(self, phases):
        self.nc = nc = bass.Bass("TRN2", target_bir_lowering=False)
        self.f = Fw(nc)
        self.phases = phases
        self.d = {}
        self.d["xT"] = nc.dram_tensor("xT", [D, S], F32, kind="ExternalInput").ap()
        self.d["pT"] = nc.dram_tensor("pT", [DEPTH, PLE, S], F32, kind="ExternalInput").ap()
        self.d["vecs"] = nc.dram_tensor("vecs", [128, NV], F32, kind="ExternalInput").ap()
        self.d["consts"] = nc.dram_tensor("consts", [128, NC_CONST], F32, kind="ExternalInput").ap()
        self.d["rope"] = nc.dram_tensor("rope", [2, 96, S], F32, kind="ExternalInput").ap()
        for k, shp in WEIGHT_SHAPES.items():
            self.d[k] = nc.dram_tensor(k, list(shp), F32, kind="ExternalInput").ap()
        self.xres = nc.dram_tensor("outT", [D, S], F32, kind="ExternalOutput").ap()
        self.scr = {}
        for nm, shp, dt in (("dsa_qT", [8, 128, S], BF16), ("dsa_kT", [2, 128, S], BF16), ("dsa_v", [S, 256], BF16),
                            ("dsa_qiT", [4, 128, S], BF16), ("dsa_kiT", [128, S], BF16), ("dsa_w", [S, 8], F32),
                            ("mla_qT", [8, 96, S], BF16), ("mla_kT", [8, 96, S], BF16), ("mla_vext", [8, S, 128], BF16),
                            ("gdn_qT", [4, 128, S], F32), ("gdn_kT", [4, 128, S], F32), ("gdn_vT", [4, 128, S], F32),
                            ("gdn_zs", [4, 128, S], F32), ("gdn_gb", [S, 8], F32), ("even_oT", [D, S], BF16)):
            self.scr[nm] = nc.dram_tensor(nm, shp, dt).ap()
        self.scr_k = {nm: [Tok() for _ in range(32)] for nm in self.scr}
        self.xsrc = self.d["xT"]
        self.xtok = [[Tok() for _ in range(8)] for _ in range(NT)]
        self.pref = None
        self.gst = ExitStack()
        self.pst = None
        self._n = 0

    def _name(self, p):
        self._n += 1
        return "%s_%d" % (p, self._n)

    def gsb(self, shape, dt, name="g"):
        return self.gst.enter_context(self.nc.sbuf_tensor(self._name(name), list(shape), dt))

    def sb(self, shape, dt, name="t"):
        return self.pst.enter_context(self.nc.sbuf_tensor(self._name(name), list(shape), dt))

    def ring(self, n, shape, dt, name="r"):
        small = int(np.prod(shape[1:])) < 64
        return Ring([(self.sb(shape, dt, name), Tok(hard=small)) for _ in range(n)])

    def mm(self, out, lhsT, rhs, start, stop, reads, writes):
        self.f.op("pe", lambda e: e.matmul(out, lhsT=lhsT, rhs=rhs, start=start, stop=stop), reads, writes)

    def tr(self, out, in_, ident, reads, writes):
        self.f.op("pe", lambda e: e.transpose(out, in_, ident), reads, writes)

    def act(self, out, in_, func, reads, writes, scale=None, bias=None):
        kw = {}
        if scale is not None:
            kw["scale"] = scale
        if bias is not None:
            kw["bias"] = bias
        self.f.op("act", lambda e: e.activation(out=out, in_=in_, func=func, **kw), reads, writes)

    def tt(self, eng, out, in0, in1, op, reads, writes):
        self.f.op(eng, lambda e: e.tensor_tensor(out=out, in0=in0, in1=in1, op=op), reads, writes)

    def ts(self, eng, out, in0, s1, op0, reads, writes, s2=None, op1=None):
        if op1 is None:
            self.f.op(eng, lambda e: e.tensor_scalar(out=out, in0=in0, scalar1=s1, scalar2=None, op0=op0), reads, writes)
        else:
            self.f.op(eng, lambda e: e.tensor_scalar(out=out, in0=in0, scalar1=s1, scalar2=s2, op0=op0, op1=op1), reads, writes)

    def stt(self, out, in0, scalar, in1, op0, op1, reads, writes):
        self.f.op("dve", lambda e: e.scalar_tensor_tensor(out=out, in0=in0, scalar=scalar, in1=in1, op0=op0, op1=op1), reads, writes)

    def cp(self, eng, out, in_, reads, writes):
        if eng == "act":
            self.f.op("act", lambda e: e.activation(out=out, in_=in_, func=AF.Copy), reads, writes)
        else:
            self.f.op(eng, lambda e: e.tensor_copy(out=out, in_=in_), reads, writes)

    def recip(self, out, in_, reads, writes):
        self.f.op("dve", lambda e: e.reciprocal(out=out, in_=in_), reads, writes)

    def dma(self, out, in_, reads, writes, q="sp"):
        self.f.dma(q, out, in_, reads, writes)

    def setup(self):
        nc = self.nc
        self.psum = []
        for i in range(8):
            t = self.gst.enter_context(nc.psum_tensor("ps%d" % i, [128, 512], F32))
            self.psum.append((t, Tok(excl=True)))
        self.vec = self.gsb([128, NV], F32, "vec")
        self.vec_k = Tok()
        self.dma(self.vec[:], self.d["vecs"], [], [self.vec_k])
        self.cst = self.gsb([128, 256], F32, "cst")
        self.cst_k = Tok()
        self.dma(self.cst[:], self.d["consts"][:, 0:256], [], [self.cst_k])
        self.cb = self.gsb([128, 256], BF16, "cstb")
        self.cb_k = Tok()
        self.cp("dve", self.cb[:], self.cst[:], [self.cst_k], [self.cb_k])
        self.ident_f = self.cst[:, C_IDENT:C_IDENT + 128]
        self.ones_f = self.cst[:, C_ONES:C_ONES + 128]
        self.ident_b = self.cb[:, C_IDENT:C_IDENT + 128]
        self.ones_b = self.cb[:, C_ONES:C_ONES + 128]

    def vcol(self, key, c=0, rows=128):
        o = VOFF[key] + c
        return self.vec[0:rows, o:o + 1]

    def lconst(self, c0, n, bf16=False):
        t = self.sb([128, n], F32, "lc")
        k = Tok()
        self.dma(t[:], self.d["consts"][:, c0:c0 + n], [], [k])
        if not bf16:
            return t, k
        tb = self.sb([128, n], BF16, "lcb")
        kb = Tok()
        self.cp("pool", tb[:], t[:], [k], [kb])
        return tb, kb

    def begin_phase(self):
        self.pst = ExitStack()

    def end_phase(self):
        self.f.barrier()
        self.pst.close()
        self.pst = None

    def load_norm(self, tt, xstage, sqring, xb, xb_k, rstd, rstd_k, ssb):
        ss, ss_k = ssb
        for c in range(8):
            xs, xs_k = xstage.next()
            self.dma(xs[:], self.xsrc[c * 128:(c + 1) * 128, tt * T:(tt + 1) * T], [self.xtok[tt][c]], [xs_k])
            sq, sq_k = sqring.next()
            self.act(sq[:], xs[:], AF.Square, [xs_k], [sq_k])
            self.cp("pool", xb[:, c, :], xs[:], [xs_k], [xb_k])
            self.mm(ss[:], self.ones_b, sq[:], c == 0, c == 7, [sq_k, self.cb_k], [ss_k])
        self.act(rstd[:], ss[:], AF.Sqrt, [ss_k], [rstd_k], scale=1.0 / D, bias=EPS)
        self.recip(rstd[:], rstd[:], [rstd_k], [rstd_k])

    def load_w(self, dst, dst_k, src, stage, gain, eng_i):
        st, st_k = stage.next()
        n = src.shape[-1]
        self.dma(st[:, 0:n], src, [], [st_k])
        eng = ("dve", "pool", "act")[eng_i % 3] if gain is None else ("dve", "act")[eng_i % 2]
        rd = [st_k, self.vec_k]
        if gain is None:
            self.cp(eng, dst, st[:, 0:n], rd, [dst_k])
        elif eng == "act":
            self.act(dst, st[:, 0:n], AF.Copy, rd, [dst_k], scale=gain)
        else:
            self.ts(eng, dst, st[:, 0:n], gain, ALU.mult, rd, [dst_k])


    def ffn_weights_begin(self, i, which):
        st = ExitStack()
        nc = self.nc
        wgu = st.enter_context(nc.sbuf_tensor(self._name("wgu"), [128, 8, 2 * DFF], BF16))
        wd = st.enter_context(nc.sbuf_tensor(self._name("wd"), [128, NJ, D], BF16))
        stage = Ring([(st.enter_context(nc.sbuf_tensor(self._name("wst"), [128, 1408], F32)), Tok()) for _ in range(2)])
        wgu_k = [[Tok() for _ in range(4)] for _ in range(8)]
        wd_k = [Tok() for _ in range(NJ)]
        Wgu = self.d["ffn%d_w_gu" % which][i]
        Wd = self.d["ffn%d_w_down" % which][i]
        gkey = ("ln_ffn%d" % which, i)
        tasks = []
        n = 0
        for c in range(8):
            for pc in range(4):
                tasks.append((wgu[:, c, pc * 1408:(pc + 1) * 1408], wgu_k[c][pc], Wgu[c * 128:(c + 1) * 128, pc * 1408:(pc + 1) * 1408], self.vcol(gkey, c), n))
                n += 1
        for j in range(NJ):
            tasks.append((wd[:, j, :], wd_k[j], Wd[j * 128:(j + 1) * 128, :], None, n))
            n += 1
        self.pref = {"key": (i, which), "st": st, "wgu": wgu, "wd": wd, "wgu_k": wgu_k, "wd_k": wd_k, "stage": stage, "tasks": tasks}

    def pref_step(self, n=None):
        if not self.pref:
            return
        tasks = self.pref["tasks"]
        k = len(tasks) if n is None else min(n, len(tasks))
        for _ in range(k):
            dst, dst_k, src, gain, idx = tasks.pop(0)
            self.load_w(dst, dst_k, src, self.pref["stage"], gain, idx)

    def next_ffn(self, i, which):
        return (i, which)

    def phase_ffn(self, i, which):
        if not (self.pref and self.pref["key"] == (i, which)):
            assert not self.pref
            self.ffn_weights_begin(i, which)
        self.begin_phase()
        pf = self.pref
        wgu, wgu_k, wd, wd_k = pf["wgu"], pf["wgu_k"], pf["wd"], pf["wd_k"]
        xstage = self.ring(4, [128, T], F32, "xst")
        sqring = self.ring(2, [128, T], BF16, "sq")
        xbs = [(self.sb([128, 8, T], BF16, "xb"), Tok())] * 2
        rstds = [(self.sb([128, T], F32, "rstd"), Tok())] * 2
        actb = self.sb([128, NJ, T], BF16, "actb")
        act_k = [Tok() for _ in range(NJ)]
        aring = self.ring(3, [128, T], F32, "A")
        oring = self.ring(3, [128, T], F32, "ob")
        ssb = self.psum[0]
        gps = [self.psum[1], self.psum[2]]
        ups = [self.psum[3], self.psum[4]]
        yps = [self.psum[5], self.psum[6]]
        self.pref_step()
        self.load_norm(0, xstage, sqring, xbs[0][0], xbs[0][1], rstds[0][0], rstds[0][1], ssb)
        for tt in range(NT):
            xb, xb_k = xbs[tt % 2]
            rstd, rstd_k = rstds[tt % 2]
            for j in range(NJ):
                gp, gp_k = gps[j % 2]
                up, up_k = ups[j % 2]
                for half, (pp, pp_k) in enumerate(((gp, gp_k), (up, up_k))):
                    col = half * DFF + j * 128
                    pc = col // 1408
                    assert (col + 127) // 1408 == pc
                    for c in range(8):
                        self.mm(pp[:], wgu[:, c, col:col + 128], xb[:, c, :], c == 0, c == 7,
                                [wgu_k[c][pc], xb_k], [pp_k])
                A, A_k = aring.next()
                self.tt("dve", A[:], gp[:], rstd[:], ALU.mult, [gp_k, rstd_k], [A_k])
                self.act(A[:], A[:], AF.Silu, [A_k], [A_k])
                self.tt("pool", A[:], A[:], rstd[:], ALU.mult, [A_k, rstd_k], [A_k])
                self.tt("dve", actb[:, j, :], up[:], A[:], ALU.mult, [up_k, A_k], [act_k[j]])
            if tt + 1 < NT:
                nb = xbs[(tt + 1) % 2]
                nr = rstds[(tt + 1) % 2]
                self.load_norm(tt + 1, xstage, sqring, nb[0], nb[1], nr[0], nr[1], ssb)
            for m in range(8):
                yp, yp_k = yps[m % 2]
                for j in range(NJ):
                    self.mm(yp[:], wd[:, j, m * 128:(m + 1) * 128], actb[:, j, :], j == 0, j == NJ - 1,
                            [wd_k[j], act_k[j]], [yp_k])
                xs, xs_k = xstage.next()
                self.dma(xs[:], self.xsrc[m * 128:(m + 1) * 128, tt * T:(tt + 1) * T], [self.xtok[tt][m]], [xs_k])
                ob, ob_k = oring.next()
                self.stt(ob[:], yp[:], 0.5, xs[:], ALU.mult, ALU.add, [yp_k, xs_k], [ob_k])
                self.dma(self.xres[m * 128:(m + 1) * 128, tt * T:(tt + 1) * T], ob[:], [ob_k], [self.xtok[tt][m]], q="act")
        self.end_phase()
        self.pref["st"].close()
        self.pref = None
        self.xsrc = self.xres

    def phase_ple(self, i):
        if i + 1 < DEPTH and ("ffn", i + 1, 1) in self.phases:
            self.ffn_weights_begin(i + 1, 1)
        self.begin_phase()
        Wg = self.d["ple_w_gate"][i]
        Wp = self.d["ple_w_in"][i]
        gkey = ("ln_ple", i)
        wg = self.sb([128, 8, D], BF16, "wg")
        wg_k = [Tok() for _ in range(8)]
        wp = self.sb([128, 2, D], BF16, "wp")
        wp_k = [Tok() for _ in range(2)]
        wstage = self.pref["stage"] if self.pref else self.ring(2, [128, D], F32, "wst")
        xstage = self.ring(3, [128, T], F32, "xst")
        sqring = self.ring(2, [128, T], BF16, "sq")
        xbs = [(self.sb([128, 8, T], BF16, "xb"), Tok()) for _ in range(2)]
        rstds = [(self.sb([128, T], F32, "rstd"), Tok()) for _ in range(2)]
        pstage = self.ring(1, [128, T], F32, "pst")
        pbs = [(self.sb([128, 2, T], BF16, "pb"), Tok()) for _ in range(2)]
        aring = self.ring(2, [128, T], F32, "A")
        oring = self.ring(2, [128, T], F32, "ob")
        ssb = self.psum[0]
        gps = [self.psum[1], self.psum[2]]
        eps_ = [self.psum[3], self.psum[4]]
        for c in range(8):
            self.load_w(wg[:, c, :], wg_k[c], Wg[c * 128:(c + 1) * 128, :], wstage, self.vcol(gkey, c), c)
        for c in range(2):
            self.load_w(wp[:, c, :], wp_k[c], Wp[c * 128:(c + 1) * 128, :], wstage, None, c)

        def load_p(tt):
            pb, pb_k = pbs[tt % 2]
            for c in range(2):
                st, st_k = pstage.next()
                self.dma(st[:], self.d["pT"][i, c * 128:(c + 1) * 128, tt * T:(tt + 1) * T], [], [st_k])
                self.cp("pool", pb[:, c, :], st[:], [st_k], [pb_k])

        pend_st = []
        self.load_norm(0, xstage, sqring, xbs[0][0], xbs[0][1], rstds[0][0], rstds[0][1], ssb)
        load_p(0)
        for tt in range(NT):
            xb, xb_k = xbs[tt % 2]
            rstd, rstd_k = rstds[tt % 2]
            pb, pb_k = pbs[tt % 2]
            if tt + 1 < NT:
                nb = xbs[(tt + 1) % 2]
                nr = rstds[(tt + 1) % 2]
                self.load_norm(tt + 1, xstage, sqring, nb[0], nb[1], nr[0], nr[1], ssb)
                load_p(tt + 1)
            for m in range(8):
                gp, gp_k = gps[m % 2]
                ep, ep_k = eps_[m % 2]
                for c in range(8):
                    self.mm(gp[:], wg[:, c, m * 128:(m + 1) * 128], xb[:, c, :], c == 0, c == 7, [wg_k[c], xb_k], [gp_k])
                for c in range(2):
                    self.mm(ep[:], wp[:, c, m * 128:(m + 1) * 128], pb[:, c, :], c == 0, c == 1, [wp_k[c], pb_k], [ep_k])
                A, A_k = aring.next()
                self.tt("dve", A[:], gp[:], rstd[:], ALU.mult, [gp_k, rstd_k], [A_k])
                self.act(A[:], A[:], AF.Sigmoid, [A_k], [A_k])
                self.tt("dve", A[:], ep[:], A[:], ALU.mult, [ep_k, A_k], [A_k])
                xs, xs_k = xstage.next()
                self.dma(xs[:], self.xsrc[m * 128:(m + 1) * 128, tt * T:(tt + 1) * T], [self.xtok[tt][m]], [xs_k])
                ob, ob_k = oring.next()
                self.tt("pool", ob[:], A[:], xs[:], ALU.add, [A_k, xs_k], [ob_k])
                pend_st.append((self.xres[m * 128:(m + 1) * 128, tt * T:(tt + 1) * T], ob, ob_k, self.xtok[tt][m]))
                if len(pend_st) > 1:
                    d_, ob_, obk_, xk_ = pend_st.pop(0)
                    self.dma(d_, ob_[:], [obk_], [xk_], q="act")
            self.pref_step(7)
        while pend_st:
            d_, ob_, obk_, xk_ = pend_st.pop(0)
            self.dma(d_, ob_[:], [obk_], [xk_], q="act")
        self.pref_step()
        self.end_phase()
        self.xsrc = self.xres


    def load_hn(self, tt, xstage, sqring, xb, xb_k, rstd, rstd_k, ssb, hn, hn_k):
        self.load_norm(tt, xstage, sqring, xb, xb_k, rstd, rstd_k, ssb)
        for c in range(8):
            self.tt(("pool", "dve")[c % 2], hn[:, c, :], xb[:, c, :], rstd[:], ALU.mult, [xb_k, rstd_k], [hn_k])

    def head_norm(self, src, src_k, rows, gaincol, div, sqring, ssring, rring, dst, dst_k):
        sq, sq_k = sqring.next()
        self.act(sq[0:rows, :], src, AF.Square, [src_k], [sq_k])
        ss, ss_k = ssring.next()
        self.mm(ss[0:rows, :], self.ones_b[0:rows, 0:rows], sq[0:rows, :], True, True, [sq_k, self.cb_k], [ss_k])
        r, r_k = rring.next()
        self.act(r[0:rows, :], ss[0:rows, :], AF.Sqrt, [ss_k], [r_k], scale=1.0 / div, bias=EPS)
        self.recip(r[0:rows, :], r[0:rows, :], [r_k], [r_k])
        self.stt(dst, src, gaincol, r[0:rows, :], ALU.mult, ALU.mult, [src_k, r_k, self.vec_k], [dst_k])

    def phase_odd_proj(self, i):
        j = i // 2
        self.begin_phase()
        W = self.d["odd_w_in"][j]
        NW = 2120
        win = self.sb([128, 8, NW + 128], BF16, "win")
        win_k = [Tok() for _ in range(8)]
        wstage = self.ring(2, [128, NW], F32, "wst")
        xstage = self.ring(4, [128, T], F32, "xst")
        sqring = self.ring(2, [128, T], BF16, "sq")
        xb, xb_k = self.sb([128, 8, T], BF16, "xb"), Tok()
        rstd, rstd_k = self.sb([128, T], F32, "rstd"), Tok()
        hns = [(self.sb([128, 8, T], BF16, "hn"), Tok()) for _ in range(2)]
        sq2 = self.ring(2, [128, T], BF16, "sq2")
        rring = self.ring(2, [128, T], F32, "rr")
        oring = self.ring(4, [128, T], BF16, "ob")
        wring = self.ring(2, [128, 8], F32, "wb")
        ssb = self.psum[0]
        pring = Ring([self.psum[1], self.psum[2], self.psum[3]])
        ssring = Ring([self.psum[4], self.psum[5]])
        sring = Ring([self.psum[6], self.psum[7]])
        for c in range(8):
            self.load_w(win[:, c, 0:NW], win_k[c], W[c * 128:(c + 1) * 128, :], wstage, self.vcol(("ln_mix", i), c), c)
            self.cp("pool", win[:, c, NW:NW + 64], win[:, c, 2048:2112], [win_k[c]], [win_k[c]])
            self.cp("pool", win[:, c, NW + 64:NW + 128], win[:, c, 2048:2112], [win_k[c]], [win_k[c]])
        sc = self.scr
        sk = self.scr_k
        self.load_hn(0, xstage, sqring, xb, xb_k, rstd, rstd_k, ssb, hns[0][0], hns[0][1])
        for tt in range(NT):
            hn, hn_k = hns[tt % 2]
            if tt + 1 < NT:
                self.load_hn(tt + 1, xstage, sqring, xb, xb_k, rstd, rstd_k, ssb, hns[(tt + 1) % 2][0], hns[(tt + 1) % 2][1])
            tsl = slice(tt * T, (tt + 1) * T)

            def proj(col, width=128):
                pp, pp_k = pring.next()
                for c in range(8):
                    self.mm(pp[0:width, :], win[:, c, col:col + width], hn[:, c, :], c == 0, c == 7, [win_k[c], hn_k], [pp_k])
                return pp, pp_k
            for h in range(8):
                pp, pp_k = proj(h * 128)
                ob, ob_k = oring.next()
                self.head_norm(pp[:], pp_k, 128, self.vcol(("dsa_q_norm", j)), 128.0, sq2, ssring, rring, ob[:], ob_k)
                self.dma(sc["dsa_qT"][h, :, tsl], ob[:], [ob_k], [sk["dsa_qT"][tt]])
            for n in range(2):
                pp, pp_k = proj(1024 + n * 128)
                ob, ob_k = oring.next()
                self.head_norm(pp[:], pp_k, 128, self.vcol(("dsa_k_norm", j)), 128.0, sq2, ssring, rring, ob[:], ob_k)
                self.dma(sc["dsa_kT"][n, :, tsl], ob[:], [ob_k], [sk["dsa_kT"][tt]])
            pp, pp_k = proj(NW)
            ob, ob_k = oring.next()
            self.head_norm(pp[:], pp_k, 128, self.vcol(("dsa_kidx_norm", j)), 128.0, sq2, ssring, rring, ob[:], ob_k)
            self.dma(sc["dsa_kiT"][:, tsl], ob[:], [ob_k], [sk["dsa_kiT"][tt]])
            for c4 in range(4):
                pp, pp_k = proj(1536 + c4 * 128)
                ob, ob_k = oring.next()
                self.cp("act", ob[:], pp[:], [pp_k], [ob_k])
                self.dma(sc["dsa_qiT"][c4, :, tsl], ob[:], [ob_k], [sk["dsa_qiT"][tt]])
            for sub in range(4):
                sp_, sp_k = sring.next()
                for c in range(8):
                    self.mm(sp_[:, 0:256], hn[:, c, sub * 128:(sub + 1) * 128], win[:, c, 1280:1536], c == 0, c == 7, [win_k[c], hn_k], [sp_k])
                ob, ob_k = oring.next()
                self.cp("act", ob[:, 0:256], sp_[:, 0:256], [sp_k], [ob_k])
                r0 = tt * T + sub * 128
                self.dma(sc["dsa_v"][r0:r0 + 128, :], ob[:, 0:256], [ob_k], [sk["dsa_v"][tt]])
                sp_, sp_k = sring.next()
                for c in range(8):
                    self.mm(sp_[:, 0:8], hn[:, c, sub * 128:(sub + 1) * 128], win[:, c, 2112:2120], c == 0, c == 7, [win_k[c], hn_k], [sp_k])
                wb, wb_k = wring.next()
                self.cp("dve", wb[:], sp_[:, 0:8], [sp_k], [wb_k])
                self.dma(sc["dsa_w"][r0:r0 + 128, :], wb[:], [wb_k], [sk["dsa_w"][tt]])
        self.end_phase()

    def phase_dsa(self, i, nq=32):
        j = i // 2
        self.begin_phase()
        sc = self.scr
        REPL = 2.0 * NEGBIG
        kT, kT_k = self.sb([128, 2, S], BF16, "kT"), Tok()
        V, V_k = self.sb([128, 32, 256], BF16, "V"), Tok()
        kiT, kiT_k = self.sb([128, S], BF16, "kiT"), Tok()
        for n in range(2):
            self.dma(kT[:, n, :], sc["dsa_kT"][n], [], [kT_k])
        for kq in range(4):
            self.dma(V[:, kq * 8:(kq + 1) * 8, :], sc["dsa_v"][kq * 1024:(kq + 1) * 1024, :].rearrange("(kt p) d -> p kt d", p=128), [], [V_k])
        self.dma(kiT[:], sc["dsa_kiT"], [], [kiT_k])
        scbs = [(self.sb([128, S], F32, "scb"), [Tok(hard=True) for _ in range(8)]) for _ in range(4)]
        mbs = [(self.sb([128, S], BF16, "mb"), Tok()) for _ in range(4)]
        bss = [(self.sb([128, 8], F32, "bs"), Tok(hard=True)) for _ in range(4)]
        qring = self.ring(4, [128, 8, 128], BF16, "q")
        qiring = self.ring(4, [128, 4, 128], BF16, "qi")
        wring = Ring([(self.sb([128, 8], F32, "w"), Tok()) for _ in range(4)])
        mTring = self.ring(2, [128, 32, 128], BF16, "mT")
        rring = self.ring(3, [128, T], F32, "relu")
        junk, junk_k = self.sb([128, S], BF16, "junk"), Tok()
        ering = self.ring(4, [128, T], BF16, "e")
        pring = self.ring(4, [128, T], BF16, "pT")
        o32ring = self.ring(1, [128, T], F32, "o32")
        rdring = self.ring(1, [128, T], F32, "rd")
        oTring = self.ring(2, [128, 8, 128], BF16, "oT")
        ps_sc = Ring([self.psum[0], self.psum[1]])
        ps_st = Ring([self.psum[2], self.psum[3], self.psum[7]])
        ps_o = self.psum[4]
        ps_d = self.psum[5]
        ps_t = self.psum[6]
        ps_tb = ps_t[0][:].bitcast(BF16)
        negt, neg_k = self.lconst(C_NEG, 128)
        neg = negt[:, :]
        scale = 128.0 ** -0.5
        oT_d = sc["even_oT"].rearrange("(c p) t -> p c t", p=128)
        qbuf = {}

        def s1(qs):
            info = {}
            for qi in qs:
                L = (qi + 1) * 128
                qsl = slice(qi * 128, L)
                qi_sb, qi_k = qiring.next()
                self.dma(qi_sb[:], sc["dsa_qiT"][:, :, qsl].rearrange("h p q -> p h q"), [], [qi_k])
                w_sb, w_k = wring.next()
                self.dma(w_sb[:], sc["dsa_w"][qsl, :], [], [w_k])
                info[qi] = (L, qsl, qi_sb, qi_k, w_sb, w_k)
            for h in range(8):
                pb = (h % 2) * 64
                for qi in qs:
                    L, qsl, qi_sb, qi_k, w_sb, w_k = info[qi]
                    scb, sc_ks = scbs[qi % 4]
                    for st in range((L + 511) // 512):
                        w_ = min(512, L - st * 512)
                        seg = slice(st * 512, st * 512 + w_)
                        ps, ps_k = ps_sc.next()
                        self.mm(ps[:, 0:w_], qi_sb[pb:pb + 64, h // 2, :], kiT[pb:pb + 64, seg], True, True, [qi_k, kiT_k], [ps_k])
                        r, r_k = rring.next()
                        self.act(r[:, 0:w_], ps[:, 0:w_], AF.Relu, [ps_k], [r_k])
                        if h == 0:
                            self.ts("dve", scb[:, seg], r[:, 0:w_], w_sb[:, 0:1], ALU.mult, [r_k, w_k], [sc_ks[st]])
                        else:
                            self.stt(scb[:, seg], r[:, 0:w_], w_sb[:, h:h + 1], scb[:, seg], ALU.mult, ALU.add, [r_k, w_k, sc_ks[st]], [sc_ks[st]])
            for qi in qs:
                if qi >= 2:
                    bounds(qi)
            for qi in qs:
                L, qsl = info[qi][0], info[qi][1]
                scb, sc_ks = scbs[qi % 4]
                self.tt("pool", scb[:, qsl], scb[:, qsl], neg, ALU.add, [sc_ks[qi // 4], neg_k], [sc_ks[qi // 4]])

        def loadq(qs):
            for qi in qs:
                qsl = slice(qi * 128, (qi + 1) * 128)
                q_sb, q_k = qring.next()
                self.dma(q_sb[:], sc["dsa_qT"][:, :, qsl].rearrange("h p q -> p h q"), [], [q_k])
                qbuf[qi] = (q_sb, q_k)

        K_IT = 16

        def bounds(qi):
            L = (qi + 1) * 128
            scb, sc_ks = scbs[qi % 4]
            mb, mb_k = mbs[qi % 4]
            bs, bs_k = bss[qi % 4]
            self.f.op("dve", lambda e: e.tensor_scalar(out=junk[:, 0:L], in0=scb[:, 0:L], scalar1=0.0, scalar2=-3.0e38, op0=ALU.add, op1=ALU.max,
                                                        accum_out=bs[:, 1:2]), sc_ks, [junk_k, bs_k])
            self.f.op("dve", lambda e: e.tensor_scalar(out=junk[:, 0:L], in0=scb[:, 0:L], scalar1=0.0, scalar2=3.0e38, op0=ALU.add, op1=ALU.min,
                                                        accum_out=bs[:, 0:1]), sc_ks, [junk_k, bs_k])
            self.tt("dve", bs[:, 2:3], bs[:, 1:2], bs[:, 0:1], ALU.subtract, [bs_k], [bs_k])
            self.ts("dve", bs[:, 2:3], bs[:, 2:3], 0.5, ALU.mult, [bs_k], [bs_k])

        def topk(qs):
            qs = [q_ for q_ in qs if q_ >= 2]
            for it in range(K_IT):
                for q_ in qs:
                    L = (q_ + 1) * 128
                    scb, sc_ks = scbs[q_ % 4]
                    mb, mb_k = mbs[q_ % 4]
                    bs, bs_k = bss[q_ % 4]
                    self.tt("dve", bs[:, 3:4], bs[:, 0:1], bs[:, 2:3], ALU.add, [bs_k], [bs_k])
                    self.f.op("dve", lambda e, L=L, scb=scb, mb=mb, bs=bs: e.tensor_scalar(out=mb[:, 0:L], in0=scb[:, 0:L], scalar1=bs[:, 3:4], scalar2=0.0,
                                                                                          op0=ALU.is_ge, op1=ALU.add, accum_out=bs[:, 4:5]),
                              sc_ks + [bs_k], [mb_k, bs_k])
                for q_ in qs:
                    bs, bs_k = bss[q_ % 4]
                    self.stt(bs[:, 5:6], bs[:, 4:5], 256.0, bs[:, 2:3], ALU.is_ge, ALU.mult, [bs_k], [bs_k])
                    self.tt("dve", bs[:, 0:1], bs[:, 0:1], bs[:, 5:6], ALU.add, [bs_k], [bs_k])
                    self.ts("dve", bs[:, 2:3], bs[:, 2:3], 0.5, ALU.mult, [bs_k], [bs_k])

        def mask(qi):
            L = (qi + 1) * 128
            scb, sc_ks = scbs[qi % 4]
            mb, mb_k = mbs[qi % 4]
            bs, bs_k = bss[qi % 4]
            if qi >= 2:
                self.ts("dve", mb[:, 0:L], scb[:, 0:L], bs[:, 0:1], ALU.is_ge, sc_ks + [bs_k], [mb_k])
            else:
                self.ts("dve", mb[:, 0:L], scb[:, 0:L], 0.1 * NEGBIG, ALU.is_ge, sc_ks, [mb_k])

        def s3(qi):
            L = (qi + 1) * 128
            qsl = slice(qi * 128, L)
            q_sb, q_k = qbuf.pop(qi)
            mb, mb_k = mbs[qi % 4]
            mT, mT_k = mTring.next()
            for k0 in range(0, qi + 1, 4):
                nk = min(4, qi + 1 - k0)
                for kk in range(nk):
                    kt = k0 + kk
                    self.tr(ps_tb[:, kk * 128:(kk + 1) * 128], mb[:, kt * 128:(kt + 1) * 128], self.ident_b, [mb_k, self.cb_k], [ps_t[1]])
                self.cp("act", mT[:, k0:k0 + nk, :].rearrange("p k q -> p (k q)"), ps_tb[:, 0:nk * 128], [ps_t[1]], [mT_k])
            oT, oT_k = oTring.next()
            for n in range(2):
                pend = []

                def stage_a(kt):
                    ps, ps_k = ps_st.next()
                    self.mm(ps[:], kT[:, n, kt * 128:(kt + 1) * 128], q_sb[:, 4 * n:4 * n + 4, :].rearrange("p h q -> p (h q)"), True, True, [kT_k, q_k], [ps_k])
                    e, e_k = ering.next()
                    self.act(e[:], ps[:], AF.Exp, [ps_k], [e_k], scale=scale)
                    pT, pT_k = pring.next()
                    self.tt("pool", pT[:].rearrange("p (h q) -> p h q", h=4), e[:].rearrange("p (h q) -> p h q", h=4),
                            mT[:, kt:kt + 1, :].to_broadcast([128, 4, 128]), ALU.mult, [e_k, mT_k], [pT_k])
                    pend.append((kt, pT, pT_k))

                def stage_b():
                    kt, pT, pT_k = pend.pop(0)
                    self.mm(ps_o[0][:], V[:, kt, n * 128:(n + 1) * 128], pT[:], kt == 0, kt == qi, [V_k, pT_k], [ps_o[1]])
                    self.mm(ps_d[0][:], self.ones_b, pT[:], kt == 0, kt == qi, [self.cb_k, pT_k], [ps_d[1]])

                for kt in range(qi + 1):
                    stage_a(kt)
                    if len(pend) > 2:
                        stage_b()
                while pend:
                    stage_b()
                rd, rd_k = rdring.next()
                self.act(rd[:], ps_d[0][:], AF.Ln, [ps_d[1]], [rd_k])
                self.act(rd[:], rd[:], AF.Exp, [rd_k], [rd_k], scale=-1.0)
                o32, o32_k = o32ring.next()
                self.cp("act", o32[:], ps_o[0][:], [ps_o[1]], [o32_k])
                self.tt("pool", oT[:, 4 * n:4 * n + 4, :].rearrange("p h q -> p (h q)"), o32[:], rd[:], ALU.mult, [o32_k, rd_k], [oT_k])
            self.dma(oT_d[:, :, qsl], oT[:], [oT_k], [self.scr_k["even_oT"][qi // 4]])

        nquad = (nq + 3) // 4

        def quad(g):
            return [q_ for q_ in range(4 * g, 4 * g + 4) if q_ < nq]

        s1(quad(0))
        for g in range(nquad):
            qs = quad(g)
            loadq(qs)
            topk(qs)
            for q_ in qs:
                mask(q_)
            if g + 1 < nquad:
                s1(quad(g + 1))
            for q_ in qs:
                s3(q_)
        self.end_phase()

    def phase_even_proj(self, i, do_gdn=True):
        j = i // 2
        self.begin_phase()
        W = self.d["even_w_in"][j]
        NW = 2728
        win = self.sb([128, 8, NW], BF16, "win")
        win_k = [Tok() for _ in range(8)]
        wstage = self.ring(2, [128, NW // 2], F32, "wst")
        wq = self.sb([128, 3, 768], BF16, "wq"); wq_k = [Tok() for _ in range(3)]
        wkv = self.sb([128, 2, 1024], BF16, "wkv"); wkv_k = [Tok() for _ in range(2)]
        wv = self.sb([128, 2, 512], BF16, "wv")
        xstage = self.ring(3, [128, T], F32, "xst")
        sqring = self.ring(2, [128, T], BF16, "sq")
        xb, xb_k = self.sb([128, 8, T], BF16, "xb"), Tok()
        rstd, rstd_k = self.sb([128, T], F32, "rstd"), Tok()
        hns = [(self.sb([128, 8, T], BF16, "hn"), Tok()) for _ in range(2)]
        sq2 = self.ring(4, [128, T], BF16, "sq2")
        rring = self.ring(4, [128, T], F32, "rr")
        rawring = self.ring(5, [128, T], F32, "raw")
        cq_raw, cq_k = self.sb([128, 3, T], F32, "cqraw"), Tok()
        cqn, cqn_k = self.sb([128, 3, T], BF16, "cqn"), Tok()
        ckvn, ckvn_k = self.sb([128, 2, T], BF16, "ckvn"), Tok()
        kpe, kpe_k = self.sb([128, T], F32, "kpe"), Tok()
        ropes = [(self.sb([96, 2, T], F32, "rope"), Tok()) for _ in range(2)]
        nb_ring = self.ring(4, [96, T], BF16, "nb")
        t1_ring = self.ring(4, [96, T], F32, "t1")
        fin_ring = self.ring(4, [96, T], BF16, "fin")
        vext_ring = self.ring(2, [128, 8, 128], BF16, "vext")
        f32o = self.ring(4, [128, T], F32, "f32o")
        gbring = self.ring(2, [128, 8], F32, "gb")
        tmp4 = self.ring(2, [128, 4], F32, "tmp4")
        ssb = self.psum[0]
        pring = Ring([self.psum[1], self.psum[2], self.psum[3]])
        ssring = Ring([self.psum[4], self.psum[5], self.psum[6]])
        sring = Ring([self.psum[7]])
        pmt, pm_k = self.lconst(C_PM, 96, True)
        pm_b = pmt[0:96, 0:96]
        for c in range(8):
            for hf in range(2):
                self.load_w(win[:, c, hf * (NW // 2):(hf + 1) * (NW // 2)], win_k[c], W[c * 128:(c + 1) * 128, hf * (NW // 2):(hf + 1) * (NW // 2)], wstage, self.vcol(("ln_mix", i), c), 2 * c + hf)
        for c in range(3):
            self.load_w(wq[:, c, :], wq_k[c], self.d["mla_w_q_up"][j][c * 128:(c + 1) * 128, :], wstage, self.vcol(("mla_q_a_norm", j), c), c)
        for c in range(2):
            self.load_w(wkv[:, c, :], wkv_k[c], self.d["mla_w_kv_up"][j][c * 128:(c + 1) * 128, :], wstage, self.vcol(("mla_kv_a_norm", j), c), c)
            self.cp("pool", wv[:, c, :].rearrange("p (h d) -> p h d", d=64),
                    wkv[:, c, :].rearrange("p (h two d) -> p h two d", two=2, d=64)[:, :, 1, :], [wkv_k[c]], [wkv_k[c]])
        for (vx, vx_k) in vext_ring.items:
            self.f.op("pool", lambda e, vx=vx: e.memset(vx[:], 1.0), [], [vx_k])
        if do_gdn:
            convbuf, conv_k = self.sb([128, 12, 3 + T], F32, "convbuf"), [Tok() for _ in range(12)]
            self.f.op("pool", lambda e: e.memset(convbuf[:, :, 0:3], 0.0), [], conv_k)
            yring = self.ring(5, [128, T], F32, "y")
            negA, negA_k = self.sb([128, 4], F32, "negA"), Tok(hard=True)
            dtb, dtb_k = self.sb([128, 4], F32, "dtb"), Tok()
            self.dma(negA[:], self.d["gdn_a_log"][j:j + 1, :].partition_broadcast(128), [], [negA_k])
            self.dma(dtb[:], self.d["gdn_dt_bias"][j:j + 1, :].partition_broadcast(128), [], [dtb_k])
            self.act(negA[:], negA[:], AF.Exp, [negA_k], [negA_k])
            self.ts("dve", negA[:], negA[:], -1.0, ALU.mult, [negA_k], [negA_k])
        sc = self.scr
        sk = self.scr_k

        def load_rope(tt):
            rp, rp_k = ropes[tt % 2]
            self.dma(rp[:], self.d["rope"][:, :, tt * T:(tt + 1) * T].rearrange("a p t -> p a t"), [], [rp_k])

        self.load_hn(0, xstage, sqring, xb, xb_k, rstd, rstd_k, ssb, hns[0][0], hns[0][1])
        load_rope(0)
        for tt in range(NT):
            hn, hn_k = hns[tt % 2]
            rp, rp_k = ropes[tt % 2]
            if tt + 1 < NT:
                self.load_hn(tt + 1, xstage, sqring, xb, xb_k, rstd, rstd_k, ssb, hns[(tt + 1) % 2][0], hns[(tt + 1) % 2][1])
                load_rope(tt + 1)
            tsl = slice(tt * T, (tt + 1) * T)

            def proj(col, width=128, pbase=0):
                pp, pp_k = pring.next()
                for c in range(8):
                    self.mm(pp[pbase:pbase + width, :], win[:, c, col:col + width], hn[:, c, :], c == 0, c == 7, [win_k[c], hn_k], [pp_k])
                return pp, pp_k

            def latent(col0, nch, raw, raw_k, dst, dst_k):
                ss, ss_k = ssring.next()
                for c in range(nch):
                    pp, pp_k = proj(col0 + c * 128)
                    self.cp("act", raw[:, c, :], pp[:], [pp_k], [raw_k])
                    sq, sq_k = sq2.next()
                    self.act(sq[:], pp[:], AF.Square, [pp_k], [sq_k])
                    self.mm(ss[:], self.ones_b, sq[:], c == 0, c == nch - 1, [sq_k, self.cb_k], [ss_k])
                r, r_k = rring.next()
                self.act(r[:], ss[:], AF.Sqrt, [ss_k], [r_k], scale=1.0 / (nch * 128), bias=EPS)
                self.recip(r[:], r[:], [r_k], [r_k])
                for c in range(nch):
                    self.tt(("dve", "pool")[c % 2], dst[:, c, :], raw[:, c, :], r[:], ALU.mult, [raw_k, r_k], [dst_k])

            latent(0, 3, cq_raw, cq_k, cqn, cqn_k)
            latent(384, 2, cq_raw, cq_k, ckvn, ckvn_k)
            pp, pp_k = proj(640, 32, 64)
            self.cp("act", kpe[64:96, :], pp[64:96, :], [pp_k], [kpe_k])

            def norm_rope_group(items, gkey):
                n_ = len(items)
                sqs, rs, nbs, rots, t1s, fins = [], [], [], [], [], []
                for (raw, raw_k, dst, dst_tok) in items:
                    sq, sq_k = sq2.next()
                    self.act(sq[0:96, :], raw[0:96, :], AF.Square, [raw_k], [sq_k])
                    sqs.append((sq, sq_k))
                for g_ in range(n_):
                    sq, sq_k = sqs[g_]
                    ss, ss_k = ssring.next()
                    self.mm(ss[0:96, :], self.ones_b[0:96, 0:96], sq[0:96, :], True, True, [sq_k, self.cb_k], [ss_k])
                    r, r_k = rring.next()
                    self.act(r[0:96, :], ss[0:96, :], AF.Sqrt, [ss_k], [r_k], scale=1.0 / 96, bias=EPS)
                    rs.append((r, r_k))
                for g_ in range(n_):
                    r, r_k = rs[g_]
                    self.recip(r[0:96, :], r[0:96, :], [r_k], [r_k])
                for g_ in range(n_):
                    raw, raw_k = items[g_][0], items[g_][1]
                    r, r_k = rs[g_]
                    nb, nb_k = nb_ring.next()
                    self.stt(nb[:], raw[0:96, :], self.vcol(gkey, 0, 96), r[0:96, :], ALU.mult, ALU.mult, [raw_k, r_k, self.vec_k], [nb_k])
                    nbs.append((nb, nb_k))
                for g_ in range(n_):
                    nb, nb_k = nbs[g_]
                    rot, rot_k = ssring.next()
                    self.mm(rot[0:96, :], pm_b, nb[:], True, True, [nb_k, pm_k], [rot_k])
                    rots.append((rot, rot_k))
                    t1, t1_k = t1_ring.next()
                    self.tt("pool", t1[:], nb[:], rp[:, 0, :], ALU.mult, [nb_k, rp_k], [t1_k])
                    t1s.append((t1, t1_k))
                for g_ in range(n_):
                    rot, rot_k = rots[g_]
                    fin, fin_k = fin_ring.next()
                    self.tt("dve", fin[:], rot[0:96, :], rp[:, 1, :], ALU.mult, [rot_k, rp_k], [fin_k])
                    fins.append((fin, fin_k))
                for g_ in range(n_):
                    fin, fin_k = fins[g_]
                    t1, t1_k = t1s[g_]
                    self.tt("pool", fin[:], fin[:], t1[:], ALU.add, [fin_k, t1_k], [fin_k])
                    self.dma(items[g_][2], fin[:], [fin_k], [items[g_][3]])

            for hg in ((0, 1, 2), (3, 4, 5), (6, 7)):
                items = []
                for h in hg:
                    pp, pp_k = pring.next()
                    for c in range(3):
                        self.mm(pp[0:96, :], wq[:, c, h * 96:(h + 1) * 96], cqn[:, c, :], c == 0, c == 2, [wq_k[c], cqn_k], [pp_k])
                    raw, raw_k = rawring.next()
                    self.cp("act", raw[0:96, :], pp[0:96, :], [pp_k], [raw_k])
                    items.append((raw, raw_k, sc["mla_qT"][h, :, tsl], sk["mla_qT"][tt]))
                norm_rope_group(items, ("mla_q_norm", j))
            for hg in ((0, 1, 2), (3, 4, 5), (6, 7)):
                items = []
                for h in hg:
                    pp, pp_k = pring.next()
                    for c in range(2):
                        self.mm(pp[0:64, :], wkv[:, c, h * 128:h * 128 + 64], ckvn[:, c, :], c == 0, c == 1, [wkv_k[c], ckvn_k], [pp_k])
                    raw, raw_k = rawring.next()
                    self.cp("act", raw[0:64, :], pp[0:64, :], [pp_k], [raw_k])
                    self.cp("pool", raw[64:96, :], kpe[64:96, :], [kpe_k], [raw_k])
                    items.append((raw, raw_k, sc["mla_kT"][h, :, tsl], sk["mla_kT"][tt]))
                norm_rope_group(items, ("mla_k_norm", j))
            for sub in range(4):
                sp_, sp_k = sring.next()
                for c in range(2):
                    self.mm(sp_[:], ckvn[:, c, sub * 128:(sub + 1) * 128], wv[:, c, :], c == 0, c == 1, [ckvn_k, wkv_k[c]], [sp_k])
                vx, vx_k = vext_ring.next()
                self.cp("act", vx[:, :, 0:64], sp_[:].rearrange("p (h d) -> p h d", d=64), [sp_k], [vx_k])
                r0 = tt * T + sub * 128
                self.dma(sc["mla_vext"][:, r0:r0 + 128, :].rearrange("h t d -> t h d"), vx[:], [vx_k], [sk["mla_vext"][tt]])
            if not do_gdn:
                continue
            cw = VOFF[("gdn_conv_w", j)]
            for g0 in range(0, 12, 4):
                grp = list(range(g0, g0 + 4))
                ys = {}
                for c in grp:
                    pp, pp_k = proj(672 + c * 128)
                    self.cp("act", convbuf[:, c, 3:3 + T], pp[:], [pp_k], [conv_k[c]])
                    y, y_k = yring.next()
                    ys[c] = (y, y_k)
                    self.act(y[:], convbuf[:, c, 0:T], AF.Copy, [conv_k[c], self.vec_k], [y_k], scale=self.vec[:, cw + c * 4:cw + c * 4 + 1])
                for tap in range(1, 4):
                    for c in grp:
                        y, y_k = ys[c]
                        self.stt(y[:], convbuf[:, c, tap:tap + T], self.vec[:, cw + c * 4 + tap:cw + c * 4 + tap + 1], y[:], ALU.mult, ALU.add,
                                 [conv_k[c], y_k, self.vec_k], [y_k])
                for c in grp:
                    y, y_k = ys[c]
                    self.cp("pool", convbuf[:, c, 0:3], convbuf[:, c, T:T + 3], [conv_k[c]], [conv_k[c]])
                    self.act(y[:], y[:], AF.Silu, [y_k], [y_k])
                if g0 < 8:
                    st_ = {}
                    for c in grp:
                        y, y_k = ys[c]
                        sq, sq_k = sq2.next()
                        self.act(sq[:], y[:], AF.Square, [y_k], [sq_k])
                        st_[c] = [sq, sq_k]
                    for c in grp:
                        sq, sq_k = st_[c]
                        ss, ss_k = ssring.next()
                        self.mm(ss[:], self.ones_b, sq[:], True, True, [sq_k, self.cb_k], [ss_k])
                        r, r_k = rring.next()
                        self.act(r[:], ss[:], AF.Sqrt, [ss_k], [r_k], scale=1.0, bias=EPS)
                        st_[c] = [r, r_k]
                    for c in grp:
                        r, r_k = st_[c]
                        self.recip(r[:], r[:], [r_k], [r_k])
                    for c in grp:
                        y, y_k = ys[c]
                        r, r_k = st_[c]
                        ob, ob_k = f32o.next()
                        self.stt(ob[:], y[:], (128.0 ** -0.5) if c < 4 else 1.0, r[:], ALU.mult, ALU.mult, [y_k, r_k], [ob_k])
                        nm = "gdn_qT" if c < 4 else "gdn_kT"
                        self.dma(sc[nm][c % 4, :, tsl], ob[:], [ob_k], [sk[nm][tt]])
                else:
                    for c in grp:
                        y, y_k = ys[c]
                        self.dma(sc["gdn_vT"][c - 8, :, tsl], y[:], [y_k], [sk["gdn_vT"][tt]])
            for c in range(4):
                pp, pp_k = proj(2208 + c * 128)
                ob, ob_k = f32o.next()
                self.act(ob[:], pp[:], AF.Silu, [pp_k], [ob_k])
                self.dma(sc["gdn_zs"][c, :, tsl], ob[:], [ob_k], [sk["gdn_zs"][tt]])
            for sub in range(4):
                sp_, sp_k = sring.next()
                for c in range(8):
                    self.mm(sp_[:, 0:8], hn[:, c, sub * 128:(sub + 1) * 128], win[:, c, 2720:2728], c == 0, c == 7, [win_k[c], hn_k], [sp_k])
                gb, gb_k = gbring.next()
                t4, t4_k = tmp4.next()
                self.tt("dve", t4[:], sp_[:, 0:4], dtb[:], ALU.add, [sp_k, dtb_k], [t4_k])
                self.act(t4[:], t4[:], AF.Exp, [t4_k], [t4_k])
                self.act(t4[:], t4[:], AF.Ln, [t4_k], [t4_k], bias=1.0)
                self.tt("dve", gb[:, 0:4], t4[:], negA[:], ALU.mult, [t4_k, negA_k], [gb_k])
                self.act(gb[:, 4:8], sp_[:, 4:8], AF.Sigmoid, [sp_k], [gb_k])
                r0 = tt * T + sub * 128
                self.dma(sc["gdn_gb"][r0:r0 + 128, :], gb[:], [gb_k], [sk["gdn_gb"][tt]])
        self.end_phase()

    def phase_mla_attn(self, i, nqt=NT):
        self.begin_phase()
        sc = self.scr
        kTs = [(self.sb([96, S], BF16, "kT"), Tok()) for _ in range(2)]
        Vs = [(self.sb([128, 32, 128], BF16, "V"), Tok()) for _ in range(2)]
        qring = self.ring(2, [96, T], BF16, "q")
        ering = self.ring(6, [128, T], BF16, "e")
        rden, rden_k = self.sb([64, T], F32, "rden"), Tok()
        oring = self.ring(2, [64, T], BF16, "o")
        ps_st = Ring([self.psum[0], self.psum[1], self.psum[2], self.psum[5]])
        ps_o = Ring([self.psum[3], self.psum[4]])
        scale = 96.0 ** -0.5
        cmt, cm_k = self.lconst(C_CM, 2048, True)
        cm = cmt[:, :]

        def load_head(h):
            kT, kT_k = kTs[h % 2]
            self.dma(kT[:], sc["mla_kT"][h], [], [kT_k])
            V, V_k = Vs[h % 2]
            self.dma(V[:], sc["mla_vext"][h].rearrange("(kt p) d -> p kt d", p=128), [], [V_k])

        load_head(0)
        for h in range(8):
            if h + 1 < 8:
                load_head(h + 1)
            kT, kT_k = kTs[h % 2]
            V, V_k = Vs[h % 2]
            for qt in range(nqt):
                q, q_k = qring.next()
                self.dma(q[:], sc["mla_qT"][h, :, qt * T:(qt + 1) * T], [], [q_k])
                po, po_k = ps_o.next()
                nk = 4 * qt + 4
                pend = []
                for kt in range(nk):
                    ps, ps_k = ps_st.next()
                    self.mm(ps[:], kT[:, kt * 128:(kt + 1) * 128], q[:], True, True, [kT_k, q_k], [ps_k])
                    e, e_k = ering.next()
                    self.act(e[:], ps[:], AF.Exp, [ps_k], [e_k], scale=scale)
                    off = kt - 4 * qt
                    if off >= 0:
                        self.tt("pool", e[:], e[:], cm[:, off * 512:(off + 1) * 512], ALU.mult, [e_k, cm_k], [e_k])
                    pend.append((kt, e, e_k))
                    if len(pend) > 2:
                        kt2, e2, e2_k = pend.pop(0)
                        self.mm(po[:], V[:, kt2, :], e2[:], kt2 == 0, kt2 == nk - 1, [V_k, e2_k], [po_k])
                while pend:
                    kt2, e2, e2_k = pend.pop(0)
                    self.mm(po[:], V[:, kt2, :], e2[:], kt2 == 0, kt2 == nk - 1, [V_k, e2_k], [po_k])
                self.recip(rden[:], po[64:128, :], [po_k], [rden_k])
                o, o_k = oring.next()
                self.tt("dve", o[:], po[0:64, :], rden[:], ALU.mult, [po_k, rden_k], [o_k])
                self.dma(sc["even_oT"][h * 64:(h + 1) * 64, qt * T:(qt + 1) * T], o[:], [o_k], [self.scr_k["even_oT"][qt]])
        self.end_phase()

    def phase_even_out(self, i, ntt=NT, wname="even_w_out"):
        j = i // 2
        if ("ffn", i, 2) in self.phases:
            self.ffn_weights_begin(i, 2)
        self.begin_phase()
        wout = self.sb([128, 8, D], BF16, "wout")
        wout_k = [Tok() for _ in range(8)]
        wstage = self.ring(2, [128, D], F32, "wst")
        for c in range(8):
            self.load_w(wout[:, c, :], wout_k[c], self.d[wname][j][c * 128:(c + 1) * 128, :], wstage, None, c)
        oring = self.ring(2, [128, 8, T], BF16, "oT")
        xstage = self.ring(4, [128, T], F32, "xst")
        obring = self.ring(3, [128, T], F32, "ob")
        yps = Ring([self.psum[0], self.psum[1], self.psum[2]])
        oT_d = self.scr["even_oT"].rearrange("(c p) t -> p c t", p=128)
        for tt in range(ntt):
            tsl = slice(tt * T, (tt + 1) * T)
            o, o_k = oring.next()
            self.dma(o[:], oT_d[:, :, tsl], [], [o_k])
            for m in range(8):
                yp, yp_k = yps.next()
                for c in range(8):
                    self.mm(yp[:], wout[:, c, m * 128:(m + 1) * 128], o[:, c, :], c == 0, c == 7, [wout_k[c], o_k], [yp_k])
                xs, xs_k = xstage.next()
                self.dma(xs[:], self.xsrc[m * 128:(m + 1) * 128, tsl], [self.xtok[tt][m]], [xs_k])
                ob, ob_k = obring.next()
                self.tt("dve", ob[:], yp[:], xs[:], ALU.add, [yp_k, xs_k], [ob_k])
                self.dma(self.xres[m * 128:(m + 1) * 128, tsl], ob[:], [ob_k], [self.xtok[tt][m]], q="act")
            self.pref_step(7)
        self.pref_step()
        self.end_phase()

    def phase_gdn(self, i, nsc=32, stop=None):
        j = i // 2
        self.begin_phase()
        sc = self.scr
        gct, gc_k0 = self.lconst(C_MS, 386)
        MS = gct[:, 0:128]
        MI = gct[:, 128:256]
        UI = gct[:, 256:384]
        CIND = gct[:, 384:386]
        self.f.barrier()
        ck = self.cst_k

        def t4(name, n=1):
            return [(self.sb([128, 4, 128], F32, name), Tok()) for _ in range(n)]
        kTs = t4("kT", 2); qTs = t4("qT", 2); vTs = t4("vT", 2); zss = t4("zs", 2)
        gbs = [(self.sb([128, 8], F32, "gb"), Tok()) for _ in range(2)]
        (GU, GU_k), (gB, gB_k), (dec, dec_k), (dmI, dmI_k), (dmS, dmS_k) = t4("GU")[0], t4("gB")[0], t4("dec")[0], t4("dmI")[0], t4("dmS")[0]
        (qk, qk_k), (Nn, Nn_k), (qkT, qkT_k) = t4("qk")[0], t4("N")[0], t4("qkT")[0]
        Ps = t4("P", 2); Qs = t4("Q", 2)
        (X, X_k), (vb, vb_k), (kbg, kbg_k), (kd, kd_k) = t4("X")[0], t4("vb")[0], t4("kbg")[0], t4("kd")[0]
        (u, u_k), (wT, wT_k), (vnew, vnew_k), (o2s, o2s_k), (o, o_k) = t4("u")[0], t4("wT")[0], t4("vnew")[0], t4("o2s")[0], t4("o")[0]
        (oTs, oTs_k), (rr, rr_k), (fin32, fin32_k) = t4("oTs")[0], t4("rr")[0], t4("fin32")[0]
        sqb, sqb_k = self.sb([128, 4, 128], BF16, "sqb"), Tok()

        def b4(name):
            return self.sb([128, 4, 128], BF16, name), Tok()
        (qTb, qTb_k), (wTb, wTb_k), (Sb, Sb_k), (vnb, vnb_k), (qkTb, qkTb_k), (kdb, kdb_k) = b4("qTb"), b4("wTb"), b4("Sb"), b4("vnb"), b4("qkTb"), b4("kdb")
        self.f.op("pool", lambda e: e.memset(Sb[:], 0.0), [], [Sb_k])
        finb = self.ring(2, [128, 4, 128], BF16, "finb")
        Sst, S_k = self.sb([128, 4, 128], F32, "Sst"), Tok()
        self.f.op("pool", lambda e: e.memset(Sst[:], 0.0), [], [S_k])
        sm, sm_k = self.sb([128, 32], F32, "sm"), Tok(hard=True)
        B = self.psum
        oT_d = sc["even_oT"].rearrange("(c p) t -> p c t", p=128)

        def F(ap):
            return ap.rearrange("p h x -> p (h x)")

        def load(sci):
            t0 = sci * 128
            for nm, bufs in (("gdn_kT", kTs), ("gdn_qT", qTs), ("gdn_vT", vTs), ("gdn_zs", zss)):
                b, b_k = bufs[sci % 2]
                self.dma(b[:], sc[nm][:, :, t0:t0 + 128].rearrange("h d t -> d h t"), [], [b_k])
            gb, gb_k = gbs[sci % 2]
            self.dma(gb[:], sc["gdn_gb"][t0:t0 + 128, :], [], [gb_k])

        load(0)
        for sci in range(nsc):
            if sci + 1 < nsc:
                load(sci + 1)
            t0 = sci * 128
            kT, kT_k = kTs[sci % 2]; qT, qT_k = qTs[sci % 2]; vT, vT_k = vTs[sci % 2]; zs, zs_k = zss[sci % 2]
            gb, gb_k = gbs[sci % 2]
            for h in range(4):
                self.act(GU[:, h, :], UI, AF.Copy, [ck, gb_k], [GU_k], scale=gb[:, h:h + 1])
                self.act(gB[:, h, :], self.ones_f, AF.Copy, [ck, gb_k], [gB_k], scale=gb[:, h:h + 1])
            for h in range(4):
                self.mm(B[3][0][:, 2 * h:2 * h + 2], GU[:, h, :], self.ones_f[:, 0:2], True, True, [GU_k, ck], [B[3][1]])
                self.mm(B[3][0][:, 8 + 2 * h:10 + 2 * h], gB[:, h, :], CIND, True, True, [gB_k, ck], [B[3][1]])
                self.mm(B[0][0][:, h * 128:(h + 1) * 128], GU[:, h, :], MS, True, True, [GU_k, ck], [B[0][1]])
            self.cp("dve", sm[:, 0:4], B[3][0][:, 0:8].rearrange("p (h two) -> p h two", two=2)[:, :, 0], [B[3][1]], [sm_k])
            self.cp("dve", sm[:, 4:12], B[3][0][:, 8:16], [B[3][1]], [sm_k])
            for h in range(4):
                self.tt("pool", sm[0:64, 12 + h:13 + h], sm[0:64, 4 + 2 * h:5 + 2 * h], sm[0:64, h:h + 1], ALU.subtract, [sm_k], [sm_k])
                self.tt("pool", sm[64:128, 12 + h:13 + h], sm[64:128, 5 + 2 * h:6 + 2 * h], sm[64:128, h:h + 1], ALU.subtract, [sm_k], [sm_k])
            self.act(sm[:, 0:16], sm[:, 0:16], AF.Exp, [sm_k], [sm_k])
            self.ts("pool", sm[:, 16:20], gb[:, 4:8], -1.0, ALU.mult, [gb_k, sm_k], [sm_k])
            self.tt("pool", sm[:, 20:24], gb[:, 4:8], sm[:, 0:4], ALU.mult, [gb_k, sm_k], [sm_k])
            self.act(F(dec[:]), B[0][0][:], AF.Exp, [B[0][1]], [dec_k])
            self.tt("pool", dmI[:], dec[:], MI.unsqueeze(1).to_broadcast([128, 4, 128]), ALU.mult, [dec_k, ck], [dmI_k])
            self.tt("pool", dmS[:], dec[:], MS.unsqueeze(1).to_broadcast([128, 4, 128]), ALU.mult, [dec_k, ck], [dmS_k])
            for h in range(4):
                self.mm(B[1][0][:, h * 128:(h + 1) * 128], kT[:, h, :], kT[:, h, :], True, True, [kT_k], [B[1][1]])
                self.mm(B[2][0][:, h * 128:(h + 1) * 128], qT[:, h, :], kT[:, h, :], True, True, [qT_k, kT_k], [B[2][1]])
            self.tt("dve", F(qk[:]), B[2][0][:], F(dmI[:]), ALU.mult, [B[2][1], dmI_k], [qk_k])
            for h in range(4):
                self.stt(Nn[:, h, :], B[1][0][:, h * 128:(h + 1) * 128], sm[:, 16 + h:17 + h], dmS[:, h, :], ALU.mult, ALU.mult, [B[1][1], sm_k, dmS_k], [Nn_k])
            for h in range(4):
                self.tr(B[0][0][:, h * 128:(h + 1) * 128], Nn[:, h, :], self.ident_f, [Nn_k, ck], [B[0][1]])
                self.tr(B[1][0][:, h * 128:(h + 1) * 128], qk[:, h, :], self.ident_f, [qk_k, ck], [B[1][1]])
            P, P_k = Ps[0]
            Q, Q_k = Nn, Nn_k
            self.cp("act", F(P[:]), B[0][0][:], [B[0][1]], [P_k])
            self.cp("act", F(qkTb[:]), B[1][0][:], [B[1][1]], [qkTb_k])
            if stop == 'A':
                continue
            self.tt("pool", X[:], P[:], self.ident_f.unsqueeze(1).to_broadcast([128, 4, 128]), ALU.add, [P_k, ck], [X_k])
            for lv in range(1, 6):
                Pn, Pn_k = Ps[lv % 2]
                Qn, Qn_k = Qs[lv % 2]
                for h in range(4):
                    if lv < 5:
                        self.mm(B[1][0][:, h * 128:(h + 1) * 128], Q[:, h, :], P[:, h, :], True, True, [Q_k, P_k], [B[1][1]])
                    self.mm(B[2][0][:, h * 128:(h + 1) * 128], P[:, h, :], Q[:, h, :], True, True, [Q_k, P_k], [B[2][1]])
                if lv < 5:
                    self.cp("act", F(Pn[:]), B[1][0][:], [B[1][1]], [Pn_k])
                self.cp("dve", F(Qn[:]), B[2][0][:], [B[2][1]], [Qn_k])
                for h in range(4):
                    self.mm(B[0][0][:, h * 128:(h + 1) * 128], Qn[:, h, :], X[:, h, :], True, True, [Qn_k, X_k], [B[0][1]])
                self.tt("dve", F(X[:]), F(X[:]), B[0][0][:], ALU.add, [X_k, B[0][1]], [X_k])
                P, P_k, Q, Q_k = Pn, Pn_k, Qn, Qn_k
            if stop == 'B':
                continue
            for h in range(4):
                self.tr(B[1][0][:, h * 128:(h + 1) * 128], vT[:, h, :], self.ident_f, [vT_k, ck], [B[1][1]])
                self.tr(B[2][0][:, h * 128:(h + 1) * 128], kT[:, h, :], self.ident_f, [kT_k, ck], [B[2][1]])
            for h in range(4):
                self.act(vb[:, h, :], B[1][0][:, h * 128:(h + 1) * 128], AF.Copy, [B[1][1], gb_k], [vb_k], scale=gb[:, 4 + h:5 + h])
                self.ts("dve", kbg[:, h, :], B[2][0][:, h * 128:(h + 1) * 128], sm[:, 20 + h:21 + h], ALU.mult, [B[2][1], sm_k], [kbg_k])
                self.act(kdb[:, h, :], B[2][0][:, h * 128:(h + 1) * 128], AF.Copy, [B[2][1], sm_k], [kdb_k], scale=sm[:, 12 + h:13 + h])
            for h in range(4):
                self.mm(B[0][0][:, h * 128:(h + 1) * 128], X[:, h, :], vb[:, h, :], True, True, [X_k, vb_k], [B[0][1]])
                self.mm(B[1][0][:, h * 128:(h + 1) * 128], kbg[:, h, :], X[:, h, :], True, True, [X_k, kbg_k], [B[1][1]])
            self.cp("act", F(u[:]), B[0][0][:], [B[0][1]], [u_k])
            self.cp("dve", F(wTb[:]), B[1][0][:], [B[1][1]], [wTb_k])
            if stop == 'C':
                continue
            self.cp("pool", qTb[:], qT[:], [qT_k], [qTb_k])
            for ch in range(2):
                r = slice(ch * 64, ch * 64 + 64)
                for h in range(4):
                    self.mm(B[4][0][r, h * 128:(h + 1) * 128], wTb[:, h, r], Sb[:, h, :], True, True, [wTb_k, Sb_k], [B[4][1]])
                self.tt("dve", F(vnb[r]), F(u[r]), B[4][0][r, :], ALU.subtract, [u_k, B[4][1]], [vnb_k])
                for h in range(4):
                    self.mm(B[5][0][r, h * 128:(h + 1) * 128], qTb[:, h, r], Sb[:, h, :], True, True, [qTb_k, Sb_k], [B[5][1]])
                for h in range(4):
                    self.mm(B[6][0][r, h * 128:(h + 1) * 128], qkTb[r, h, r], vnb[r, h, :], True, True, [qkTb_k, vnb_k], [B[6][1]])
                self.cp("act", F(o2s[r]), B[6][0][r, :], [B[6][1]], [o2s_k])
                for h in range(4):
                    self.stt(o[r, h, :], B[5][0][r, h * 128:(h + 1) * 128], sm[r, h:h + 1], o2s[r, h, :], ALU.mult, ALU.add, [B[5][1], sm_k, o2s_k], [o_k])
                for h in range(4):
                    self.mm(B[7][0][:, h * 128:(h + 1) * 128], kdb[r, h, :], vnb[r, h, :], True, True, [kdb_k, vnb_k], [B[7][1]])
                for h in range(4):
                    self.stt(Sst[:, h, :], Sst[:, h, :], sm[:, 4 + 2 * h + ch:5 + 2 * h + ch], B[7][0][:, h * 128:(h + 1) * 128], ALU.mult, ALU.add, [S_k, sm_k, B[7][1]], [S_k])
                self.cp("pool", Sb[:], Sst[:], [S_k], [Sb_k])
            if stop == 'D':
                continue
            for h in range(4):
                self.tr(B[2][0][:, h * 128:(h + 1) * 128], o[:, h, :], self.ident_f, [o_k, ck], [B[2][1]])
            self.cp("act", F(oTs[:]), B[2][0][:], [B[2][1]], [oTs_k])
            self.act(F(sqb[:]), B[2][0][:], AF.Square, [B[2][1]], [sqb_k])
            self.mm(B[3][0][:], self.ones_b, F(sqb[:]), True, True, [sqb_k, self.cb_k], [B[3][1]])
            self.act(F(rr[:]), B[3][0][:], AF.Sqrt, [B[3][1]], [rr_k], scale=1.0 / 128, bias=EPS)
            self.recip(F(rr[:]), F(rr[:]), [rr_k], [rr_k])
            self.stt(F(fin32[:]), F(oTs[:]), self.vcol(("gdn_out_norm", j)), F(rr[:]), ALU.mult, ALU.mult, [oTs_k, rr_k, self.vec_k], [fin32_k])
            fb, fb_k = finb.next()
            self.tt("pool", fb[:], fin32[:], zs[:], ALU.mult, [fin32_k, zs_k], [fb_k])
            self.dma(oT_d[:, 4:8, t0:t0 + 128], fb[:], [fb_k], [self.scr_k["even_oT"][sci // 4]])
        self.end_phase()

    def phase_zero_gdn(self):
        self.begin_phase()
        z, z_k = self.sb([128, T], BF16, "z"), Tok()
        self.f.op("pool", lambda e: e.memset(z[:], 0.0), [], [z_k])
        for c in range(4, 8):
            for tt in range(NT):
                self.dma(self.scr["even_oT"][c * 128:(c + 1) * 128, tt * T:(tt + 1) * T], z[:], [z_k], [self.scr_k["even_oT"][tt]])
        self.end_phase()

    def phase_copy(self):
        self.begin_phase()
        xstage = self.ring(4, [128, T], F32, "xst")
        for tt in range(NT):
            for c in range(8):
                xs, xs_k = xstage.next()
                self.dma(xs[:], self.xsrc[c * 128:(c + 1) * 128, tt * T:(tt + 1) * T], [self.xtok[tt][c]], [xs_k])
                self.dma(self.xres[c * 128:(c + 1) * 128, tt * T:(tt + 1) * T], xs[:], [xs_k], [self.xtok[tt][c]])
        self.end_phase()
        self.xsrc = self.xres

    def build(self):
        self.setup()
        for ph in self.phases:
            kind = ph[0]
            if kind == "ffn":
                self.phase_ffn(ph[1], ph[2])
            elif kind == "ple":
                self.phase_ple(ph[1])
            elif kind == "copy":
                self.phase_copy()
            elif kind == "evenproj":
                self.phase_even_proj(ph[1], *ph[2:])
            elif kind == "mla":
                self.phase_mla_attn(ph[1], *ph[2:])
            elif kind == "evenout":
                self.phase_even_out(ph[1], *ph[2:])
            elif kind == "gdn":
                self.phase_gdn(ph[1], *ph[2:])
            elif kind == "oddout":
                self.phase_even_out(ph[1], ph[2] if len(ph) > 2 else NT, "odd_w_out")
            elif kind == "zero_gdn":
                self.phase_zero_gdn()
            elif kind == "oddproj":
                self.phase_odd_proj(ph[1])
            elif kind == "dsa":
                self.phase_dsa(ph[1], *ph[2:])
            else:
                raise ValueError(ph)
        self.f.barrier()
        self.f.emit()
        self.gst.close()
        self.f.close()
        return self.nc


def all_phases():
    ph = []
    for i in range(DEPTH):
        ph.append(("ffn", i, 1))
        if i % 2 == 0:
            ph += [("evenproj", i), ("mla", i), ("gdn", i), ("evenout", i)]
        else:
            ph += [("oddproj", i), ("dsa", i), ("oddout", i)]
        ph.append(("ffn", i, 2))
        ph.append(("ple", i))
    return ph


def make_in_maps(inputs, cores):
    vecs = pack_vecs(inputs)
    consts = make_consts()
    rope = make_rope()
    x = np.asarray(inputs["x"])
    p = np.asarray(inputs["p"])
    shared = {k: np.ascontiguousarray(np.asarray(inputs[k], dtype=np.float32)) for k in WEIGHT_SHAPES}
    maps = []
    for b in cores:
        m = dict(shared)
        m["xT"] = np.ascontiguousarray(x[b].T)
        m["pT"] = np.ascontiguousarray(p[:, b].transpose(0, 2, 1))
        m["vecs"] = vecs
        m["consts"] = consts
        m["rope"] = rope
        maps.append(m)
    return maps


def kernel(**inputs):
    prog = Prog(all_phases())
    nc = prog.build()
    maps = make_in_maps(inputs, list(range(8)))
    res = run_bass_kernel_spmd(nc, maps, core_ids=list(range(8)))
    out = np.stack([np.ascontiguousarray(r["outT"].T) for r in res.results], axis=0)
    return out.astype(np.float32)
```

```python
import numpy as np
from contextlib import ExitStack
import concourse.bass as bass
import concourse.mybir as mybir
from concourse.bass_utils import run_bass_kernel_spmd

F32 = mybir.dt.float32
BF16 = mybir.dt.bfloat16
AF = mybir.ActivationFunctionType
ALU = mybir.AluOpType

S = 4096
D = 1024
T = 512
NT = S // T
DFF = 2816
NJ = DFF // 128
EPS = 1e-6
DEPTH = 4
PLE = 256

SAME_ENGINE_SYNC = ("act", "dve", "pool")
SYNC_ALL_SAME = True


class Tok:
    __slots__ = ("w", "re", "rd", "excl", "hard")

    def __init__(self, excl=False, hard=False):
        self.w = None
        self.re = {}
        self.rd = []
        self.hard = hard
        self.excl = excl


class Fw:
    ENG = ("pe", "act", "dve", "pool", "sp")

    def __init__(self, nc, n_dma_sems=48):
        self.nc = nc
        self.stack = ExitStack()
        self.sem = {e: self.stack.enter_context(nc.semaphore("s_" + e)) for e in self.ENG}
        self.dsem = [self.stack.enter_context(nc.semaphore("d%d" % i)) for i in range(n_dma_sems)]
        self.dcnt = [0] * n_dma_sems
        self.dnext = 0
        self.ops = {e: [] for e in self.ENG}
        self.nops = {e: 0 for e in self.ENG}
        self.signal = {e: set() for e in self.ENG}
        self.seen_e = {e: {f: -1 for f in self.ENG} for e in self.ENG}
        self.seen_d = {e: [0] * n_dma_sems for e in self.ENG}

    def _need(self, eng, ev, hard=True):
        if ev is None:
            return
        if ev[0] == "e":
            _, f, idx = ev
            if f == eng and (eng not in SAME_ENGINE_SYNC or not (hard or SYNC_ALL_SAME)):
                return
            if self.seen_e[eng][f] >= idx:
                return
            self.seen_e[eng][f] = idx
            self.signal[f].add(idx)
            self.ops[eng].append(("wait_e", f, idx))
        else:
            _, s, val = ev
            if self.seen_d[eng][s] >= val:
                return
            self.seen_d[eng][s] = val
            self.ops[eng].append(("wait_d", s, val))

    def _deps(self, eng, reads, writes):
        for t in reads:
            self._need(eng, t.w, t.hard)
            if t.excl:
                for f, idx in t.re.items():
                    if f != eng:
                        self._need(eng, ("e", f, idx))
        for t in writes:
            self._need(eng, t.w, t.hard)
            for f, idx in t.re.items():
                self._need(eng, ("e", f, idx), t.hard)
            for r in t.rd:
                self._need(eng, r)

    def _commit(self, ev, reads, writes):
        for t in reads:
            if ev[0] == "e":
                t.re[ev[1]] = ev[2]
            else:
                t.rd.append(ev)
        for t in writes:
            t.w = ev
            t.re = {}
            t.rd = []

    def op(self, eng, fn, reads=(), writes=()):
        self._deps(eng, reads, writes)
        idx = self.nops[eng]
        self.nops[eng] += 1
        self.ops[eng].append(("op", fn, idx))
        ev = ("e", eng, idx)
        self._commit(ev, reads, writes)
        return ev

    def dma(self, q, out, in_, reads=(), writes=(), **kw):
        self._deps(q, reads, writes)
        s = self.dnext
        self.dnext = (self.dnext + 1) % len(self.dsem)
        if self.dcnt[s] > 0:
            self._need(q, ("d", s, self.dcnt[s]))
        self.dcnt[s] += 16
        val = self.dcnt[s]
        self.ops[q].append(("dma", out, in_, s, kw))
        ev = ("d", s, val)
        self._commit(ev, reads, writes)
        return ev

    def barrier(self):
        last = {}
        for e in self.ENG:
            if self.nops[e] > 0:
                last[e] = ("e", e, self.nops[e] - 1)
        for e in self.ENG:
            for f_, ev in last.items():
                if f_ != e:
                    self._need(e, ev)
            for s in range(len(self.dsem)):
                if self.dcnt[s] > 0:
                    self._need(e, ("d", s, self.dcnt[s]))

    def emit(self):
        nc = self.nc
        cnt = {}
        for e in self.ENG:
            m = {}
            c = 0
            for idx in sorted(self.signal[e]):
                c += 1
                m[idx] = c
            cnt[e] = m
        self.sigcount = {e: len(cnt[e]) for e in self.ENG}

        def run(e):
            def body(engh):
                for it in self.ops[e]:
                    k = it[0]
                    if k == "op":
                        ins = it[1](engh)
                        if it[2] in cnt[e]:
                            ins.then_inc(self.sem[e], 1)
                    elif k == "wait_e":
                        engh.wait_ge(self.sem[it[1]], cnt[it[1]][it[2]])
                    elif k == "wait_d":
                        engh.wait_ge(self.dsem[it[1]], it[2])
                    elif k == "dma":
                        engh.dma_start(out=it[1], in_=it[2], **it[4]).then_inc(self.dsem[it[3]], 16)
            return body

        with nc.Block() as block:
            block.sync(run("sp"))
            block.scalar(run("act"))
            block.vector(run("dve"))
            block.gpsimd(run("pool"))
            block.tensor(run("pe"))

    def close(self):
        self.stack.close()


class Ring:
    def __init__(self, items):
        self.items = items
        self.i = 0

    def next(self):
        it = self.items[self.i]
        self.i = (self.i + 1) % len(self.items)
        return it


def _vec_layout():
    off = {}
    n = 0
    for i in range(DEPTH):
        for nm in ("ln_ffn1", "ln_mix", "ln_ffn2", "ln_ple"):
            off[(nm, i)] = n
            n += 8
    for j in range(2):
        off[("mla_q_a_norm", j)] = n; n += 3
        off[("mla_kv_a_norm", j)] = n; n += 2
        off[("mla_q_norm", j)] = n; n += 1
        off[("mla_k_norm", j)] = n; n += 1
        off[("gdn_conv_w", j)] = n; n += 48
        off[("gdn_out_norm", j)] = n; n += 1
        off[("dsa_q_norm", j)] = n; n += 1
        off[("dsa_k_norm", j)] = n; n += 1
        off[("dsa_kidx_norm", j)] = n; n += 1
    return off, n


VOFF, NV = _vec_layout()


def pack_vecs(inp):
    v = np.zeros((128, NV), np.float32)
    for i in range(DEPTH):
        for nm in ("ln_ffn1", "ln_mix", "ln_ffn2", "ln_ple"):
            o = VOFF[(nm, i)]
            v[:, o:o + 8] = np.asarray(inp[nm][i]).reshape(8, 128).T
    for j in range(2):
        o = VOFF[("mla_q_a_norm", j)]; v[:, o:o + 3] = np.asarray(inp["mla_q_a_norm"][j]).reshape(3, 128).T
        o = VOFF[("mla_kv_a_norm", j)]; v[:, o:o + 2] = np.asarray(inp["mla_kv_a_norm"][j]).reshape(2, 128).T
        o = VOFF[("mla_q_norm", j)]; v[:96, o] = np.asarray(inp["mla_q_norm"][j])
        o = VOFF[("mla_k_norm", j)]; v[:96, o] = np.asarray(inp["mla_k_norm"][j])
        o = VOFF[("gdn_conv_w", j)]
        cw = np.asarray(inp["gdn_conv_w"][j])
        v[:, o:o + 48] = cw.reshape(4, 12, 128).transpose(2, 1, 0).reshape(128, 48)
        o = VOFF[("gdn_out_norm", j)]; v[:, o] = np.asarray(inp["gdn_out_norm"][j])
        o = VOFF[("dsa_q_norm", j)]; v[:, o] = np.asarray(inp["dsa_q_norm"][j])
        o = VOFF[("dsa_k_norm", j)]; v[:, o] = np.asarray(inp["dsa_k_norm"][j])
        o = VOFF[("dsa_kidx_norm", j)]; v[:64, o] = np.asarray(inp["dsa_kidx_norm"][j]); v[64:, o] = np.asarray(inp["dsa_kidx_norm"][j])
    return v


C_IDENT = 0
C_ONES = 128
C_NEG = 256
C_PM = 384
C_CM = 512
C_MS = 2560
C_MI = 2688
C_UI = 2816
C_CIND = 2944
NC_CONST = 2946
NEGBIG = -1.0e30


def make_consts():
    c = np.zeros((128, NC_CONST), np.float32)
    c[:, C_IDENT:C_IDENT + 128] = np.eye(128, dtype=np.float32)
    c[:, C_ONES:C_ONES + 128] = 1.0
    c[:, C_NEG:C_NEG + 128] = np.triu(np.full((128, 128), NEGBIG, np.float32), 1)
    for m in range(64, 80):
        c[m + 16, C_PM + m] = 1.0
        c[m, C_PM + m + 16] = 1.0
    a_ = np.arange(128)
    same = (a_[:, None] // 64) == (a_[None, :] // 64)
    c[:, C_MS:C_MS + 128] = (same & (a_[None, :] < a_[:, None])).astype(np.float32)
    c[:, C_MI:C_MI + 128] = (same & (a_[None, :] <= a_[:, None])).astype(np.float32)
    c[:, C_UI:C_UI + 128] = (same & (a_[:, None] <= a_[None, :])).astype(np.float32)
    c[:64, C_CIND] = 1.0
    c[64:, C_CIND + 1] = 1.0
    kk = np.arange(128)[:, None]
    qq = np.arange(512)[None, :]
    for off in range(4):
        c[:, C_CM + off * 512:C_CM + (off + 1) * 512] = (off * 128 + kk <= qq).astype(np.float32)
    return c


def make_rope():
    half = 16
    inv_freq = 10000.0 ** (-np.arange(half, dtype=np.float64) / half)
    ang = np.arange(S, dtype=np.float64)[None, :] * inv_freq[:, None]
    cos = np.ones((96, S), np.float64)
    sin = np.zeros((96, S), np.float64)
    cos[64:80] = np.cos(ang); cos[80:96] = np.cos(ang)
    sin[64:80] = -np.sin(ang); sin[80:96] = np.sin(ang)
    return np.stack([cos, sin]).astype(np.float32)


WEIGHT_SHAPES = {
    "ffn1_w_gu": (4, D, 2 * DFF), "ffn1_w_down": (4, DFF, D),
    "ffn2_w_gu": (4, D, 2 * DFF), "ffn2_w_down": (4, DFF, D),
    "even_w_in": (2, D, 2728), "mla_w_q_up": (2, 384, 768), "mla_w_kv_up": (2, 256, 1024),
    "even_w_out": (2, 1024, D), "odd_w_in": (2, D, 2120), "odd_w_out": (2, 1024, D),
    "ple_w_in": (4, PLE, D), "ple_w_gate": (4, D, D),
    "gdn_a_log": (2, 4), "gdn_dt_bias": (2, 4),
}


class Prog:
    def __init__(self, phases):
        self.nc = nc = bass.Bass("TRN2", target_bir_lowering=False)
        self.f = Fw(nc)
        self.phases = phases
        self.d = {}
        self.d["xT"] = nc.dram_tensor("xT", [D, S], F32, kind="ExternalInput").ap()
        self.d["pT"] = nc.dram_tensor("pT", [DEPTH, PLE, S], F32, kind="ExternalInput").ap()
        self.d["vecs"] = nc.dram_tensor("vecs", [128, NV], F32, kind="ExternalInput").ap()
        self.d["consts"] = nc.dram_tensor("consts", [128, NC_CONST], F32, kind="ExternalInput").ap()
        self.d["rope"] = nc.dram_tensor("rope", [2, 96, S], F32, kind="ExternalInput").ap()
        for k, shp in WEIGHT_SHAPES.items():
            self.d[k] = nc.dram_tensor(k, list(shp), F32, kind="ExternalInput").ap()
        self.xres = nc.dram_tensor("outT", [D, S], F32, kind="ExternalOutput").ap()
        self.scr = {}
        for nm, shp, dt in (("dsa_qT", [8, 128, S], BF16), ("dsa_kT", [2, 128, S], BF16), ("dsa_v", [S, 256], BF16),
                            ("dsa_qiT", [4, 128, S], BF16), ("dsa_kiT", [128, S], BF16), ("dsa_w", [S, 8], F32),
                            ("mla_qT", [8, 96, S], BF16), ("mla_kT", [8, 96, S], BF16), ("mla_vext", [8, S, 128], BF16),
                            ("gdn_qT", [4, 128, S], F32), ("gdn_kT", [4, 128, S], F32), ("gdn_vT", [4, 128, S], F32),
                            ("gdn_zs", [4, 128, S], F32), ("gdn_gb", [S, 8], F32), ("even_oT", [D, S], BF16)):
            self.scr[nm] = nc.dram_tensor(nm, shp, dt).ap()
        self.scr_k = {nm: [Tok() for _ in range(32)] for nm in self.scr}
        self.xsrc = self.d["xT"]
        self.xtok = [[Tok() for _ in range(8)] for _ in range(NT)]
        self.pref = None
        self.gst = ExitStack()
        self.pst = None
        self._n = 0

    def _name(self, p):
        self._n += 1
        return "%s_%d" % (p, self._n)

    def gsb(self, shape, dt, name="g"):
        return self.gst.enter_context(self.nc.sbuf_tensor(self._name(name), list(shape), dt))

    def sb(self, shape, dt, name="t"):
        return self.pst.enter_context(self.nc.sbuf_tensor(self._name(name), list(shape), dt))

    def ring(self, n, shape, dt, name="r"):
        small = int(np.prod(shape[1:])) < 64
        return Ring([(self.sb(shape, dt, name), Tok(hard=small)) for _ in range(n)])

    def mm(self, out, lhsT, rhs, start, stop, reads, writes):
        self.f.op("pe", lambda e: e.matmul(out, lhsT=lhsT, rhs=rhs, start=start, stop=stop), reads, writes)

    def tr(self, out, in_, ident, reads, writes):
        self.f.op("pe", lambda e: e.transpose(out, in_, ident), reads, writes)

    def act(self, out, in_, func, reads, writes, scale=None, bias=None):
        kw = {}
        if scale is not None:
            kw["scale"] = scale
        if bias is not None:
            kw["bias"] = bias
        self.f.op("act", lambda e: e.activation(out=out, in_=in_, func=func, **kw), reads, writes)

    def tt(self, eng, out, in0, in1, op, reads, writes):
        self.f.op(eng, lambda e: e.tensor_tensor(out=out, in0=in0, in1=in1, op=op), reads, writes)

    def ts(self, eng, out, in0, s1, op0, reads, writes, s2=None, op1=None):
        if op1 is None:
            self.f.op(eng, lambda e: e.tensor_scalar(out=out, in0=in0, scalar1=s1, scalar2=None, op0=op0), reads, writes)
        else:
            self.f.op(eng, lambda e: e.tensor_scalar(out=out, in0=in0, scalar1=s1, scalar2=s2, op0=op0, op1=op1), reads, writes)

    def stt(self, out, in0, scalar, in1, op0, op1, reads, writes):
        self.f.op("dve", lambda e: e.scalar_tensor_tensor(out=out, in0=in0, scalar=scalar, in1=in1, op0=op0, op1=op1), reads, writes)

    def cp(self, eng, out, in_, reads, writes):
        if eng == "act":
            self.f.op("act", lambda e: e.activation(out=out, in_=in_, func=AF.Copy), reads, writes)
        else:
            self.f.op(eng, lambda e: e.tensor_copy(out=out, in_=in_), reads, writes)

    def recip(self, out, in_, reads, writes):
        self.f.op("dve", lambda e: e.reciprocal(out=out, in_=in_), reads, writes)

    def dma(self, out, in_, reads, writes, q="sp"):
        self.f.dma(q, out, in_, reads, writes)

    def setup(self):
        nc = self.nc
        self.psum = []
        for i in range(8):
            t = self.gst.enter_context(nc.psum_tensor("ps%d" % i, [128, 512], F32))
            self.psum.append((t, Tok(excl=True)))
        self.vec = self.gsb([128, NV], F32, "vec")
        self.vec_k = Tok()
        self.dma(self.vec[:], self.d["vecs"], [], [self.vec_k])
        self.cst = self.gsb([128, 256], F32, "cst")
        self.cst_k = Tok()
        self.dma(self.cst[:], self.d["consts"][:, 0:256], [], [self.cst_k])
        self.cb = self.gsb([128, 256], BF16, "cstb")
        self.cb_k = Tok()
        self.cp("dve", self.cb[:], self.cst[:], [self.cst_k], [self.cb_k])
        self.ident_f = self.cst[:, C_IDENT:C_IDENT + 128]
        self.ones_f = self.cst[:, C_ONES:C_ONES + 128]
        self.ident_b = self.cb[:, C_IDENT:C_IDENT + 128]
        self.ones_b = self.cb[:, C_ONES:C_ONES + 128]

    def vcol(self, key, c=0, rows=128):
        o = VOFF[key] + c
        return self.vec[0:rows, o:o + 1]

    def lconst(self, c0, n, bf16=False):
        t = self.sb([128, n], F32, "lc")
        k = Tok()
        self.dma(t[:], self.d["consts"][:, c0:c0 + n], [], [k])
        if not bf16:
            return t, k
        tb = self.sb([128, n], BF16, "lcb")
        kb = Tok()
        self.cp("pool", tb[:], t[:], [k], [kb])
        return tb, kb

    def begin_phase(self):
        self.pst = ExitStack()

    def end_phase(self):
        self.f.barrier()
        self.pst.close()
        self.pst = None

    def load_norm(self, tt, xstage, sqring, xb, xb_k, rstd, rstd_k, ssb):
        ss, ss_k = ssb
        for c in range(8):
            xs, xs_k = xstage.next()
            self.dma(xs[:], self.xsrc[c * 128:(c + 1) * 128, tt * T:(tt + 1) * T], [self.xtok[tt][c]], [xs_k])
            sq, sq_k = sqring.next()
            self.act(sq[:], xs[:], AF.Square, [xs_k], [sq_k])
            self.cp("pool", xb[:, c, :], xs[:], [xs_k], [xb_k])
            self.mm(ss[:], self.ones_b, sq[:], c == 0, c == 7, [sq_k, self.cb_k], [ss_k])
        self.act(rstd[:], ss[:], AF.Sqrt, [ss_k], [rstd_k], scale=1.0 / D, bias=EPS)
        self.recip(rstd[:], rstd[:], [rstd_k], [rstd_k])

    def load_w(self, dst, dst_k, src, stage, gain, eng_i):
        st, st_k = stage.next()
        n = src.shape[-1]
        self.dma(st[:, 0:n], src, [], [st_k])
        eng = ("dve", "pool", "act")[eng_i % 3] if gain is None else ("dve", "act")[eng_i % 2]
        rd = [st_k, self.vec_k]
        if gain is None:
            self.cp(eng, dst, st[:, 0:n], rd, [dst_k])
        elif eng == "act":
            self.act(dst, st[:, 0:n], AF.Copy, rd, [dst_k], scale=gain)
        else:
            self.ts(eng, dst, st[:, 0:n], gain, ALU.mult, rd, [dst_k])


    def ffn_weights_begin(self, i, which):
        st = ExitStack()
        nc = self.nc
        wgu = st.enter_context(nc.sbuf_tensor(self._name("wgu"), [128, 8, 2 * DFF], BF16))
        wd = st.enter_context(nc.sbuf_tensor(self._name("wd"), [128, NJ, D], BF16))
        stage = Ring([(st.enter_context(nc.sbuf_tensor(self._name("wst"), [128, 1408], F32)), Tok()) for _ in range(2)])
        wgu_k = [[Tok() for _ in range(4)] for _ in range(8)]
        wd_k = [Tok() for _ in range(NJ)]
        Wgu = self.d["ffn%d_w_gu" % which][i]
        Wd = self.d["ffn%d_w_down" % which][i]
        gkey = ("ln_ffn%d" % which, i)
        tasks = []
        n = 0
        for c in range(8):
            for pc in range(4):
                tasks.append((wgu[:, c, pc * 1408:(pc + 1) * 1408], wgu_k[c][pc], Wgu[c * 128:(c + 1) * 128, pc * 1408:(pc + 1) * 1408], self.vcol(gkey, c), n))
                n += 1
        for j in range(NJ):
            tasks.append((wd[:, j, :], wd_k[j], Wd[j * 128:(j + 1) * 128, :], None, n))
            n += 1
        self.pref = {"key": (i, which), "st": st, "wgu": wgu, "wd": wd, "wgu_k": wgu_k, "wd_k": wd_k, "stage": stage, "tasks": tasks}

    def pref_step(self, n=None):
        if not self.pref:
            return
        tasks = self.pref["tasks"]
        k = len(tasks) if n is None else min(n, len(tasks))
        for _ in range(k):
            dst, dst_k, src, gain, idx = tasks.pop(0)
            self.load_w(dst, dst_k, src, self.pref["stage"], gain, idx)

    def next_ffn(self, i, which):
        return (i, which)

    def phase_ffn(self, i, which):
        if not (self.pref and self.pref["key"] == (i, which)):
            assert not self.pref
            self.ffn_weights_begin(i, which)
        self.begin_phase()
        pf = self.pref
        wgu, wgu_k, wd, wd_k = pf["wgu"], pf["wgu_k"], pf["wd"], pf["wd_k"]
        xstage = self.ring(4, [128, T], F32, "xst")
        sqring = self.ring(2, [128, T], BF16, "sq")
        xbs = [(self.sb([128, 8, T], BF16, "xb"), Tok())] * 2
        rstds = [(self.sb([128, T], F32, "rstd"), Tok())] * 2
        actb = self.sb([128, NJ, T], BF16, "actb")
        act_k = [Tok() for _ in range(NJ)]
        aring = self.ring(3, [128, T], F32, "A")
        oring = self.ring(3, [128, T], F32, "ob")
        ssb = self.psum[0]
        gps = [self.psum[1], self.psum[2]]
        ups = [self.psum[3], self.psum[4]]
        yps = [self.psum[5], self.psum[6]]
        self.pref_step()
        self.load_norm(0, xstage, sqring, xbs[0][0], xbs[0][1], rstds[0][0], rstds[0][1], ssb)
        for tt in range(NT):
            xb, xb_k = xbs[tt % 2]
            rstd, rstd_k = rstds[tt % 2]
            for j in range(NJ):
                gp, gp_k = gps[j % 2]
                up, up_k = ups[j % 2]
                for half, (pp, pp_k) in enumerate(((gp, gp_k), (up, up_k))):
                    col = half * DFF + j * 128
                    pc = col // 1408
                    assert (col + 127) // 1408 == pc
                    for c in range(8):
                        self.mm(pp[:], wgu[:, c, col:col + 128], xb[:, c, :], c == 0, c == 7,
                                [wgu_k[c][pc], xb_k], [pp_k])
                A, A_k = aring.next()
                self.tt("dve", A[:], gp[:], rstd[:], ALU.mult, [gp_k, rstd_k], [A_k])
                self.act(A[:], A[:], AF.Silu, [A_k], [A_k])
                self.tt("pool", A[:], A[:], rstd[:], ALU.mult, [A_k, rstd_k], [A_k])
                self.tt("dve", actb[:, j, :], up[:], A[:], ALU.mult, [up_k, A_k], [act_k[j]])
            if tt + 1 < NT:
                nb = xbs[(tt + 1) % 2]
                nr = rstds[(tt + 1) % 2]
                self.load_norm(tt + 1, xstage, sqring, nb[0], nb[1], nr[0], nr[1], ssb)
            for m in range(8):
                yp, yp_k = yps[m % 2]
                for j in range(NJ):
                    self.mm(yp[:], wd[:, j, m * 128:(m + 1) * 128], actb[:, j, :], j == 0, j == NJ - 1,
                            [wd_k[j], act_k[j]], [yp_k])
                xs, xs_k = xstage.next()
                self.dma(xs[:], self.xsrc[m * 128:(m + 1) * 128, tt * T:(tt + 1) * T], [self.xtok[tt][m]], [xs_k])
                ob, ob_k = oring.next()
                self.stt(ob[:], yp[:], 0.5, xs[:], ALU.mult, ALU.add, [yp_k, xs_k], [ob_k])
                self.dma(self.xres[m * 128:(m + 1) * 128, tt * T:(tt + 1) * T], ob[:], [ob_k], [self.xtok[tt][m]], q="act")
        self.end_phase()
        self.pref["st"].close()
        self.pref = None
        self.xsrc = self.xres

    def phase_ple(self, i):
        if i + 1 < DEPTH and ("ffn", i + 1, 1) in self.phases:
            self.ffn_weights_begin(i + 1, 1)
        self.begin_phase()
        Wg = self.d["ple_w_gate"][i]
        Wp = self.d["ple_w_in"][i]
        gkey = ("ln_ple", i)
        wg = self.sb([128, 8, D], BF16, "wg")
        wg_k = [Tok() for _ in range(8)]
        wp = self.sb([128, 2, D], BF16, "wp")
        wp_k = [Tok() for _ in range(2)]
        wstage = self.pref["stage"] if self.pref else self.ring(2, [128, D], F32, "wst")
        xstage = self.ring(3, [128, T], F32, "xst")
        sqring = self.ring(2, [128, T], BF16, "sq")
        xbs = [(self.sb([128, 8, T], BF16, "xb"), Tok()) for _ in range(2)]
        rstds = [(self.sb([128, T], F32, "rstd"), Tok()) for _ in range(2)]
        pstage = self.ring(1, [128, T], F32, "pst")
        pbs = [(self.sb([128, 2, T], BF16, "pb"), Tok()) for _ in range(2)]
        aring = self.ring(2, [128, T], F32, "A")
        oring = self.ring(2, [128, T], F32, "ob")
        ssb = self.psum[0]
        gps = [self.psum[1], self.psum[2]]
        eps_ = [self.psum[3], self.psum[4]]
        for c in range(8):
            self.load_w(wg[:, c, :], wg_k[c], Wg[c * 128:(c + 1) * 128, :], wstage, self.vcol(gkey, c), c)
        for c in range(2):
            self.load_w(wp[:, c, :], wp_k[c], Wp[c * 128:(c + 1) * 128, :], wstage, None, c)

        def load_p(tt):
            pb, pb_k = pbs[tt % 2]
            for c in range(2):
                st, st_k = pstage.next()
                self.dma(st[:], self.d["pT"][i, c * 128:(c + 1) * 128, tt * T:(tt + 1) * T], [], [st_k])
                self.cp("pool", pb[:, c, :], st[:], [st_k], [pb_k])

        pend_st = []
        self.load_norm(0, xstage, sqring, xbs[0][0], xbs[0][1], rstds[0][0], rstds[0][1], ssb)
        load_p(0)
        for tt in range(NT):
            xb, xb_k = xbs[tt % 2]
            rstd, rstd_k = rstds[tt % 2]
            pb, pb_k = pbs[tt % 2]
            if tt + 1 < NT:
                nb = xbs[(tt + 1) % 2]
                nr = rstds[(tt + 1) % 2]
                self.load_norm(tt + 1, xstage, sqring, nb[0], nb[1], nr[0], nr[1], ssb)
                load_p(tt + 1)
            for m in range(8):
                gp, gp_k = gps[m % 2]
                ep, ep_k = eps_[m % 2]
                for c in range(8):
                    self.mm(gp[:], wg[:, c, m * 128:(m + 1) * 128], xb[:, c, :], c == 0, c == 7, [wg_k[c], xb_k], [gp_k])
                for c in range(2):
                    self.mm(ep[:], wp[:, c, m * 128:(m + 1) * 128], pb[:, c, :], c == 0, c == 1, [wp_k[c], pb_k], [ep_k])
                A, A_k = aring.next()
                self.tt("dve", A[:], gp[:], rstd[:], ALU.mult, [gp_k, rstd_k], [A_k])
                self.act(A[:], A[:], AF.Sigmoid, [A_k], [A_k])
                self.tt("dve", A[:], ep[:], A[:], ALU.mult, [ep_k, A_k], [A_k])
                xs, xs_k = xstage.next()
                self.dma(xs[:], self.xsrc[m * 128:(m + 1) * 128, tt * T:(tt + 1) * T], [self.xtok[tt][m]], [xs_k])
                ob, ob_k = oring.next()
                self.tt("pool", ob[:], A[:], xs[:], ALU.add, [A_k, xs_k], [ob_k])
                pend_st.append((self.xres[m * 128:(m + 1) * 128, tt * T:(tt + 1) * T], ob, ob_k, self.xtok[tt][m]))
                if len(pend_st) > 1:
                    d_, ob_, obk_, xk_ = pend_st.pop(0)
                    self.dma(d_, ob_[:], [obk_], [xk_], q="act")
            self.pref_step(7)
        while pend_st:
            d_, ob_, obk_, xk_ = pend_st.pop(0)
            self.dma(d_, ob_[:], [obk_], [xk_], q="act")
        self.pref_step()
        self.end_phase()
        self.xsrc = self.xres


    def load_hn(self, tt, xstage, sqring, xb, xb_k, rstd, rstd_k, ssb, hn, hn_k):
        self.load_norm(tt, xstage, sqring, xb, xb_k, rstd, rstd_k, ssb)
        for c in range(8):
            self.tt(("pool", "dve")[c % 2], hn[:, c, :], xb[:, c, :], rstd[:], ALU.mult, [xb_k, rstd_k], [hn_k])

    def head_norm(self, src, src_k, rows, gaincol, div, sqring, ssring, rring, dst, dst_k):
        sq, sq_k = sqring.next()
        self.act(sq[0:rows, :], src, AF.Square, [src_k], [sq_k])
        ss, ss_k = ssring.next()
        self.mm(ss[0:rows, :], self.ones_b[0:rows, 0:rows], sq[0:rows, :], True, True, [sq_k, self.cb_k], [ss_k])
        r, r_k = rring.next()
        self.act(r[0:rows, :], ss[0:rows, :], AF.Sqrt, [ss_k], [r_k], scale=1.0 / div, bias=EPS)
        self.recip(r[0:rows, :], r[0:rows, :], [r_k], [r_k])
        self.stt(dst, src, gaincol, r[0:rows, :], ALU.mult, ALU.mult, [src_k, r_k, self.vec_k], [dst_k])

    def phase_odd_proj(self, i):
        j = i // 2
        self.begin_phase()
        W = self.d["odd_w_in"][j]
        NW = 2120
        win = self.sb([128, 8, NW + 128], BF16, "win")
        win_k = [Tok() for _ in range(8)]
        wstage = self.ring(2, [128, NW], F32, "wst")
        xstage = self.ring(4, [128, T], F32, "xst")
        sqring = self.ring(2, [128, T], BF16, "sq")
        xb, xb_k = self.sb([128, 8, T], BF16, "xb"), Tok()
        rstd, rstd_k = self.sb([128, T], F32, "rstd"), Tok()
        hns = [(self.sb([128, 8, T], BF16, "hn"), Tok()) for _ in range(2)]
        sq2 = self.ring(2, [128, T], BF16, "sq2")
        rring = self.ring(2, [128, T], F32, "rr")
        oring = self.ring(4, [128, T], BF16, "ob")
        wring = self.ring(2, [128, 8], F32, "wb")
        ssb = self.psum[0]
        pring = Ring([self.psum[1], self.psum[2], self.psum[3]])
        ssring = Ring([self.psum[4], self.psum[5]])
        sring = Ring([self.psum[6], self.psum[7]])
        for c in range(8):
            self.load_w(win[:, c, 0:NW], win_k[c], W[c * 128:(c + 1) * 128, :], wstage, self.vcol(("ln_mix", i), c), c)
            self.cp("pool", win[:, c, NW:NW + 64], win[:, c, 2048:2112], [win_k[c]], [win_k[c]])
            self.cp("pool", win[:, c, NW + 64:NW + 128], win[:, c, 2048:2112], [win_k[c]], [win_k[c]])
        sc = self.scr
        sk = self.scr_k
        self.load_hn(0, xstage, sqring, xb, xb_k, rstd, rstd_k, ssb, hns[0][0], hns[0][1])
        for tt in range(NT):
            hn, hn_k = hns[tt % 2]
            if tt + 1 < NT:
                self.load_hn(tt + 1, xstage, sqring, xb, xb_k, rstd, rstd_k, ssb, hns[(tt + 1) % 2][0], hns[(tt + 1) % 2][1])
            tsl = slice(tt * T, (tt + 1) * T)

            def proj(col, width=128):
                pp, pp_k = pring.next()
                for c in range(8):
                    self.mm(pp[0:width, :], win[:, c, col:col + width], hn[:, c, :], c == 0, c == 7, [win_k[c], hn_k], [pp_k])
                return pp, pp_k
            for h in range(8):
                pp, pp_k = proj(h * 128)
                ob, ob_k = oring.next()
                self.head_norm(pp[:], pp_k, 128, self.vcol(("dsa_q_norm", j)), 128.0, sq2, ssring, rring, ob[:], ob_k)
                self.dma(sc["dsa_qT"][h, :, tsl], ob[:], [ob_k], [sk["dsa_qT"][tt]])
            for n in range(2):
                pp, pp_k = proj(1024 + n * 128)
                ob, ob_k = oring.next()
                self.head_norm(pp[:], pp_k, 128, self.vcol(("dsa_k_norm", j)), 128.0, sq2, ssring, rring, ob[:], ob_k)
                self.dma(sc["dsa_kT"][n, :, tsl], ob[:], [ob_k], [sk["dsa_kT"][tt]])
            pp, pp_k = proj(NW)
            ob, ob_k = oring.next()
            self.head_norm(pp[:], pp_k, 128, self.vcol(("dsa_kidx_norm", j)), 128.0, sq2, ssring, rring, ob[:], ob_k)
            self.dma(sc["dsa_kiT"][:, tsl], ob[:], [ob_k], [sk["dsa_kiT"][tt]])
            for c4 in range(4):
                pp, pp_k = proj(1536 + c4 * 128)
                ob, ob_k = oring.next()
                self.cp("act", ob[:], pp[:], [pp_k], [ob_k])
                self.dma(sc["dsa_qiT"][c4, :, tsl], ob[:], [ob_k], [sk["dsa_qiT"][tt]])
            for sub in range(4):
                sp_, sp_k = sring.next()
                for c in range(8):
                    self.mm(sp_[:, 0:256], hn[:, c, sub * 128:(sub + 1) * 128], win[:, c, 1280:1536], c == 0, c == 7, [win_k[c], hn_k], [sp_k])
                ob, ob_k = oring.next()
                self.cp("act", ob[:, 0:256], sp_[:, 0:256], [sp_k], [ob_k])
                r0 = tt * T + sub * 128
                self.dma(sc["dsa_v"][r0:r0 + 128, :], ob[:, 0:256], [ob_k], [sk["dsa_v"][tt]])
                sp_, sp_k = sring.next()
                for c in range(8):
                    self.mm(sp_[:, 0:8], hn[:, c, sub * 128:(sub + 1) * 128], win[:, c, 2112:2120], c == 0, c == 7, [win_k[c], hn_k], [sp_k])
                wb, wb_k = wring.next()
                self.cp("dve", wb[:], sp_[:, 0:8], [sp_k], [wb_k])
                self.dma(sc["dsa_w"][r0:r0 + 128, :], wb[:], [wb_k], [sk["dsa_w"][tt]])
        self.end_phase()

    def phase_dsa(self, i, nq=32):
        j = i // 2
        self.begin_phase()
        sc = self.scr
        REPL = 2.0 * NEGBIG
        kT, kT_k = self.sb([128, 2, S], BF16, "kT"), Tok()
        V, V_k = self.sb([128, 32, 256], BF16, "V"), Tok()
        kiT, kiT_k = self.sb([128, S], BF16, "kiT"), Tok()
        for n in range(2):
            self.dma(kT[:, n, :], sc["dsa_kT"][n], [], [kT_k])
        for kq in range(4):
            self.dma(V[:, kq * 8:(kq + 1) * 8, :], sc["dsa_v"][kq * 1024:(kq + 1) * 1024, :].rearrange("(kt p) d -> p kt d", p=128), [], [V_k])
        self.dma(kiT[:], sc["dsa_kiT"], [], [kiT_k])
        scbs = [(self.sb([128, S], F32, "scb"), [Tok(hard=True) for _ in range(8)]) for _ in range(4)]
        mbs = [(self.sb([128, S], BF16, "mb"), Tok()) for _ in range(4)]
        bss = [(self.sb([128, 8], F32, "bs"), Tok(hard=True)) for _ in range(4)]
        qring = self.ring(4, [128, 8, 128], BF16, "q")
        qiring = self.ring(4, [128, 4, 128], BF16, "qi")
        wring = Ring([(self.sb([128, 8], F32, "w"), Tok()) for _ in range(4)])
        mTring = self.ring(2, [128, 32, 128], BF16, "mT")
        rring = self.ring(3, [128, T], F32, "relu")
        junk, junk_k = self.sb([128, S], BF16, "junk"), Tok()
        ering = self.ring(4, [128, T], BF16, "e")
        pring = self.ring(4, [128, T], BF16, "pT")
        o32ring = self.ring(1, [128, T], F32, "o32")
        rdring = self.ring(1, [128, T], F32, "rd")
        oTring = self.ring(2, [128, 8, 128], BF16, "oT")
        ps_sc = Ring([self.psum[0], self.psum[1]])
        ps_st = Ring([self.psum[2], self.psum[3], self.psum[7]])
        ps_o = self.psum[4]
        ps_d = self.psum[5]
        ps_t = self.psum[6]
        ps_tb = ps_t[0][:].bitcast(BF16)
        negt, neg_k = self.lconst(C_NEG, 128)
        neg = negt[:, :]
        scale = 128.0 ** -0.5
        oT_d = sc["even_oT"].rearrange("(c p) t -> p c t", p=128)
        qbuf = {}

        def s1(qs):
            info = {}
            for qi in qs:
                L = (qi + 1) * 128
                qsl = slice(qi * 128, L)
                qi_sb, qi_k = qiring.next()
                self.dma(qi_sb[:], sc["dsa_qiT"][:, :, qsl].rearrange("h p q -> p h q"), [], [qi_k])
                w_sb, w_k = wring.next()
                self.dma(w_sb[:], sc["dsa_w"][qsl, :], [], [w_k])
                info[qi] = (L, qsl, qi_sb, qi_k, w_sb, w_k)
            for h in range(8):
                pb = (h % 2) * 64
                for qi in qs:
                    L, qsl, qi_sb, qi_k, w_sb, w_k = info[qi]
                    scb, sc_ks = scbs[qi % 4]
                    for st in range((L + 511) // 512):
                        w_ = min(512, L - st * 512)
                        seg = slice(st * 512, st * 512 + w_)
                        ps, ps_k = ps_sc.next()
                        self.mm(ps[:, 0:w_], qi_sb[pb:pb + 64, h // 2, :], kiT[pb:pb + 64, seg], True, True, [qi_k, kiT_k], [ps_k])
                        r, r_k = rring.next()
                        self.act(r[:, 0:w_], ps[:, 0:w_], AF.Relu, [ps_k], [r_k])
                        if h == 0:
                            self.ts("dve", scb[:, seg], r[:, 0:w_], w_sb[:, 0:1], ALU.mult, [r_k, w_k], [sc_ks[st]])
                        else:
                            self.stt(scb[:, seg], r[:, 0:w_], w_sb[:, h:h + 1], scb[:, seg], ALU.mult, ALU.add, [r_k, w_k, sc_ks[st]], [sc_ks[st]])
            for qi in qs:
                if qi >= 2:
                    bounds(qi)
            for qi in qs:
                L, qsl = info[qi][0], info[qi][1]
                scb, sc_ks = scbs[qi % 4]
                self.tt("pool", scb[:, qsl], scb[:, qsl], neg, ALU.add, [sc_ks[qi // 4], neg_k], [sc_ks[qi // 4]])

        def loadq(qs):
            for qi in qs:
                qsl = slice(qi * 128, (qi + 1) * 128)
                q_sb, q_k = qring.next()
                self.dma(q_sb[:], sc["dsa_qT"][:, :, qsl].rearrange("h p q -> p h q"), [], [q_k])
                qbuf[qi] = (q_sb, q_k)

        K_IT = 16

        def bounds(qi):
            L = (qi + 1) * 128
            scb, sc_ks = scbs[qi % 4]
            mb, mb_k = mbs[qi % 4]
            bs, bs_k = bss[qi % 4]
            self.f.op("dve", lambda e: e.tensor_scalar(out=junk[:, 0:L], in0=scb[:, 0:L], scalar1=0.0, scalar2=-3.0e38, op0=ALU.add, op1=ALU.max,
                                                        accum_out=bs[:, 1:2]), sc_ks, [junk_k, bs_k])
            self.f.op("dve", lambda e: e.tensor_scalar(out=junk[:, 0:L], in0=scb[:, 0:L], scalar1=0.0, scalar2=3.0e38, op0=ALU.add, op1=ALU.min,
                                                        accum_out=bs[:, 0:1]), sc_ks, [junk_k, bs_k])
            self.tt("dve", bs[:, 2:3], bs[:, 1:2], bs[:, 0:1], ALU.subtract, [bs_k], [bs_k])
            self.ts("dve", bs[:, 2:3], bs[:, 2:3], 0.5, ALU.mult, [bs_k], [bs_k])

        def topk(qs):
            qs = [q_ for q_ in qs if q_ >= 2]
            for it in range(K_IT):
                for q_ in qs:
                    L = (q_ + 1) * 128
                    scb, sc_ks = scbs[q_ % 4]
                    mb, mb_k = mbs[q_ % 4]
                    bs, bs_k = bss[q_ % 4]
                    self.tt("dve", bs[:, 3:4], bs[:, 0:1], bs[:, 2:3], ALU.add, [bs_k], [bs_k])
                    self.f.op("dve", lambda e, L=L, scb=scb, mb=mb, bs=bs: e.tensor_scalar(out=mb[:, 0:L], in0=scb[:, 0:L], scalar1=bs[:, 3:4], scalar2=0.0,
                                                                                          op0=ALU.is_ge, op1=ALU.add, accum_out=bs[:, 4:5]),
                              sc_ks + [bs_k], [mb_k, bs_k])
                for q_ in qs:
                    bs, bs_k = bss[q_ % 4]
                    self.stt(bs[:, 5:6], bs[:, 4:5], 256.0, bs[:, 2:3], ALU.is_ge, ALU.mult, [bs_k], [bs_k])
                    self.tt("dve", bs[:, 0:1], bs[:, 0:1], bs[:, 5:6], ALU.add, [bs_k], [bs_k])
                    self.ts("dve", bs[:, 2:3], bs[:, 2:3], 0.5, ALU.mult, [bs_k], [bs_k])

        def mask(qi):
            L = (qi + 1) * 128
            scb, sc_ks = scbs[qi % 4]
            mb, mb_k = mbs[qi % 4]
            bs, bs_k = bss[qi % 4]
            if qi >= 2:
                self.ts("dve", mb[:, 0:L], scb[:, 0:L], bs[:, 0:1], ALU.is_ge, sc_ks + [bs_k], [mb_k])
            else:
                self.ts("dve", mb[:, 0:L], scb[:, 0:L], 0.1 * NEGBIG, ALU.is_ge, sc_ks, [mb_k])

        def s3(qi):
            L = (qi + 1) * 128
            qsl = slice(qi * 128, L)
            q_sb, q_k = qbuf.pop(qi)
            mb, mb_k = mbs[qi % 4]
            mT, mT_k = mTring.next()
            for k0 in range(0, qi + 1, 4):
                nk = min(4, qi + 1 - k0)
                for kk in range(nk):
                    kt = k0 + kk
                    self.tr(ps_tb[:, kk * 128:(kk + 1) * 128], mb[:, kt * 128:(kt + 1) * 128], self.ident_b, [mb_k, self.cb_k], [ps_t[1]])
                self.cp("act", mT[:, k0:k0 + nk, :].rearrange("p k q -> p (k q)"), ps_tb[:, 0:nk * 128], [ps_t[1]], [mT_k])
            oT, oT_k = oTring.next()
            for n in range(2):
                pend = []

                def stage_a(kt):
                    ps, ps_k = ps_st.next()
                    self.mm(ps[:], kT[:, n, kt * 128:(kt + 1) * 128], q_sb[:, 4 * n:4 * n + 4, :].rearrange("p h q -> p (h q)"), True, True, [kT_k, q_k], [ps_k])
                    e, e_k = ering.next()
                    self.act(e[:], ps[:], AF.Exp, [ps_k], [e_k], scale=scale)
                    pT, pT_k = pring.next()
                    self.tt("pool", pT[:].rearrange("p (h q) -> p h q", h=4), e[:].rearrange("p (h q) -> p h q", h=4),
                            mT[:, kt:kt + 1, :].to_broadcast([128, 4, 128]), ALU.mult, [e_k, mT_k], [pT_k])
                    pend.append((kt, pT, pT_k))

                def stage_b():
                    kt, pT, pT_k = pend.pop(0)
                    self.mm(ps_o[0][:], V[:, kt, n * 128:(n + 1) * 128], pT[:], kt == 0, kt == qi, [V_k, pT_k], [ps_o[1]])
                    self.mm(ps_d[0][:], self.ones_b, pT[:], kt == 0, kt == qi, [self.cb_k, pT_k], [ps_d[1]])

                for kt in range(qi + 1):
                    stage_a(kt)
                    if len(pend) > 2:
                        stage_b()
                while pend:
                    stage_b()
                rd, rd_k = rdring.next()
                self.act(rd[:], ps_d[0][:], AF.Ln, [ps_d[1]], [rd_k])
                self.act(rd[:], rd[:], AF.Exp, [rd_k], [rd_k], scale=-1.0)
                o32, o32_k = o32ring.next()
                self.cp("act", o32[:], ps_o[0][:], [ps_o[1]], [o32_k])
                self.tt("pool", oT[:, 4 * n:4 * n + 4, :].rearrange("p h q -> p (h q)"), o32[:], rd[:], ALU.mult, [o32_k, rd_k], [oT_k])
            self.dma(oT_d[:, :, qsl], oT[:], [oT_k], [self.scr_k["even_oT"][qi // 4]])

        nquad = (nq + 3) // 4

        def quad(g):
            return [q_ for q_ in range(4 * g, 4 * g + 4) if q_ < nq]

        s1(quad(0))
        for g in range(nquad):
            qs = quad(g)
            loadq(qs)
            topk(qs)
            for q_ in qs:
                mask(q_)
            if g + 1 < nquad:
                s1(quad(g + 1))
            for q_ in qs:
                s3(q_)
        self.end_phase()

    def phase_even_proj(self, i, do_gdn=True):
        j = i // 2
        self.begin_phase()
        W = self.d["even_w_in"][j]
        NW = 2728
        win = self.sb([128, 8, NW], BF16, "win")
        win_k = [Tok() for _ in range(8)]
        wstage = self.ring(2, [128, NW // 2], F32, "wst")
        wq = self.sb([128, 3, 768], BF16, "wq"); wq_k = [Tok() for _ in range(3)]
        wkv = self.sb([128, 2, 1024], BF16, "wkv"); wkv_k = [Tok() for _ in range(2)]
        wv = self.sb([128, 2, 512], BF16, "wv")
        xstage = self.ring(3, [128, T], F32, "xst")
        sqring = self.ring(2, [128, T], BF16, "sq")
        xb, xb_k = self.sb([128, 8, T], BF16, "xb"), Tok()
        rstd, rstd_k = self.sb([128, T], F32, "rstd"), Tok()
        hns = [(self.sb([128, 8, T], BF16, "hn"), Tok()) for _ in range(2)]
        sq2 = self.ring(4, [128, T], BF16, "sq2")
        rring = self.ring(4, [128, T], F32, "rr")
        rawring = self.ring(5, [128, T], F32, "raw")
        cq_raw, cq_k = self.sb([128, 3, T], F32, "cqraw"), Tok()
        cqn, cqn_k = self.sb([128, 3, T], BF16, "cqn"), Tok()
        ckvn, ckvn_k = self.sb([128, 2, T], BF16, "ckvn"), Tok()
        kpe, kpe_k = self.sb([128, T], F32, "kpe"), Tok()
        ropes = [(self.sb([96, 2, T], F32, "rope"), Tok()) for _ in range(2)]
        nb_ring = self.ring(4, [96, T], BF16, "nb")
        t1_ring = self.ring(4, [96, T], F32, "t1")
        fin_ring = self.ring(4, [96, T], BF16, "fin")
        vext_ring = self.ring(2, [128, 8, 128], BF16, "vext")
        f32o = self.ring(4, [128, T], F32, "f32o")
        gbring = self.ring(2, [128, 8], F32, "gb")
        tmp4 = self.ring(2, [128, 4], F32, "tmp4")
        ssb = self.psum[0]
        pring = Ring([self.psum[1], self.psum[2], self.psum[3]])
        ssring = Ring([self.psum[4], self.psum[5], self.psum[6]])
        sring = Ring([self.psum[7]])
        pmt, pm_k = self.lconst(C_PM, 96, True)
        pm_b = pmt[0:96, 0:96]
        for c in range(8):
            for hf in range(2):
                self.load_w(win[:, c, hf * (NW // 2):(hf + 1) * (NW // 2)], win_k[c], W[c * 128:(c + 1) * 128, hf * (NW // 2):(hf + 1) * (NW // 2)], wstage, self.vcol(("ln_mix", i), c), 2 * c + hf)
        for c in range(3):
            self.load_w(wq[:, c, :], wq_k[c], self.d["mla_w_q_up"][j][c * 128:(c + 1) * 128, :], wstage, self.vcol(("mla_q_a_norm", j), c), c)
        for c in range(2):
            self.load_w(wkv[:, c, :], wkv_k[c], self.d["mla_w_kv_up"][j][c * 128:(c + 1) * 128, :], wstage, self.vcol(("mla_kv_a_norm", j), c), c)
            self.cp("pool", wv[:, c, :].rearrange("p (h d) -> p h d", d=64),
                    wkv[:, c, :].rearrange("p (h two d) -> p h two d", two=2, d=64)[:, :, 1, :], [wkv_k[c]], [wkv_k[c]])
        for (vx, vx_k) in vext_ring.items:
            self.f.op("pool", lambda e, vx=vx: e.memset(vx[:], 1.0), [], [vx_k])
        if do_gdn:
            convbuf, conv_k = self.sb([128, 12, 3 + T], F32, "convbuf"), [Tok() for _ in range(12)]
            self.f.op("pool", lambda e: e.memset(convbuf[:, :, 0:3], 0.0), [], conv_k)
            yring = self.ring(5, [128, T], F32, "y")
            negA, negA_k = self.sb([128, 4], F32, "negA"), Tok(hard=True)
            dtb, dtb_k = self.sb([128, 4], F32, "dtb"), Tok()
            self.dma(negA[:], self.d["gdn_a_log"][j:j + 1, :].partition_broadcast(128), [], [negA_k])
            self.dma(dtb[:], self.d["gdn_dt_bias"][j:j + 1, :].partition_broadcast(128), [], [dtb_k])
            self.act(negA[:], negA[:], AF.Exp, [negA_k], [negA_k])
            self.ts("dve", negA[:], negA[:], -1.0, ALU.mult, [negA_k], [negA_k])
        sc = self.scr
        sk = self.scr_k

        def load_rope(tt):
            rp, rp_k = ropes[tt % 2]
            self.dma(rp[:], self.d["rope"][:, :, tt * T:(tt + 1) * T].rearrange("a p t -> p a t"), [], [rp_k])

        self.load_hn(0, xstage, sqring, xb, xb_k, rstd, rstd_k, ssb, hns[0][0], hns[0][1])
        load_rope(0)
        for tt in range(NT):
            hn, hn_k = hns[tt % 2]
            rp, rp_k = ropes[tt % 2]
            if tt + 1 < NT:
                self.load_hn(tt + 1, xstage, sqring, xb, xb_k, rstd, rstd_k, ssb, hns[(tt + 1) % 2][0], hns[(tt + 1) % 2][1])
                load_rope(tt + 1)
            tsl = slice(tt * T, (tt + 1) * T)

            def proj(col, width=128, pbase=0):
                pp, pp_k = pring.next()
                for c in range(8):
                    self.mm(pp[pbase:pbase + width, :], win[:, c, col:col + width], hn[:, c, :], c == 0, c == 7, [win_k[c], hn_k], [pp_k])
                return pp, pp_k

            def latent(col0, nch, raw, raw_k, dst, dst_k):
                ss, ss_k = ssring.next()
                for c in range(nch):
                    pp, pp_k = proj(col0 + c * 128)
                    self.cp("act", raw[:, c, :], pp[:], [pp_k], [raw_k])
                    sq, sq_k = sq2.next()
                    self.act(sq[:], pp[:], AF.Square, [pp_k], [sq_k])
                    self.mm(ss[:], self.ones_b, sq[:], c == 0, c == nch - 1, [sq_k, self.cb_k], [ss_k])
                r, r_k = rring.next()
                self.act(r[:], ss[:], AF.Sqrt, [ss_k], [r_k], scale=1.0 / (nch * 128), bias=EPS)
                self.recip(r[:], r[:], [r_k], [r_k])
                for c in range(nch):
                    self.tt(("dve", "pool")[c % 2], dst[:, c, :], raw[:, c, :], r[:], ALU.mult, [raw_k, r_k], [dst_k])

            latent(0, 3, cq_raw, cq_k, cqn, cqn_k)
            latent(384, 2, cq_raw, cq_k, ckvn, ckvn_k)
            pp, pp_k = proj(640, 32, 64)
            self.cp("act", kpe[64:96, :], pp[64:96, :], [pp_k], [kpe_k])

            def norm_rope_group(items, gkey):
                n_ = len(items)
                sqs, rs, nbs, rots, t1s, fins = [], [], [], [], [], []
                for (raw, raw_k, dst, dst_tok) in items:
                    sq, sq_k = sq2.next()
                    self.act(sq[0:96, :], raw[0:96, :], AF.Square, [raw_k], [sq_k])
                    sqs.append((sq, sq_k))
                for g_ in range(n_):
                    sq, sq_k = sqs[g_]
                    ss, ss_k = ssring.next()
                    self.mm(ss[0:96, :], self.ones_b[0:96, 0:96], sq[0:96, :], True, True, [sq_k, self.cb_k], [ss_k])
                    r, r_k = rring.next()
                    self.act(r[0:96, :], ss[0:96, :], AF.Sqrt, [ss_k], [r_k], scale=1.0 / 96, bias=EPS)
                    rs.append((r, r_k))
                for g_ in range(n_):
                    r, r_k = rs[g_]
                    self.recip(r[0:96, :], r[0:96, :], [r_k], [r_k])
                for g_ in range(n_):
                    raw, raw_k = items[g_][0], items[g_][1]
                    r, r_k = rs[g_]
                    nb, nb_k = nb_ring.next()
                    self.stt(nb[:], raw[0:96, :], self.vcol(gkey, 0, 96), r[0:96, :], ALU.mult, ALU.mult, [raw_k, r_k, self.vec_k], [nb_k])
                    nbs.append((nb, nb_k))
                for g_ in range(n_):
                    nb, nb_k = nbs[g_]
                    rot, rot_k = ssring.next()
                    self.mm(rot[0:96, :], pm_b, nb[:], True, True, [nb_k, pm_k], [rot_k])
                    rots.append((rot, rot_k))
                    t1, t1_k = t1_ring.next()
                    self.tt("pool", t1[:], nb[:], rp[:, 0, :], ALU.mult, [nb_k, rp_k], [t1_k])
                    t1s.append((t1, t1_k))
                for g_ in range(n_):
                    rot, rot_k = rots[g_]
                    fin, fin_k = fin_ring.next()
                    self.tt("dve", fin[:], rot[0:96, :], rp[:, 1, :], ALU.mult, [rot_k, rp_k], [fin_k])
                    fins.append((fin, fin_k))
                for g_ in range(n_):
                    fin, fin_k = fins[g_]
                    t1, t1_k = t1s[g_]
                    self.tt("pool", fin[:], fin[:], t1[:], ALU.add, [fin_k, t1_k], [fin_k])
                    self.dma(items[g_][2], fin[:], [fin_k], [items[g_][3]])

            for hg in ((0, 1, 2), (3, 4, 5), (6, 7)):
                items = []
                for h in hg:
                    pp, pp_k = pring.next()
                    for c in range(3):
                        self.mm(pp[0:96, :], wq[:, c, h * 96:(h + 1) * 96], cqn[:, c, :], c == 0, c == 2, [wq_k[c], cqn_k], [pp_k])
                    raw, raw_k = rawring.next()
                    self.cp("act", raw[0:96, :], pp[0:96, :], [pp_k], [raw_k])
                    items.append((raw, raw_k, sc["mla_qT"][h, :, tsl], sk["mla_qT"][tt]))
                norm_rope_group(items, ("mla_q_norm", j))
            for hg in ((0, 1, 2), (3, 4, 5), (6, 7)):
                items = []
                for h in hg:
                    pp, pp_k = pring.next()
                    for c in range(2):
                        self.mm(pp[0:64, :], wkv[:, c, h * 128:h * 128 + 64], ckvn[:, c, :], c == 0, c == 1, [wkv_k[c], ckvn_k], [pp_k])
                    raw, raw_k = rawring.next()
                    self.cp("act", raw[0:64, :], pp[0:64, :], [pp_k], [raw_k])
                    self.cp("pool", raw[64:96, :], kpe[64:96, :], [kpe_k], [raw_k])
                    items.append((raw, raw_k, sc["mla_kT"][h, :, tsl], sk["mla_kT"][tt]))
                norm_rope_group(items, ("mla_k_norm", j))
            for sub in range(4):
                sp_, sp_k = sring.next()
                for c in range(2):
                    self.mm(sp_[:], ckvn[:, c, sub * 128:(sub + 1) * 128], wv[:, c, :], c == 0, c == 1, [ckvn_k, wkv_k[c]], [sp_k])
                vx, vx_k = vext_ring.next()
                self.cp("act", vx[:, :, 0:64], sp_[:].rearrange("p (h d) -> p h d", d=64), [sp_k], [vx_k])
                r0 = tt * T + sub * 128
                self.dma(sc["mla_vext"][:, r0:r0 + 128, :].rearrange("h t d -> t h d"), vx[:], [vx_k], [sk["mla_vext"][tt]])
            if not do_gdn:
                continue
            cw = VOFF[("gdn_conv_w", j)]
            for g0 in range(0, 12, 4):
                grp = list(range(g0, g0 + 4))
                ys = {}
                for c in grp:
                    pp, pp_k = proj(672 + c * 128)
                    self.cp("act", convbuf[:, c, 3:3 + T], pp[:], [pp_k], [conv_k[c]])
                    y, y_k = yring.next()
                    ys[c] = (y, y_k)
                    self.act(y[:], convbuf[:, c, 0:T], AF.Copy, [conv_k[c], self.vec_k], [y_k], scale=self.vec[:, cw + c * 4:cw + c * 4 + 1])
                for tap in range(1, 4):
                    for c in grp:
                        y, y_k = ys[c]
                        self.stt(y[:], convbuf[:, c, tap:tap + T], self.vec[:, cw + c * 4 + tap:cw + c * 4 + tap + 1], y[:], ALU.mult, ALU.add,
                                 [conv_k[c], y_k, self.vec_k], [y_k])
                for c in grp:
                    y, y_k = ys[c]
                    self.cp("pool", convbuf[:, c, 0:3], convbuf[:, c, T:T + 3], [conv_k[c]], [conv_k[c]])
                    self.act(y[:], y[:], AF.Silu, [y_k], [y_k])
                if g0 < 8:
                    st_ = {}
                    for c in grp:
                        y, y_k = ys[c]
                        sq, sq_k = sq2.next()
                        self.act(sq[:], y[:], AF.Square, [y_k], [sq_k])
                        st_[c] = [sq, sq_k]
                    for c in grp:
                        sq, sq_k = st_[c]
                        ss, ss_k = ssring.next()
                        self.mm(ss[:], self.ones_b, sq[:], True, True, [sq_k, self.cb_k], [ss_k])
                        r, r_k = rring.next()
                        self.act(r[:], ss[:], AF.Sqrt, [ss_k], [r_k], scale=1.0, bias=EPS)
                        st_[c] = [r, r_k]
                    for c in grp:
                        r, r_k = st_[c]
                        self.recip(r[:], r[:], [r_k], [r_k])
                    for c in grp:
                        y, y_k = ys[c]
                        r, r_k = st_[c]
                        ob, ob_k = f32o.next()
                        self.stt(ob[:], y[:], (128.0 ** -0.5) if c < 4 else 1.0, r[:], ALU.mult, ALU.mult, [y_k, r_k], [ob_k])
                        nm = "gdn_qT" if c < 4 else "gdn_kT"
                        self.dma(sc[nm][c % 4, :, tsl], ob[:], [ob_k], [sk[nm][tt]])
                else:
                    for c in grp:
                        y, y_k = ys[c]
                        self.dma(sc["gdn_vT"][c - 8, :, tsl], y[:], [y_k], [sk["gdn_vT"][tt]])
            for c in range(4):
                pp, pp_k = proj(2208 + c * 128)
                ob, ob_k = f32o.next()
                self.act(ob[:], pp[:], AF.Silu, [pp_k], [ob_k])
                self.dma(sc["gdn_zs"][c, :, tsl], ob[:], [ob_k], [sk["gdn_zs"][tt]])
            for sub in range(4):
                sp_, sp_k = sring.next()
                for c in range(8):
                    self.mm(sp_[:, 0:8], hn[:, c, sub * 128:(sub + 1) * 128], win[:, c, 2720:2728], c == 0, c == 7, [win_k[c], hn_k], [sp_k])
                gb, gb_k = gbring.next()
                t4, t4_k = tmp4.next()
                self.tt("dve", t4[:], sp_[:, 0:4], dtb[:], ALU.add, [sp_k, dtb_k], [t4_k])
                self.act(t4[:], t4[:], AF.Exp, [t4_k], [t4_k])
                self.act(t4[:], t4[:], AF.Ln, [t4_k], [t4_k], bias=1.0)
                self.tt("dve", gb[:, 0:4], t4[:], negA[:], ALU.mult, [t4_k, negA_k], [gb_k])
                self.act(gb[:, 4:8], sp_[:, 4:8], AF.Sigmoid, [sp_k], [gb_k])
                r0 = tt * T + sub * 128
                self.dma(sc["gdn_gb"][r0:r0 + 128, :], gb[:], [gb_k], [sk["gdn_gb"][tt]])
        self.end_phase()

    def phase_mla_attn(self, i, nqt=NT):
        self.begin_phase()
        sc = self.scr
        kTs = [(self.sb([96, S], BF16, "kT"), Tok()) for _ in range(2)]
        Vs = [(self.sb([128, 32, 128], BF16, "V"), Tok()) for _ in range(2)]
        qring = self.ring(2, [96, T], BF16, "q")
        ering = self.ring(7, [128, T], BF16, "e")
        rden, rden_k = self.sb([64, T], F32, "rden"), Tok()
        oring = self.ring(2, [64, T], BF16, "o")
        ps_st = Ring([self.psum[0], self.psum[1], self.psum[2], self.psum[5]])
        ps_o = Ring([self.psum[3], self.psum[4]])
        scale = 96.0 ** -0.5
        cmt, cm_k = self.lconst(C_CM, 2048, True)
        cm = cmt[:, :]

        def load_head(h):
            kT, kT_k = kTs[h % 2]
            self.dma(kT[:], sc["mla_kT"][h], [], [kT_k])
            V, V_k = Vs[h % 2]
            self.dma(V[:], sc["mla_vext"][h].rearrange("(kt p) d -> p kt d", p=128), [], [V_k])

        load_head(0)
        for h in range(8):
            if h + 1 < 8:
                load_head(h + 1)
            kT, kT_k = kTs[h % 2]
            V, V_k = Vs[h % 2]
            for qt in range(nqt):
                q, q_k = qring.next()
                self.dma(q[:], sc["mla_qT"][h, :, qt * T:(qt + 1) * T], [], [q_k])
                po, po_k = ps_o.next()
                nk = 4 * qt + 4
                pend = []
                for kt in range(nk):
                    ps, ps_k = ps_st.next()
                    self.mm(ps[:], kT[:, kt * 128:(kt + 1) * 128], q[:], True, True, [kT_k, q_k], [ps_k])
                    e, e_k = ering.next()
                    self.act(e[:], ps[:], AF.Exp, [ps_k], [e_k], scale=scale)
                    off = kt - 4 * qt
                    if off >= 0:
                        self.tt("pool", e[:], e[:], cm[:, off * 512:(off + 1) * 512], ALU.mult, [e_k, cm_k], [e_k])
                    pend.append((kt, e, e_k))
                    if len(pend) > 3:
                        kt2, e2, e2_k = pend.pop(0)
                        self.mm(po[:], V[:, kt2, :], e2[:], kt2 == 0, kt2 == nk - 1, [V_k, e2_k], [po_k])
                while pend:
                    kt2, e2, e2_k = pend.pop(0)
                    self.mm(po[:], V[:, kt2, :], e2[:], kt2 == 0, kt2 == nk - 1, [V_k, e2_k], [po_k])
                self.recip(rden[:], po[64:128, :], [po_k], [rden_k])
                o, o_k = oring.next()
                self.tt("dve", o[:], po[0:64, :], rden[:], ALU.mult, [po_k, rden_k], [o_k])
                self.dma(sc["even_oT"][h * 64:(h + 1) * 64, qt * T:(qt + 1) * T], o[:], [o_k], [self.scr_k["even_oT"][qt]])
        self.end_phase()

    def phase_even_out(self, i, ntt=NT, wname="even_w_out"):
        j = i // 2
        if ("ffn", i, 2) in self.phases:
            self.ffn_weights_begin(i, 2)
        self.begin_phase()
        wout = self.sb([128, 8, D], BF16, "wout")
        wout_k = [Tok() for _ in range(8)]
        wstage = self.ring(2, [128, D], F32, "wst")
        for c in range(8):
            self.load_w(wout[:, c, :], wout_k[c], self.d[wname][j][c * 128:(c + 1) * 128, :], wstage, None, c)
        oring = self.ring(2, [128, 8, T], BF16, "oT")
        xstage = self.ring(4, [128, T], F32, "xst")
        obring = self.ring(3, [128, T], F32, "ob")
        yps = Ring([self.psum[0], self.psum[1], self.psum[2]])
        oT_d = self.scr["even_oT"].rearrange("(c p) t -> p c t", p=128)
        for tt in range(ntt):
            tsl = slice(tt * T, (tt + 1) * T)
            o, o_k = oring.next()
            self.dma(o[:], oT_d[:, :, tsl], [], [o_k])
            for m in range(8):
                yp, yp_k = yps.next()
                for c in range(8):
                    self.mm(yp[:], wout[:, c, m * 128:(m + 1) * 128], o[:, c, :], c == 0, c == 7, [wout_k[c], o_k], [yp_k])
                xs, xs_k = xstage.next()
                self.dma(xs[:], self.xsrc[m * 128:(m + 1) * 128, tsl], [self.xtok[tt][m]], [xs_k])
                ob, ob_k = obring.next()
                self.tt("dve", ob[:], yp[:], xs[:], ALU.add, [yp_k, xs_k], [ob_k])
                self.dma(self.xres[m * 128:(m + 1) * 128, tsl], ob[:], [ob_k], [self.xtok[tt][m]], q="act")
            self.pref_step(7)
        self.pref_step()
        self.end_phase()

    def phase_gdn(self, i, nsc=32, stop=None):
        j = i // 2
        self.begin_phase()
        sc = self.scr
        gct, gc_k0 = self.lconst(C_MS, 386)
        MS = gct[:, 0:128]
        MI = gct[:, 128:256]
        UI = gct[:, 256:384]
        CIND = gct[:, 384:386]
        self.f.barrier()
        ck = self.cst_k

        def t4(name, n=1):
            return [(self.sb([128, 4, 128], F32, name), Tok()) for _ in range(n)]
        kTs = t4("kT", 2); qTs = t4("qT", 2); vTs = t4("vT", 2); zss = t4("zs", 2)
        gbs = [(self.sb([128, 8], F32, "gb"), Tok()) for _ in range(2)]
        (GU, GU_k), (gB, gB_k), (dec, dec_k), (dmI, dmI_k), (dmS, dmS_k) = t4("GU")[0], t4("gB")[0], t4("dec")[0], t4("dmI")[0], t4("dmS")[0]
        (qk, qk_k), (Nn, Nn_k), (qkT, qkT_k) = t4("qk")[0], t4("N")[0], t4("qkT")[0]
        Ps = t4("P", 2); Qs = t4("Q", 2)
        (X, X_k), (vb, vb_k), (kbg, kbg_k), (kd, kd_k) = t4("X")[0], t4("vb")[0], t4("kbg")[0], t4("kd")[0]
        (u, u_k), (wT, wT_k), (vnew, vnew_k), (o2s, o2s_k), (o, o_k) = t4("u")[0], t4("wT")[0], t4("vnew")[0], t4("o2s")[0], t4("o")[0]
        (oTs, oTs_k), (rr, rr_k), (fin32, fin32_k) = t4("oTs")[0], t4("rr")[0], t4("fin32")[0]
        sqb, sqb_k = self.sb([128, 4, 128], BF16, "sqb"), Tok()

        def b4(name):
            return self.sb([128, 4, 128], BF16, name), Tok()
        (qTb, qTb_k), (wTb, wTb_k), (Sb, Sb_k), (vnb, vnb_k), (qkTb, qkTb_k), (kdb, kdb_k) = b4("qTb"), b4("wTb"), b4("Sb"), b4("vnb"), b4("qkTb"), b4("kdb")
        self.f.op("pool", lambda e: e.memset(Sb[:], 0.0), [], [Sb_k])
        finb = self.ring(2, [128, 4, 128], BF16, "finb")
        Sst, S_k = self.sb([128, 4, 128], F32, "Sst"), Tok()
        self.f.op("pool", lambda e: e.memset(Sst[:], 0.0), [], [S_k])
        sm, sm_k = self.sb([128, 32], F32, "sm"), Tok(hard=True)
        B = self.psum
        oT_d = sc["even_oT"].rearrange("(c p) t -> p c t", p=128)

        def F(ap):
            return ap.rearrange("p h x -> p (h x)")

        def load(sci):
            t0 = sci * 128
            for nm, bufs in (("gdn_kT", kTs), ("gdn_qT", qTs), ("gdn_vT", vTs), ("gdn_zs", zss)):
                b, b_k = bufs[sci % 2]
                self.dma(b[:], sc[nm][:, :, t0:t0 + 128].rearrange("h d t -> d h t"), [], [b_k])
            gb, gb_k = gbs[sci % 2]
            self.dma(gb[:], sc["gdn_gb"][t0:t0 + 128, :], [], [gb_k])

        load(0)
        for sci in range(nsc):
            if sci + 1 < nsc:
                load(sci + 1)
            t0 = sci * 128
            kT, kT_k = kTs[sci % 2]; qT, qT_k = qTs[sci % 2]; vT, vT_k = vTs[sci % 2]; zs, zs_k = zss[sci % 2]
            gb, gb_k = gbs[sci % 2]
            for h in range(4):
                self.act(GU[:, h, :], UI, AF.Copy, [ck, gb_k], [GU_k], scale=gb[:, h:h + 1])
                self.act(gB[:, h, :], self.ones_f, AF.Copy, [ck, gb_k], [gB_k], scale=gb[:, h:h + 1])
            for h in range(4):
                self.mm(B[3][0][:, 2 * h:2 * h + 2], GU[:, h, :], self.ones_f[:, 0:2], True, True, [GU_k, ck], [B[3][1]])
                self.mm(B[3][0][:, 8 + 2 * h:10 + 2 * h], gB[:, h, :], CIND, True, True, [gB_k, ck], [B[3][1]])
                self.mm(B[0][0][:, h * 128:(h + 1) * 128], GU[:, h, :], MS, True, True, [GU_k, ck], [B[0][1]])
            self.cp("dve", sm[:, 0:4], B[3][0][:, 0:8].rearrange("p (h two) -> p h two", two=2)[:, :, 0], [B[3][1]], [sm_k])
            self.cp("dve", sm[:, 4:12], B[3][0][:, 8:16], [B[3][1]], [sm_k])
            for h in range(4):
                self.tt("pool", sm[0:64, 12 + h:13 + h], sm[0:64, 4 + 2 * h:5 + 2 * h], sm[0:64, h:h + 1], ALU.subtract, [sm_k], [sm_k])
                self.tt("pool", sm[64:128, 12 + h:13 + h], sm[64:128, 5 + 2 * h:6 + 2 * h], sm[64:128, h:h + 1], ALU.subtract, [sm_k], [sm_k])
            self.act(sm[:, 0:16], sm[:, 0:16], AF.Exp, [sm_k], [sm_k])
            self.ts("pool", sm[:, 16:20], gb[:, 4:8], -1.0, ALU.mult, [gb_k, sm_k], [sm_k])
            self.tt("pool", sm[:, 20:24], gb[:, 4:8], sm[:, 0:4], ALU.mult, [gb_k, sm_k], [sm_k])
            self.act(F(dec[:]), B[0][0][:], AF.Exp, [B[0][1]], [dec_k])
            self.tt("pool", dmI[:], dec[:], MI.unsqueeze(1).to_broadcast([128, 4, 128]), ALU.mult, [dec_k, ck], [dmI_k])
            self.tt("pool", dmS[:], dec[:], MS.unsqueeze(1).to_broadcast([128, 4, 128]), ALU.mult, [dec_k, ck], [dmS_k])
            for h in range(4):
                self.mm(B[1][0][:, h * 128:(h + 1) * 128], kT[:, h, :], kT[:, h, :], True, True, [kT_k], [B[1][1]])
                self.mm(B[2][0][:, h * 128:(h + 1) * 128], qT[:, h, :], kT[:, h, :], True, True, [qT_k, kT_k], [B[2][1]])
            self.tt("dve", F(qk[:]), B[2][0][:], F(dmI[:]), ALU.mult, [B[2][1], dmI_k], [qk_k])
            for h in range(4):
                self.stt(Nn[:, h, :], B[1][0][:, h * 128:(h + 1) * 128], sm[:, 16 + h:17 + h], dmS[:, h, :], ALU.mult, ALU.mult, [B[1][1], sm_k, dmS_k], [Nn_k])
            for h in range(4):
                self.tr(B[0][0][:, h * 128:(h + 1) * 128], Nn[:, h, :], self.ident_f, [Nn_k, ck], [B[0][1]])
                self.tr(B[1][0][:, h * 128:(h + 1) * 128], qk[:, h, :], self.ident_f, [qk_k, ck], [B[1][1]])
            P, P_k = Ps[0]
            Q, Q_k = Nn, Nn_k
            self.cp("act", F(P[:]), B[0][0][:], [B[0][1]], [P_k])
            self.cp("act", F(qkTb[:]), B[1][0][:], [B[1][1]], [qkTb_k])
            if stop == 'A':
                continue
            self.tt("pool", X[:], P[:], self.ident_f.unsqueeze(1).to_broadcast([128, 4, 128]), ALU.add, [P_k, ck], [X_k])
            for lv in range(1, 6):
                Pn, Pn_k = Ps[lv % 2]
                Qn, Qn_k = Qs[lv % 2]
                for h in range(4):
                    if lv < 5:
                        self.mm(B[1][0][:, h * 128:(h + 1) * 128], Q[:, h, :], P[:, h, :], True, True, [Q_k, P_k], [B[1][1]])
                    self.mm(B[2][0][:, h * 128:(h + 1) * 128], P[:, h, :], Q[:, h, :], True, True, [Q_k, P_k], [B[2][1]])
                if lv < 5:
                    self.cp("act", F(Pn[:]), B[1][0][:], [B[1][1]], [Pn_k])
                self.cp("dve", F(Qn[:]), B[2][0][:], [B[2][1]], [Qn_k])
                for h in range(4):
                    self.mm(B[0][0][:, h * 128:(h + 1) * 128], Qn[:, h, :], X[:, h, :], True, True, [Qn_k, X_k], [B[0][1]])
                self.tt("dve", F(X[:]), F(X[:]), B[0][0][:], ALU.add, [X_k, B[0][1]], [X_k])
                P, P_k, Q, Q_k = Pn, Pn_k, Qn, Qn_k
            if stop == 'B':
                continue
            for h in range(4):
                self.tr(B[1][0][:, h * 128:(h + 1) * 128], vT[:, h, :], self.ident_f, [vT_k, ck], [B[1][1]])
                self.tr(B[2][0][:, h * 128:(h + 1) * 128], kT[:, h, :], self.ident_f, [kT_k, ck], [B[2][1]])
            for h in range(4):
                self.act(vb[:, h, :], B[1][0][:, h * 128:(h + 1) * 128], AF.Copy, [B[1][1], gb_k], [vb_k], scale=gb[:, 4 + h:5 + h])
                self.ts("dve", kbg[:, h, :], B[2][0][:, h * 128:(h + 1) * 128], sm[:, 20 + h:21 + h], ALU.mult, [B[2][1], sm_k], [kbg_k])
                self.act(kdb[:, h, :], B[2][0][:, h * 128:(h + 1) * 128], AF.Copy, [B[2][1], sm_k], [kdb_k], scale=sm[:, 12 + h:13 + h])
            for h in range(4):
                self.mm(B[0][0][:, h * 128:(h + 1) * 128], X[:, h, :], vb[:, h, :], True, True, [X_k, vb_k], [B[0][1]])
                self.mm(B[1][0][:, h * 128:(h + 1) * 128], kbg[:, h, :], X[:, h, :], True, True, [X_k, kbg_k], [B[1][1]])
            self.cp("act", F(u[:]), B[0][0][:], [B[0][1]], [u_k])
            self.cp("dve", F(wTb[:]), B[1][0][:], [B[1][1]], [wTb_k])
            if stop == 'C':
                continue
            self.cp("pool", qTb[:], qT[:], [qT_k], [qTb_k])
            for ch in range(2):
                r = slice(ch * 64, ch * 64 + 64)
                for h in range(4):
                    self.mm(B[4][0][r, h * 128:(h + 1) * 128], wTb[:, h, r], Sb[:, h, :], True, True, [wTb_k, Sb_k], [B[4][1]])
                self.tt("dve", F(vnb[r]), F(u[r]), B[4][0][r, :], ALU.subtract, [u_k, B[4][1]], [vnb_k])
                for h in range(4):
                    self.mm(B[5][0][r, h * 128:(h + 1) * 128], qTb[:, h, r], Sb[:, h, :], True, True, [qTb_k, Sb_k], [B[5][1]])
                for h in range(4):
                    self.mm(B[6][0][r, h * 128:(h + 1) * 128], qkTb[r, h, r], vnb[r, h, :], True, True, [qkTb_k, vnb_k], [B[6][1]])
                self.cp("act", F(o2s[r]), B[6][0][r, :], [B[6][1]], [o2s_k])
                for h in range(4):
                    self.stt(o[r, h, :], B[5][0][r, h * 128:(h + 1) * 128], sm[r, h:h + 1], o2s[r, h, :], ALU.mult, ALU.add, [B[5][1], sm_k, o2s_k], [o_k])
                for h in range(4):
                    self.mm(B[7][0][:, h * 128:(h + 1) * 128], kdb[r, h, :], vnb[r, h, :], True, True, [kdb_k, vnb_k], [B[7][1]])
                for h in range(4):
                    self.stt(Sst[:, h, :], Sst[:, h, :], sm[:, 4 + 2 * h + ch:5 + 2 * h + ch], B[7][0][:, h * 128:(h + 1) * 128], ALU.mult, ALU.add, [S_k, sm_k, B[7][1]], [S_k])
                self.cp("pool", Sb[:], Sst[:], [S_k], [Sb_k])
            if stop == 'D':
                continue
            for h in range(4):
                self.tr(B[2][0][:, h * 128:(h + 1) * 128], o[:, h, :], self.ident_f, [o_k, ck], [B[2][1]])
            self.cp("act", F(oTs[:]), B[2][0][:], [B[2][1]], [oTs_k])
            self.act(F(sqb[:]), B[2][0][:], AF.Square, [B[2][1]], [sqb_k])
            self.mm(B[3][0][:], self.ones_b, F(sqb[:]), True, True, [sqb_k, self.cb_k], [B[3][1]])
            self.act(F(rr[:]), B[3][0][:], AF.Sqrt, [B[3][1]], [rr_k], scale=1.0 / 128, bias=EPS)
            self.recip(F(rr[:]), F(rr[:]), [rr_k], [rr_k])
            self.stt(F(fin32[:]), F(oTs[:]), self.vcol(("gdn_out_norm", j)), F(rr[:]), ALU.mult, ALU.mult, [oTs_k, rr_k, self.vec_k], [fin32_k])
            fb, fb_k = finb.next()
            self.tt("pool", fb[:], fin32[:], zs[:], ALU.mult, [fin32_k, zs_k], [fb_k])
            self.dma(oT_d[:, 4:8, t0:t0 + 128], fb[:], [fb_k], [self.scr_k["even_oT"][sci // 4]])
        self.end_phase()

    def phase_zero_gdn(self):
        self.begin_phase()
        z, z_k = self.sb([128, T], BF16, "z"), Tok()
        self.f.op("pool", lambda e: e.memset(z[:], 0.0), [], [z_k])
        for c in range(4, 8):
            for tt in range(NT):
                self.dma(self.scr["even_oT"][c * 128:(c + 1) * 128, tt * T:(tt + 1) * T], z[:], [z_k], [self.scr_k["even_oT"][tt]])
        self.end_phase()

    def phase_copy(self):
        self.begin_phase()
        xstage = self.ring(4, [128, T], F32, "xst")
        for tt in range(NT):
            for c in range(8):
                xs, xs_k = xstage.next()
                self.dma(xs[:], self.xsrc[c * 128:(c + 1) * 128, tt * T:(tt + 1) * T], [self.xtok[tt][c]], [xs_k])
                self.dma(self.xres[c * 128:(c + 1) * 128, tt * T:(tt + 1) * T], xs[:], [xs_k], [self.xtok[tt][c]])
        self.end_phase()
        self.xsrc = self.xres

    def build(self):
        self.setup()
        for ph in self.phases:
            kind = ph[0]
            if kind == "ffn":
                self.phase_ffn(ph[1], ph[2])
            elif kind == "ple":
                self.phase_ple(ph[1])
            elif kind == "copy":
                self.phase_copy()
            elif kind == "evenproj":
                self.phase_even_proj(ph[1], *ph[2:])
            elif kind == "mla":
                self.phase_mla_attn(ph[1], *ph[2:])
            elif kind == "evenout":
                self.phase_even_out(ph[1], *ph[2:])
            elif kind == "gdn":
                self.phase_gdn(ph[1], *ph[2:])
            elif kind == "oddout":
                self.phase_even_out(ph[1], ph[2] if len(ph) > 2 else NT, "odd_w_out")
            elif kind == "zero_gdn":
                self.phase_zero_gdn()
            elif kind == "oddproj":
                self.phase_odd_proj(ph[1])
            elif kind == "dsa":
                self.phase_dsa(ph[1], *ph[2:])
            else:
                raise ValueError(ph)
        self.f.barrier()
        self.f.emit()
        self.gst.close()
        self.f.close()
        return self.nc


def all_phases():
    ph = []
    for i in range(DEPTH):
        ph.append(("ffn", i, 1))
        if i % 2 == 0:
            ph += [("evenproj", i), ("mla", i), ("gdn", i), ("evenout", i)]
        else:
            ph += [("oddproj", i), ("dsa", i), ("oddout", i)]
        ph.append(("ffn", i, 2))
        ph.append(("ple", i))
    return ph


def make_in_maps(inputs, cores):
    vecs = pack_vecs(inputs)
    consts = make_consts()
    rope = make_rope()
    x = np.asarray(inputs["x"])
    p = np.asarray(inputs["p"])
    shared = {k: np.ascontiguousarray(np.asarray(inputs[k], dtype=np.float32)) for k in WEIGHT_SHAPES}
    maps = []
    for b in cores:
        m = dict(shared)
        m["xT"] = np.ascontiguousarray(x[b].T)
        m["pT"] = np.ascontiguousarray(p[:, b].transpose(0, 2, 1))
        m["vecs"] = vecs
        m["consts"] = consts
        m["rope"] = rope
        maps.append(m)
    return maps


def kernel(**inputs):
    prog = Prog(all_phases())
    nc = prog.build()
    maps = make_in_maps(inputs, list(range(8)))
    res = run_bass_kernel_spmd(nc, maps, core_ids=list(range(8)))
    out = np.stack([np.ascontiguousarray(r["outT"].T) for r in res.results], axis=0)
    return out.astype(np.float32)
```
